# Optimizing a Trainium2 kernel written in Bass

```python
import math
import jax
import jax.numpy as jnp
from jax import lax
import numpy as np

D_MODEL = 1024
BATCH = 32
SEQ = 2048
DEPTH = 2

GRID_W = 64
CTX_LEN = 256
F32 = jnp.float32
EPS = 1e-6
HEAD_DIM = 64
ROPE_FREQS = HEAD_DIM // 4
ROPE_BASE = 10000.0
QUERY_BLOCK = 128
S5_WIDTH = D_MODEL // 2
S5_GROUP_CH = 16
S5_GROUPS = S5_WIDTH // S5_GROUP_CH
S5_STATE = 64
S5_DT_MIN = 1e-3
S5_DT_MAX = 1e-1
GQA_HEADS = (D_MODEL // 2) // HEAD_DIM
GQA_KV_HEADS = GQA_HEADS // 2
HYENA_WIDTH = D_MODEL // 2
HYENA_ORDER = 2
HYENA_BANDS = 16
HYENA_POS_DIM = 1 + 2 * HYENA_BANDS
HYENA_HIDDEN = 64
SHORT_CONV = 3
HYENA_DECAY_MIN = 3.07
HYENA_DECAY_MAX = 15.35
NA_HEADS = (D_MODEL // 2) // HEAD_DIM
NA_WIN_ROWS = 8
NA_WIN_COLS = 16
FFN_DIM = 7 * D_MODEL // 2
MOE_EXPERTS = 8
MOE_TOP_K = 2
MOE_FFN_DIM = 7 * D_MODEL // 2
MOE_BLOCK = 256
EVEN_IN = S5_WIDTH + (GQA_HEADS + 2 * GQA_KV_HEADS) * HEAD_DIM
EVEN_MIX = S5_WIDTH + GQA_HEADS * HEAD_DIM
ODD_IN = 3 * HYENA_WIDTH + 3 * NA_HEADS * HEAD_DIM
ODD_MIX = HYENA_WIDTH + NA_HEADS * HEAD_DIM

kernel_name = 'hybrid_s5_gqa_hyena_natten_moe_flow_block'


def rmsnorm(x, g):
    xf = x.astype(F32)
    y = xf * lax.rsqrt(jnp.mean(xf * xf, axis=-1, keepdims=True) + EPS)
    return (y * g.astype(F32)).astype(x.dtype)


def adaln(cvec, w, b):
    m = jax.nn.silu(cvec) @ w + b
    return jnp.split(m[:, None, :], 6, axis=-1)


def modulate(h, shift, scale):
    return h * (1.0 + scale) + shift


def swiglu(h, w1, w3, w2):
    return (jax.nn.silu(h @ w1) * (h @ w3)) @ w2


def s5_discretise(lam_re, lam_im, log_dt, b_re, b_im):
    dt = jnp.exp(log_dt.astype(F32))[:, None]
    lr = lam_re.astype(F32)
    li = lam_im.astype(F32)
    mag = jnp.exp(lr * dt)
    ar = mag * jnp.cos(li * dt)
    ai = mag * jnp.sin(li * dt)
    nr = ar - 1.0
    den = lr * lr + li * li
    kr = (nr * lr + ai * li) / den
    ki = (ai * lr - nr * li) / den
    br = b_re.astype(F32)
    bi = b_im.astype(F32)
    bbr = kr[..., None] * br - ki[..., None] * bi
    bbi = kr[..., None] * bi + ki[..., None] * br
    return ar, ai, bbr, bbi


def s5_drive(u, bbr, bbi):
    return (jnp.einsum('blgp,gnp->blgn', u, bbr), jnp.einsum('blgp,gnp->blgn', u, bbi))


def complex_affine_combine(e1, e2):
    a1r, a1i, b1r, b1i = e1
    a2r, a2i, b2r, b2i = e2
    return (a2r * a1r - a2i * a1i,
            a2r * a1i + a2i * a1r,
            a2r * b1r - a2i * b1i + b2r,
            a2r * b1i + a2i * b1r + b2i)


def s5_scan(ar, ai, br, bi, reverse):
    L = br.shape[1]
    a_r = jnp.broadcast_to(ar, (1, L) + ar.shape)
    a_i = jnp.broadcast_to(ai, (1, L) + ai.shape)
    _, _, hr, hi = lax.associative_scan(complex_affine_combine, (a_r, a_i, br, bi), reverse=reverse, axis=1)
    return hr, hi


def s5_readout(hr, hi, cr, ci):
    return (jnp.einsum('blgn,gpn->blgp', hr, cr.astype(F32))
            - jnp.einsum('blgn,gpn->blgp', hi, ci.astype(F32)))


def s5_glu(y, w_glu):
    g = jax.nn.gelu(y)
    return g * jax.nn.sigmoid(g @ w_glu.astype(F32))


def s5_mixer(u_x, u_c, lam_re, lam_im, log_dt, b_re, b_im, c_re, c_im, d_skip, w_glu, ctx_out):
    bsz, L, width = u_x.shape
    lc = u_c.shape[1]
    ux = u_x.astype(F32).reshape(bsz, L, S5_GROUPS, S5_GROUP_CH)
    uc = u_c.astype(F32).reshape(bsz, lc, S5_GROUPS, S5_GROUP_CH)
    d = d_skip.astype(F32).reshape(S5_GROUPS, S5_GROUP_CH)
    y_x = ux * d
    y_c = uc * d if ctx_out else None
    for direction in range(2):
        rev = direction == 1
        ar, ai, bbr, bbi = s5_discretise(lam_re[direction], lam_im[direction], log_dt[direction],
                                         b_re[direction], b_im[direction])
        hcr, hci = s5_scan(ar, ai, *s5_drive(uc, bbr, bbi), rev)
        end = 0 if rev else lc - 1
        h0r, h0i = hcr[:, end], hci[:, end]
        bxr, bxi = s5_drive(ux, bbr, bbi)
        first = L - 1 if rev else 0
        bxr = bxr.at[:, first].add(ar * h0r - ai * h0i)
        bxi = bxi.at[:, first].add(ar * h0i + ai * h0r)
        hxr, hxi = s5_scan(ar, ai, bxr, bxi, rev)
        y_x = y_x + s5_readout(hxr, hxi, c_re[direction], c_im[direction])
        if ctx_out:
            y_c = y_c + s5_readout(hcr, hci, c_re[direction], c_im[direction])
    out_x = s5_glu(y_x.reshape(bsz, L, width), w_glu)
    out_c = s5_glu(y_c.reshape(bsz, lc, width), w_glu) if ctx_out else None
    return out_x, out_c


def axial_rope_angles(L):
    t = jnp.arange(L)
    pos = jnp.stack([t // GRID_W, t % GRID_W], axis=-1).astype(F32)
    inv = ROPE_BASE ** (-jnp.arange(ROPE_FREQS, dtype=F32) / ROPE_FREQS)
    return pos[:, :, None] * inv


def apply_axial_rope(x, ang):
    b, L, h, dh = x.shape
    xs = x.astype(F32).reshape(b, L, h, 2, 2, ROPE_FREQS)
    x1, x2 = xs[..., 0, :], xs[..., 1, :]
    cos = jnp.cos(ang)[None, :, None]
    sin = jnp.sin(ang)[None, :, None]
    out = jnp.stack([x1 * cos - x2 * sin, x2 * cos + x1 * sin], axis=-2)
    return out.reshape(b, L, h, dh).astype(x.dtype)


def attend(q, k, v):
    s = jnp.einsum('bqhgd,bkhd->bhgqk', q, k).astype(F32) * (HEAD_DIM ** -0.5)
    p = jax.nn.softmax(s, axis=-1).astype(v.dtype)
    return jnp.einsum('bhgqk,bkhd->bqhgd', p, v)


def gqa_mixer(q_x, k_x, v_x, q_c, k_c, v_c, q_g, k_g, ang, ctx_out):
    b, L = q_x.shape[:2]
    lc = q_c.shape[1]
    grp = GQA_HEADS // GQA_KV_HEADS
    q_x = apply_axial_rope(rmsnorm(q_x.reshape(b, L, GQA_HEADS, HEAD_DIM), q_g), ang)
    k_x = apply_axial_rope(rmsnorm(k_x.reshape(b, L, GQA_KV_HEADS, HEAD_DIM), k_g), ang)
    v_x = v_x.reshape(b, L, GQA_KV_HEADS, HEAD_DIM)
    k_c = rmsnorm(k_c.reshape(b, lc, GQA_KV_HEADS, HEAD_DIM), k_g)
    v_c = v_c.reshape(b, lc, GQA_KV_HEADS, HEAD_DIM)
    k_all = jnp.concatenate([k_x, k_c], axis=1)
    v_all = jnp.concatenate([v_x, v_c], axis=1)
    nb = L // QUERY_BLOCK
    qb = q_x.reshape(b, nb, QUERY_BLOCK, GQA_KV_HEADS, grp, HEAD_DIM).transpose(1, 0, 2, 3, 4, 5)
    o = lax.map(lambda qblk: attend(qblk, k_all, v_all), qb)
    o_x = o.transpose(1, 0, 2, 3, 4, 5).reshape(b, L, GQA_HEADS * HEAD_DIM)
    o_c = None
    if ctx_out:
        qc = rmsnorm(q_c.reshape(b, lc, GQA_HEADS, HEAD_DIM), q_g).reshape(b, lc, GQA_KV_HEADS, grp, HEAD_DIM)
        o_c = attend(qc, k_c, v_c).reshape(b, lc, GQA_HEADS * HEAD_DIM)
    return o_x, o_c


def even_mixer(hx, hc, w_in, w_out, lam_re, lam_im, log_dt, b_re, b_im, c_re, c_im, d_skip, w_glu,
               q_g, k_g, ang, ctx_out):
    splits = [S5_WIDTH, S5_WIDTH + GQA_HEADS * HEAD_DIM, S5_WIDTH + (GQA_HEADS + GQA_KV_HEADS) * HEAD_DIM]
    u_x, q_x, k_x, v_x = jnp.split(hx @ w_in, splits, axis=-1)
    u_c, q_c, k_c, v_c = jnp.split(hc @ w_in, splits, axis=-1)
    a_x, a_c = s5_mixer(u_x, u_c, lam_re, lam_im, log_dt, b_re, b_im, c_re, c_im, d_skip, w_glu, ctx_out)
    g_x, g_c = gqa_mixer(q_x, k_x, v_x, q_c, k_c, v_c, q_g, k_g, ang, ctx_out)
    ox = jnp.concatenate([a_x.astype(g_x.dtype), g_x], axis=-1) @ w_out
    oc = jnp.concatenate([a_c.astype(g_c.dtype), g_c], axis=-1) @ w_out if ctx_out else None
    return ox, oc


def short_conv(x, w, b):
    y = lax.conv_general_dilated(x, w[:, None, :].astype(x.dtype), window_strides=(1,),
                                 padding=((SHORT_CONV // 2, SHORT_CONV // 2),),
                                 dimension_numbers=('NWC', 'WIO', 'NWC'),
                                 feature_group_count=x.shape[-1])
    return y + b


def hyena_filter_spectrum(L, f_w1, f_b1, f_w2, f_b2, f_w3, f_freq, f_decay):
    k = jnp.arange(L, dtype=F32)
    t = k / max(L - 1, 1)
    bands = jnp.linspace(1e-4, HYENA_BANDS - 1, HYENA_BANDS, dtype=F32)
    ang = (2.0 * math.pi / L) * k[:, None] * bands[None, :]
    z = jnp.concatenate([t[:, None], jnp.cos(ang), -jnp.sin(ang)], axis=-1)
    freq = f_freq.astype(F32)
    h = jnp.sin(freq * (z @ f_w1.astype(F32) + f_b1.astype(F32)))
    h = jnp.sin(freq * (h @ f_w2.astype(F32) + f_b2.astype(F32)))
    h = h @ f_w3.astype(F32)
    h = h * jnp.exp(-t[:, None] * jnp.abs(f_decay.astype(F32)))
    h = h / (jnp.sum(jnp.abs(h), axis=0, keepdims=True) + EPS)
    h = h.reshape(L, 2, HYENA_ORDER, HYENA_WIDTH)
    spec_fwd = jnp.fft.rfft(h[:, 0], n=2 * L, axis=0)
    spec_bwd = jnp.fft.rfft(h[:, 1], n=2 * L, axis=0)
    return spec_fwd + jnp.conj(spec_bwd)


def hyena_mixer(proj, conv_w, conv_b, spec, hy_d):
    L = proj.shape[1]
    u = short_conv(proj, conv_w, conv_b).astype(F32)
    v, g1, g2 = jnp.split(u, 3, axis=-1)
    z = v
    for o, gate in enumerate((g1, g2)):
        zf = jnp.fft.rfft(z, n=2 * L, axis=1)
        y = jnp.fft.irfft(zf * spec[None, :, o], n=2 * L, axis=1)[:, :L]
        z = gate * (y + hy_d[o].astype(F32) * z)
    return z


def na_mixer(q_x, k_x, v_x, k_c, v_c, rpb):
    b, L = q_x.shape[:2]
    rows = L // GRID_W
    wr = min(NA_WIN_ROWS, rows)
    shp = (b, rows, GRID_W, NA_HEADS, HEAD_DIM)
    q_rows = q_x.reshape(shp).transpose(1, 0, 2, 3, 4)
    k_grid = k_x.reshape(shp)
    v_grid = v_x.reshape(shp)
    col = jnp.arange(GRID_W)
    col_start = jnp.clip(col - NA_WIN_COLS // 2, 0, GRID_W - NA_WIN_COLS)
    col_mask = (col[None, :] >= col_start[:, None]) & (col[None, :] < col_start[:, None] + NA_WIN_COLS)
    dc_idx = jnp.clip(col[None, :] - col[:, None] + NA_WIN_COLS - 1, 0, 2 * NA_WIN_COLS - 2)
    scale = HEAD_DIM ** -0.5

    def row_step(args):
        r, q = args
        r0 = jnp.clip(r - wr // 2, 0, rows - wr)
        k_band = lax.dynamic_slice_in_dim(k_grid, r0, wr, axis=1)
        v_band = lax.dynamic_slice_in_dim(v_grid, r0, wr, axis=1)
        dr_idx = r0 + jnp.arange(wr) - r + NA_WIN_ROWS - 1
        bias = rpb[:, dr_idx[None, :, None], dc_idx[:, None, :]].astype(F32)
        s_loc = jnp.einsum('bqhd,bwkhd->bhqwk', q, k_band).astype(F32) * scale + bias
        s_loc = jnp.where(col_mask[:, None, :], s_loc, -jnp.inf)
        s_ctx = jnp.einsum('bqhd,bchd->bhqc', q, k_c).astype(F32) * scale
        s = jnp.concatenate([s_loc.reshape(b, NA_HEADS, GRID_W, wr * GRID_W), s_ctx], axis=-1)
        p = jax.nn.softmax(s, axis=-1).astype(v_band.dtype)
        p_loc = p[..., :wr * GRID_W].reshape(b, NA_HEADS, GRID_W, wr, GRID_W)
        p_ctx = p[..., wr * GRID_W:]
        return (jnp.einsum('bhqwk,bwkhd->bqhd', p_loc, v_band)
                + jnp.einsum('bhqc,bchd->bqhd', p_ctx, v_c))

    o = lax.map(row_step, (jnp.arange(rows), q_rows))
    return o.transpose(1, 0, 2, 3, 4).reshape(b, L, NA_HEADS * HEAD_DIM)


def odd_mixer(hx, hc, w_in, w_out, conv_w, conv_b, f_w1, f_b1, f_w2, f_b2, f_w3, f_freq, f_decay,
              hy_d, rpb, ctx_out):
    b, L, _ = hx.shape
    lc = hc.shape[1]
    hy_w = 3 * HYENA_WIDTH
    na_w = NA_HEADS * HEAD_DIM

    def heads(t):
        return t.reshape(t.shape[0], t.shape[1], NA_HEADS, HEAD_DIM)

    px = hx @ w_in
    hy_x, q_x, k_x, v_x = jnp.split(px, [hy_w, hy_w + na_w, hy_w + 2 * na_w], axis=-1)
    if ctx_out:
        hy_c, q_c, k_c, v_c = jnp.split(hc @ w_in, [hy_w, hy_w + na_w, hy_w + 2 * na_w], axis=-1)
    else:
        k_c, v_c = jnp.split(hc @ w_in[:, hy_w + na_w:], 2, axis=-1)
    filt = (f_w1, f_b1, f_w2, f_b2, f_w3, f_freq, f_decay)
    o_hy = hyena_mixer(hy_x, conv_w, conv_b, hyena_filter_spectrum(L, *filt), hy_d)
    o_na = na_mixer(heads(q_x), heads(k_x), heads(v_x), heads(k_c), heads(v_c), rpb)
    ox = jnp.concatenate([o_hy.astype(o_na.dtype), o_na], axis=-1) @ w_out
    oc = None
    if ctx_out:
        oc_hy = hyena_mixer(hy_c, conv_w, conv_b, hyena_filter_spectrum(lc, *filt), hy_d)
        oc_na = attend(heads(q_c)[:, :, :, None, :], heads(k_c), heads(v_c)).reshape(b, lc, na_w)
        oc = jnp.concatenate([oc_hy.astype(oc_na.dtype), oc_na], axis=-1) @ w_out
    return ox, oc


def moe_swiglu(h, router, w1, w3, w2):
    b, L, d = h.shape
    T = b * L
    n_assign = T * MOE_TOP_K
    xf = h.reshape(T, d)
    logits = (xf @ router).astype(F32)
    top_val, top_idx = lax.top_k(logits, MOE_TOP_K)
    top_w = jax.nn.softmax(top_val, axis=-1)
    e_flat = top_idx.reshape(-1)
    tok_flat = jnp.arange(n_assign) // MOE_TOP_K
    w_flat = top_w.reshape(-1)
    order = jnp.argsort(e_flat)
    e_s, tok_s, w_s = e_flat[order], tok_flat[order], w_flat[order]
    counts = jnp.bincount(e_flat, length=MOE_EXPERTS)
    start = jnp.cumsum(counts) - counts
    padded = (counts + MOE_BLOCK - 1) // MOE_BLOCK * MOE_BLOCK
    pend = jnp.cumsum(padded)
    pstart = pend - padded
    slot = pstart[e_s] + jnp.arange(n_assign) - start[e_s]
    n_slots = n_assign + MOE_EXPERTS * MOE_BLOCK
    slot_tok = jnp.full((n_slots,), T, jnp.int32).at[slot].set(tok_s.astype(jnp.int32))
    block_e = jnp.clip(jnp.searchsorted(pend, jnp.arange(n_slots // MOE_BLOCK) * MOE_BLOCK, side='right'),
                       0, MOE_EXPERTS - 1)
    x_pad = jnp.concatenate([xf, jnp.zeros((1, d), xf.dtype)], axis=0)

    def run_block(args):
        toks, e = args
        xb = x_pad[toks]
        return (jax.nn.silu(xb @ w1[e]) * (xb @ w3[e])) @ w2[e]

    y_slots = lax.map(run_block, (slot_tok.reshape(-1, MOE_BLOCK), block_e)).reshape(n_slots, d)
    y = jax.ops.segment_sum(y_slots[slot] * w_s[:, None].astype(y_slots.dtype), tok_s, num_segments=T)
    return y.reshape(b, L, d)


def setup_inputs(seed: int = 0) -> dict:
    key = jax.random.key(seed)
    keys = iter(jax.random.split(key, 64))
    ne, no = (DEPTH + 1) // 2, DEPTH // 2
    G, N, P = S5_GROUPS, S5_STATE, S5_GROUP_CH

    def normal(shape, scale):
        return scale * jax.random.normal(next(keys), shape, F32)

    def gain(shape):
        return 1.0 + normal(shape, 0.05)

    D = D_MODEL
    return {
        'x': normal((BATCH, SEQ, D), 1.0),
        'c': normal((BATCH, D), 1.0),
        'ctx': normal((BATCH, CTX_LEN, D), 1.0),
        'c_ctx': normal((D,), 1.0),
        'mod_w': normal((DEPTH, D, 6 * D), D ** -0.5),
        'mod_b': normal((DEPTH, 6 * D), 0.02),
        'norm1_g': gain((DEPTH, D)),
        'norm2_g': gain((DEPTH, D)),
        'ev_w_in': normal((ne, D, EVEN_IN), D ** -0.5),
        'ev_w_out': normal((ne, EVEN_MIX, D), EVEN_MIX ** -0.5),
        's5_lam_re': -0.5 + normal((ne, 2, G, N), 0.01),
        's5_lam_im': math.pi * jnp.arange(N, dtype=F32) + normal((ne, 2, G, N), 0.01),
        's5_log_dt': jax.random.uniform(next(keys), (ne, 2, G), F32, math.log(S5_DT_MIN), math.log(S5_DT_MAX)),
        's5_b_re': normal((ne, 2, G, N, P), (2 * P) ** -0.5),
        's5_b_im': normal((ne, 2, G, N, P), (2 * P) ** -0.5),
        's5_c_re': normal((ne, 2, G, P, N), (2 * N) ** -0.5),
        's5_c_im': normal((ne, 2, G, P, N), (2 * N) ** -0.5),
        's5_d': normal((ne, S5_WIDTH), 1.0),
        's5_w_glu': normal((ne, S5_WIDTH, S5_WIDTH), S5_WIDTH ** -0.5),
        'gqa_q_g': gain((ne, HEAD_DIM)),
        'gqa_k_g': gain((ne, HEAD_DIM)),
        'ffn_w1': normal((ne, D, FFN_DIM), D ** -0.5),
        'ffn_w3': normal((ne, D, FFN_DIM), D ** -0.5),
        'ffn_w2': normal((ne, FFN_DIM, D), FFN_DIM ** -0.5),
        'od_w_in': normal((no, D, ODD_IN), D ** -0.5),
        'od_w_out': normal((no, ODD_MIX, D), ODD_MIX ** -0.5),
        'hy_conv_w': normal((no, SHORT_CONV, 3 * HYENA_WIDTH), SHORT_CONV ** -0.5),
        'hy_conv_b': normal((no, 3 * HYENA_WIDTH), 0.02),
        'hy_w1': normal((no, HYENA_POS_DIM, HYENA_HIDDEN), HYENA_POS_DIM ** -0.5),
        'hy_b1': normal((no, HYENA_HIDDEN), 0.1),
        'hy_w2': normal((no, HYENA_HIDDEN, HYENA_HIDDEN), HYENA_HIDDEN ** -0.5),
        'hy_b2': normal((no, HYENA_HIDDEN), 0.1),
        'hy_w3': normal((no, HYENA_HIDDEN, 2 * HYENA_ORDER * HYENA_WIDTH), HYENA_HIDDEN ** -0.5),
        'hy_freq': 1.0 + normal((no, HYENA_HIDDEN), 0.05),
        'hy_decay': jax.random.uniform(next(keys), (no, 2 * HYENA_ORDER * HYENA_WIDTH), F32,
                                       HYENA_DECAY_MIN, HYENA_DECAY_MAX),
        'hy_d': normal((no, HYENA_ORDER, HYENA_WIDTH), 1.0),
        'na_rpb': normal((no, NA_HEADS, 2 * NA_WIN_ROWS - 1, 2 * NA_WIN_COLS - 1), 0.02),
        'moe_router': normal((no, D, MOE_EXPERTS), D ** -0.5),
        'moe_w1': normal((no, MOE_EXPERTS, D, MOE_FFN_DIM), D ** -0.5),
        'moe_w3': normal((no, MOE_EXPERTS, D, MOE_FFN_DIM), D ** -0.5),
        'moe_w2': normal((no, MOE_EXPERTS, MOE_FFN_DIM, D), MOE_FFN_DIM ** -0.5),
        'final_g': gain((D,)),
    }


def reference(x, c, ctx, c_ctx, mod_w, mod_b, norm1_g, norm2_g,
              ev_w_in, ev_w_out, s5_lam_re, s5_lam_im, s5_log_dt, s5_b_re, s5_b_im, s5_c_re, s5_c_im,
              s5_d, s5_w_glu, gqa_q_g, gqa_k_g, ffn_w1, ffn_w3, ffn_w2,
              od_w_in, od_w_out, hy_conv_w, hy_conv_b, hy_w1, hy_b1, hy_w2, hy_b2, hy_w3, hy_freq,
              hy_decay, hy_d, na_rpb, moe_router, moe_w1, moe_w3, moe_w2, final_g):
    L = x.shape[1]
    ang = axial_rope_angles(L)
    cx = ctx

    def channel_mix(h, layer):
        i = layer // 2
        if layer % 2 == 0:
            return swiglu(h, ffn_w1[i], ffn_w3[i], ffn_w2[i])
        return moe_swiglu(h, moe_router[i], moe_w1[i], moe_w3[i], moe_w2[i])

    for layer in range(DEPTH):
        last = layer == DEPTH - 1
        i = layer // 2
        sh1, sc1, g1, sh2, sc2, g2 = adaln(c, mod_w[layer], mod_b[layer])
        csh1, csc1, cg1, csh2, csc2, cg2 = adaln(c_ctx[None], mod_w[layer], mod_b[layer])
        hx = modulate(rmsnorm(x, norm1_g[layer]), sh1, sc1)
        hc = modulate(rmsnorm(cx, norm1_g[layer]), csh1, csc1)
        if layer % 2 == 0:
            ox, oc = even_mixer(hx, hc, ev_w_in[i], ev_w_out[i], s5_lam_re[i], s5_lam_im[i], s5_log_dt[i],
                                s5_b_re[i], s5_b_im[i], s5_c_re[i], s5_c_im[i], s5_d[i], s5_w_glu[i],
                                gqa_q_g[i], gqa_k_g[i], ang, not last)
        else:
            ox, oc = odd_mixer(hx, hc, od_w_in[i], od_w_out[i], hy_conv_w[i], hy_conv_b[i], hy_w1[i], hy_b1[i],
                               hy_w2[i], hy_b2[i], hy_w3[i], hy_freq[i], hy_decay[i], hy_d[i], na_rpb[i],
                               not last)
        x = x + g1 * ox
        x = x + g2 * channel_mix(modulate(rmsnorm(x, norm2_g[layer]), sh2, sc2), layer)
        if not last:
            cx = cx + cg1 * oc
            cx = cx + cg2 * channel_mix(modulate(rmsnorm(cx, norm2_g[layer]), csh2, csc2), layer)
    return rmsnorm(x, final_g)
```

```python
import numpy as np
import ml_dtypes
from contextlib import ExitStack
import concourse.bass as bass
import concourse.mybir as mybir
from concourse.bass_utils import run_bass_kernel_spmd

F32 = mybir.dt.float32
BF16 = mybir.dt.bfloat16
AF = mybir.ActivationFunctionType
ALU = mybir.AluOpType

NCORES = 8
NB = 4
L = 2048
LC = 256
T = L + LC
D = 1024
KT = 8
EPS = 1e-6
NDMA = 48
import os as _os
S5E = _os.environ.get("S5E", "pool,dve,pool").split(",")


class Buf:
    __slots__ = ("w", "r", "name")

    def __init__(self, name=""):
        self.w = {}
        self.r = {}
        self.name = name


class Ctx:
    def __init__(self, nc):
        self.nc = nc
        self.eng = {"pe": nc.tensor, "act": nc.scalar, "dve": nc.vector, "pool": nc.gpsimd, "sp": nc.sync}
        self.csem = {e: nc.alloc_semaphore("c_" + e) for e in ("pe", "act", "dve", "pool")}
        self.tick = {e: 0 for e in ("pe", "act", "dve", "pool")}
        self.seen = {e: {} for e in self.eng}
        self.dsem = [nc.alloc_semaphore("d%d" % i) for i in range(NDMA)]
        self.dcum = [0] * NDMA
        self.dnext = 0
        self.nwait = 0
        self.ninst = 0
        self.psb = []
        for i in range(8):
            t = nc.alloc_psum_tensor("psb%d" % i, [128, 512], F32)
            self.psb.append((t, Buf("ps%d" % i)))
        self.psn = {}

    def _sem(self, k):
        return self.csem[k[1]] if k[0] == "c" else self.dsem[k[1]]

    def _wait(self, e, toks):
        need = {}
        for k, v in toks:
            if self.seen[e].get(k, 0) >= v:
                continue
            if need.get(k, 0) < v:
                need[k] = v
        for k, v in need.items():
            self.eng[e].wait_ge(self._sem(k), v)
            self.seen[e][k] = v
            self.nwait += 1

    def _deps(self, e, reads, writes, pwrites, is_dma):
        toks = []
        me = ("c", e)
        for b in reads:
            for k, v in b.w.items():
                if k == me and e == "pe":
                    continue
                toks.append((k, v))
        for b in writes:
            for k, v in list(b.w.items()) + list(b.r.items()):
                if k == me and not is_dma:
                    continue
                toks.append((k, v))
        for b in pwrites:
            for k, v in b.r.items():
                if k == me and not is_dma:
                    continue
                toks.append((k, v))
        return toks

    def _book(self, tok, reads, writes, pwrites):
        k, v = tok
        for b in reads:
            b.r[k] = v
        for b in writes:
            b.w = {k: v}
            b.r = {}
        for b in pwrites:
            b.w[k] = v

    def op(self, e, fn, reads=(), writes=(), pwrites=()):
        self._wait(e, self._deps(e, reads, writes, pwrites, False))
        inst = fn(self.eng[e])
        self.tick[e] += 1
        inst.then_inc(self.csem[e], 1)
        self.ninst += 1
        self._book((("c", e), self.tick[e]), reads, writes, pwrites)
        return inst

    def dma(self, q, out, in_, reads=(), writes=(), pwrites=(), **kw):
        self._wait(q, self._deps(q, reads, writes, pwrites, True))
        slot = self.dnext
        self.dnext = (self.dnext + 1) % NDMA
        if self.dcum[slot] > 0:
            self._wait(q, [(("d", slot), self.dcum[slot])])
        inst = self.eng[q].dma_start(out=out, in_=in_, **kw)
        self.dcum[slot] += 16
        inst.then_inc(self.dsem[slot], 16)
        self.ninst += 1
        self._book((("d", slot), self.dcum[slot]), reads, writes, pwrites)

    def barrier(self):
        toks = [(("c", e), self.tick[e]) for e in self.tick if self.tick[e] > 0]
        toks += [(("d", i), self.dcum[i]) for i in range(NDMA) if self.dcum[i] > 0]
        for e in self.eng:
            self._wait(e, toks)

    def finish(self):
        toks = [(("d", i), self.dcum[i]) for i in range(NDMA) if self.dcum[i] > 0]
        toks += [(("c", e), self.tick[e]) for e in self.tick if self.tick[e] > 0]
        self._wait("sp", toks)

    def ps(self, group, banks):
        i = self.psn.get(group, 0)
        self.psn[group] = i + 1
        return self.psb[banks[i % len(banks)]]

    def mm(self, out, lhsT, rhs, start, stop, reads, writes):
        return self.op("pe", lambda e: e.matmul(out, lhsT=lhsT, rhs=rhs, start=start, stop=stop), reads, writes)

    def tr(self, out, in_, ident, reads, writes):
        return self.op("pe", lambda e: e.transpose(out, in_, ident), reads, writes)

    def act(self, out, in_, func, reads, writes, scale=1.0, bias=None, pw=()):
        if bias is None:
            return self.op("act", lambda e: e.activation(out=out, in_=in_, func=func, scale=scale), reads, writes, pw)
        return self.op("act", lambda e: e.activation(out=out, in_=in_, func=func, scale=scale, bias=bias), reads, writes, pw)

    def tt(self, out, in0, in1, op, reads, writes, eng="dve", pw=()):
        return self.op(eng, lambda e: e.tensor_tensor(out=out, in0=in0, in1=in1, op=op), reads, writes, pw)

    def ts(self, out, in0, s1, op0, reads, writes, s2=None, op1=None, eng="dve", pw=()):
        if op1 is None:
            return self.op(eng, lambda e: e.tensor_scalar(out=out, in0=in0, scalar1=s1, scalar2=None, op0=op0), reads, writes, pw)
        return self.op(eng, lambda e: e.tensor_scalar(out=out, in0=in0, scalar1=s1, scalar2=s2, op0=op0, op1=op1), reads, writes, pw)

    def stt(self, out, in0, scalar, in1, op0, op1, reads, writes, pw=()):
        return self.op("dve", lambda e: e.scalar_tensor_tensor(out=out, in0=in0, scalar=scalar, in1=in1, op0=op0, op1=op1), reads, writes, pw)

    def cp(self, out, in_, reads, writes, eng="dve", pw=()):
        if eng == "act":
            return self.op("act", lambda e: e.copy(out=out, in_=in_), reads, writes, pw)
        return self.op(eng, lambda e: e.tensor_copy(out=out, in_=in_), reads, writes, pw)

    def recip(self, out, in_, reads, writes, pw=()):
        return self.op("dve", lambda e: e.reciprocal(out=out, in_=in_), reads, writes, pw)

    def memset(self, out, val, writes, eng="dve"):
        return self.op(eng, lambda e: e.memset(out, val), (), writes)


class SB:
    _n = [0]

    def __init__(self, es, nc, name, shape, dtype):
        SB._n[0] += 1
        self.t = es.enter_context(nc.sbuf_tensor("sb%d_%s" % (SB._n[0], name), list(shape), dtype))
        self.b = Buf(name)

    def __getitem__(self, idx):
        return self.t[idx]


BLOCKS = [(0, 256)] + [(256 + 512 * i, 512) for i in range(4)]


def build_program(stop_after=None, dbg=()):
    nc = bass.Bass("TRN2", target_bir_lowering=False)
    C = Ctx(nc)
    dbg = set(dbg)

    def din(name, shape, dt=F32):
        return nc.dram_tensor(name, list(shape), dt, kind="ExternalInput").ap()

    def dscr(name, shape, dt=F32):
        kind = "ExternalOutput" if name in dbg else "Internal"
        return nc.dram_tensor(name, list(shape), dt, kind=kind).ap()

    x_in = din("x", [NB, L, D])
    ctx_in = din("ctx", [NB, LC, D])
    cT_in = din("cT", [128, KT, 5])
    modw_in = din("mod_w", [2, D, 6 * D])
    modbT_in = din("mod_bT", [128, 2, 48])
    gvec_in = din("gvecT", [128, 5, KT])
    ident_in = din("ident", [128, 128])
    out_d = nc.dram_tensor("out", [NB, L, D], F32, kind="ExternalOutput").ap()

    xT_d = dscr("xT", [NB, D, T])
    xT_buf = [[Buf("xT%d_%d" % (b, i)) for i in range(len(BLOCKS))] for b in range(NB)]

    with ExitStack() as gs:
        ident = SB(gs, nc, "ident", [128, 128], F32)
        ones_bf = SB(gs, nc, "ones_bf", [128, 128], BF16)
        eps_t = SB(gs, nc, "eps_t", [128, 1], F32)
        modT = SB(gs, nc, "modT", [128, 2, 48, 5], F32)
        gvec = SB(gs, nc, "gvec", [128, 5, KT], F32)
        A1 = SB(gs, nc, "A1", [128, 2, 2, KT, 5], F32)
        C.dma("sp", ident[:], ident_in, writes=[ident.b])
        C.dma("sp", gvec[:], gvec_in, writes=[gvec.b])
        C.memset(ones_bf[:], 1.0, [ones_bf.b])
        C.memset(eps_t[:], EPS, [eps_t.b])

        es0 = ExitStack()
        with ExitStack() as es_dummy:
            es = es0
            cT = SB(es, nc, "cT", [128, KT, 5], F32)
            scT = SB(es, nc, "scT", [128, KT, 5], F32)
            mbT = SB(es, nc, "mbT", [128, 2, 48], F32)
            wch = [SB(es, nc, "wch%d" % i, [128, KT, 768], F32) for i in range(2)]
            C.dma("sp", cT[:], cT_in, writes=[cT.b])
            C.dma("sp", mbT[:], modbT_in, writes=[mbT.b])
            C.act(scT[:], cT[:], AF.Silu, [cT.b], [scT.b])
            n = 0
            for l in range(2):
                for ch in range(8):
                    w = wch[n % 2]
                    n += 1
                    C.dma("sp", w[:], modw_in[l].rearrange("(k p) n -> p k n", p=128)[:, :, ch * 768:(ch + 1) * 768],
                          writes=[w.b])
                    pt, pb = C.ps("g", [0, 1])
                    for jj in range(6):
                        for k in range(KT):
                            C.mm(pt[:, jj * 8:jj * 8 + 5], w[:, k, jj * 128:(jj + 1) * 128], scT[:, k, :],
                                 k == 0, k == KT - 1, [w.b, scT.b], [pb])
                    for jj in range(6):
                        j = ch * 6 + jj
                        C.ts(modT[:, l, j, :], pt[:, jj * 8:jj * 8 + 5], mbT[:, l, j:j + 1], ALU.add,
                             [pb, mbT.b], [], pw=[modT.b])
            for l in range(2):
                for nrm in range(2):
                    gi = l if nrm == 0 else 2 + l
                    for j5 in range(5):
                        C.stt(A1[:, l, nrm, :, j5], modT[:, l, (3 * nrm + 1) * 8:(3 * nrm + 2) * 8, j5], 1.0,
                              gvec[:, gi, :], ALU.add, ALU.mult, [modT.b, gvec.b], [], pw=[A1.b])

        def modvec(l, which, k, j5):
            return modT[:, l, which * 8 + k, j5:j5 + 1]

        with ExitStack() as es:
            xin = [SB(es, nc, "xin%d" % i, [128, D], F32) for i in range(3)]
            xo = [SB(es, nc, "xo%d" % i, [128, KT, 256], F32) for i in range(2)]
            n = 0
            for b in range(NB):
                for blk in range(T // 256):
                    o = xo[blk % 2]
                    for half in range(2):
                        xi = xin[n % 3]
                        n += 1
                        s0 = blk * 256 + half * 128
                        src = ctx_in[b, s0:s0 + 128, :] if s0 < LC else x_in[b, s0 - LC:s0 - LC + 128, :]
                        C.dma("sp", xi[:], src, writes=[xi.b])
                        for kk in range(2):
                            pt, pb = C.ps("g", [0, 1, 2, 3])
                            for k4 in range(4):
                                k = kk * 4 + k4
                                C.tr(pt[:, k4 * 128:(k4 + 1) * 128], xi[:, k * 128:(k + 1) * 128], ident[:],
                                     [xi.b, ident.b], [pb])
                            dst = o[:, kk * 4:(kk + 1) * 4, half * 128:(half + 1) * 128]
                            srcp = pt[:].rearrange("p (a t) -> p a t", a=4)
                            C.cp(dst, srcp, [pb], [], eng=("dve" if kk == 0 else "act"), pw=[o.b])
                    bi = _blk_index(blk * 256)
                    C.dma("sp", xT_d[b].rearrange("(k p) s -> p k s", p=128)[:, :, blk * 256:(blk + 1) * 256], o[:],
                          reads=[o.b], pwrites=[xT_buf[b][bi]])
        C.barrier()
        es0.close()

        def norm_block(xt, n, sq, rstd, scale, bias, hT):
            C.act(sq[:, :, :n], xt[:, :, :n], AF.Square, [xt.b], [sq.b])
            pt, pb = C.ps("n", [6, 7])
            for k in range(KT):
                C.mm(pt[:, :n], ones_bf[:], sq[:, k, :n], k == 0, k == KT - 1, [ones_bf.b, sq.b], [pb])
            C.act(rstd[:, :n], pt[:, :n], AF.Sqrt, [pb, eps_t.b], [rstd.b], scale=1.0 / D, bias=eps_t[:, 0:1])
            C.recip(rstd[:, :n], rstd[:, :n], [rstd.b], [rstd.b])
            C.tt(xt[:, :, :n], xt[:, :, :n], rstd[:, :n].unsqueeze(1).broadcast_to([128, KT, n]), ALU.mult,
                 [xt.b, rstd.b], [xt.b])
            rb = [xt.b] + scale.bufs + (bias.bufs if bias is not None else [])
            for k in range(KT):
                if bias is None:
                    if k % 2 == 0:
                        C.act(hT[:, k, :n], xt[:, k, :n], AF.Identity, rb, [], scale=scale(k), pw=[hT.b])
                    else:
                        C.ts(hT[:, k, :n], xt[:, k, :n], scale(k), ALU.mult, rb, [], pw=[hT.b])
                else:
                    if k % 2 == 0:
                        C.act(hT[:, k, :n], xt[:, k, :n], AF.Identity, rb, [], scale=scale(k), bias=bias(k), pw=[hT.b])
                    else:
                        C.ts(hT[:, k, :n], xt[:, k, :n], scale(k), ALU.mult, rb, [], s2=bias(k), op1=ALU.add, pw=[hT.b])

        class VecFn:
            def __init__(self, fn, bufs):
                self.fn = fn
                self.bufs = bufs

            def __call__(self, k):
                return self.fn(k)

        ev_w_in = din("ev_w_in", [D, 1536])
        rope_cs = din("rope_cs", [128, 2, T])
        gqk_in = din("gqk", [128, 4])
        uT_d = dscr("uT", [NB, 512, T], BF16)
        qT_d = dscr("qT", [NB, 4, 128, T], BF16)
        kT_d = dscr("kT", [NB, 4, 128, T], BF16)
        vtok_d = dscr("vtok", [NB, T, 256], BF16)
        attnT_d = dscr("attnT", [NB, 4, 128, T], BF16)

        def load_w_bf16(dst_sb, src_ap, nk, ncols, chunk=512):
            v = src_ap.rearrange("(k p) n -> p k n", p=128)
            for c0 in range(0, ncols, chunk):
                c1 = min(ncols, c0 + chunk)
                C.dma("pool", dst_sb[:, :, c0:c1], v[:, :, c0:c1], pwrites=[dst_sb.b])

        def stage_l0_inproj():
            with ExitStack() as es:
                Win = SB(es, nc, "Win", [128, KT, 1536], BF16)
                Wqr = SB(es, nc, "Wqr", [128, KT, 512], BF16)
                Wkd = SB(es, nc, "Wkd", [128, KT, 512], BF16)
                Wkr = SB(es, nc, "Wkr", [128, KT, 512], BF16)
                bd = SB(es, nc, "bd", [128, 128], BF16)
                rcs = SB(es, nc, "rcs", [128, 2, T], F32)
                gqk = SB(es, nc, "gqk", [128, 4], F32)
                tabs = SB(es, nc, "tabs", [128, 4, T], F32)
                xt2 = [SB(es, nc, "ax%d" % i, [128, KT, 512], F32) for i in range(2)]
                sq = SB(es, nc, "asq", [128, KT, 512], BF16)
                rstd = SB(es, nc, "arstd", [128, 512], F32)
                hT = SB(es, nc, "ahT", [128, KT, 512], BF16)
                ub = [SB(es, nc, "aub%d" % i, [128, 4, 512], BF16) for i in range(2)]
                sqh = [SB(es, nc, "asqh%d" % i, [128, 512], BF16) for i in range(2)]
                rsh = [SB(es, nc, "arsh%d" % i, [128, 512], F32) for i in range(2)]
                t1 = [SB(es, nc, "at1%d" % i, [128, 512], F32) for i in range(2)]
                t2 = [SB(es, nc, "at2%d" % i, [128, 512], F32) for i in range(2)]
                qo = [SB(es, nc, "aqo%d" % i, [128, 512], BF16) for i in range(3)]
                vo = [SB(es, nc, "avo%d" % i, [128, 256], BF16) for i in range(3)]
                load_w_bf16(Win, ev_w_in, KT, 1536)
                C.dma("sp", rcs[:], rope_cs, writes=[rcs.b])
                C.dma("sp", gqk[:], gqk_in, writes=[gqk.b])
                C.memset(bd[:], 0.0, [bd.b])
                C.op("dve", lambda e: e.memset(bd[0:64, 0:64], 1.0), [], [], [bd.b])
                C.op("dve", lambda e: e.memset(bd[64:128, 64:128], 1.0), [], [], [bd.b])
                for i, (g, cs) in enumerate([(0, 0), (1, 1), (2, 0), (3, 1)]):
                    C.ts(tabs[:, i, :], rcs[:, cs, :], gqk[:, g:g + 1], ALU.mult, [rcs.b, gqk.b], [], pw=[tabs.b])
                for g in range(4):
                    C.cp(Wkd[:, :, g * 128:(g + 1) * 128].rearrange("p k (u d) -> p k u d", u=2),
                         Win[:, :, 1024 + g * 64:1024 + (g + 1) * 64].unsqueeze(2).broadcast_to([128, KT, 2, 64]),
                         [Win.b], [], pw=[Wkd.b])
                for src, dst in ((Win[:, :, 512:1024], Wqr), (Wkd[:, :, :], Wkr)):
                    sv = src.rearrange("p k (ha hf f) -> p k ha hf f", hf=2, f=16)
                    dv = dst[:, :, :].rearrange("p k (ha hf f) -> p k ha hf f", hf=2, f=16)
                    C.ts(dv[:, :, :, 0, :], sv[:, :, :, 1, :], -1.0, ALU.mult, [Win.b, Wkd.b], [], pw=[dst.b])
                    C.cp(dv[:, :, :, 1, :], sv[:, :, :, 0, :], [Win.b, Wkd.b], [], pw=[dst.b])
                nblk = 0
                nq = 0
                nv = 0
                for b in range(NB):
                    for bi, (s0, n) in enumerate(BLOCKS):
                        xt = xt2[nblk % 2]
                        u = ub[nblk % 2]
                        nblk += 1
                        j5 = 4 if bi == 0 else b
                        C.dma("sp", xt[:, :, :n], xT_view(b, s0, n), writes=[xt.b])
                        norm_block(xt, n, sq, rstd,
                                   VecFn(lambda k, j5=j5: A1[:, 0, 0, k, j5:j5 + 1], [A1.b]),
                                   VecFn(lambda k, j5=j5: modvec(0, 0, k, j5), [modT.b]), hT)
                        for m in range(4):
                            pt, pb = C.ps("g", [0, 1, 2, 3, 4, 5])
                            for k in range(KT):
                                C.mm(pt[:, :n], Win[:, k, m * 128:(m + 1) * 128], hT[:, k, :n], k == 0, k == KT - 1,
                                     [Win.b, hT.b], [pb])
                            C.cp(u[:, m, :n], pt[:, :n], [pb], [], eng="act", pw=[u.b])
                        C.dma("sp", uT_d[b].rearrange("(m p) s -> p m s", p=128)[:, :, s0:s0 + n], u[:, :, :n], reads=[u.b])
                        for which in range(2):
                            for m in range(4):
                                if which == 0:
                                    W0, c0, W1_ = Win, 512 + m * 128, Wqr
                                    dst = qT_d[b, m, :, s0:s0 + n]
                                else:
                                    W0, c0, W1_ = Wkd, m * 128, Wkr
                                    dst = kT_d[b, m, :, s0:s0 + n]
                                pq, pqb = C.ps("g", [0, 1, 2, 3, 4, 5])
                                for k in range(KT):
                                    C.mm(pq[:, :n], W0[:, k, c0:c0 + 128], hT[:, k, :n], k == 0, k == KT - 1, [W0.b, hT.b], [pqb])
                                pr, prb = C.ps("g", [0, 1, 2, 3, 4, 5])
                                for k in range(KT):
                                    C.mm(pr[:, :n], W1_[:, k, m * 128:(m + 1) * 128], hT[:, k, :n], k == 0, k == KT - 1,
                                         [W1_.b, hT.b], [prb])
                                i2 = nq % 2
                                o = qo[nq % 3]
                                nq += 1
                                C.act(sqh[i2][:, :n], pq[:, :n], AF.Square, [pqb], [sqh[i2].b])
                                pss, psb_ = C.ps("g", [0, 1, 2, 3, 4, 5])
                                C.mm(pss[:, :n], bd[:], sqh[i2][:, :n], True, True, [bd.b, sqh[i2].b], [psb_])
                                C.act(rsh[i2][:, :n], pss[:, :n], AF.Sqrt, [psb_, eps_t.b], [rsh[i2].b], scale=1.0 / 64, bias=eps_t[:, 0:1])
                                C.recip(rsh[i2][:, :n], rsh[i2][:, :n], [rsh[i2].b], [rsh[i2].b])
                                C.tt(t1[i2][:, :n], pq[:, :n], tabs[:, 2 * which, s0:s0 + n], ALU.mult, [pqb, tabs.b], [t1[i2].b])
                                C.tt(t2[i2][:, :n], pr[:, :n], tabs[:, 2 * which + 1, s0:s0 + n], ALU.mult, [prb, tabs.b], [t2[i2].b])
                                C.tt(t1[i2][:, :n], t1[i2][:, :n], t2[i2][:, :n], ALU.add, [t1[i2].b, t2[i2].b], [t1[i2].b], eng="pool")
                                C.tt(o[:, :n], t1[i2][:, :n], rsh[i2][:, :n], ALU.mult, [t1[i2].b, rsh[i2].b], [o.b])
                                C.dma("sp", dst, o[:, :n], reads=[o.b])
                        for tt_ in range(n // 128):
                            pv, pvb = C.ps("g", [0, 1, 2, 3, 4, 5])
                            for k in range(KT):
                                C.mm(pv[:, :256], hT[:, k, tt_ * 128:(tt_ + 1) * 128], Win[:, k, 1280:1536], k == 0, k == KT - 1,
                                     [hT.b, Win.b], [pvb])
                            o = vo[nv % 3]
                            nv += 1
                            C.cp(o[:], pv[:, :256], [pvb], [o.b], eng="act")
                            C.dma("sp", vtok_d[b, s0 + tt_ * 128:s0 + (tt_ + 1) * 128, :], o[:], reads=[o.b])

        def gen_l0_attn(es):
            if True:
                k_ = SB(es, nc, "bkT", [128, T], BF16)
                q_ = SB(es, nc, "bqT", [128, T], BF16)
                v_ = SB(es, nc, "bvt", [128, 18, 64], BF16)
                pT = [SB(es, nc, "bpT%d" % i, [128, 512], BF16) for i in range(2)]
                numS = [SB(es, nc, "bnumS%d" % i, [128, 512], F32) for i in range(2)]
                denS = [SB(es, nc, "bdenS%d" % i, [128, 512], F32) for i in range(2)]
                ao = [SB(es, nc, "bao%d" % i, [128, 512], BF16) for i in range(2)]
                npT = 0
                nao = 0
                pend = None

                def finish(ns_, ds_, o, b, g, s0, n):
                    C.recip(ds_[:, :n], ds_[:, :n], [ds_.b], [ds_.b])
                    C.tt(o[:, :n], ns_[:, :n], ds_[:, :n], ALU.mult, [ns_.b, ds_.b], [o.b])
                    C.dma("sp", attnT_d[b, g, :, s0:s0 + n], o[:, :n], reads=[o.b])
                for b in range(NB):
                    for g in range(4):
                        C.dma("sp", k_[:], kT_d[b, g], writes=[k_.b])
                        C.dma("sp", q_[:], qT_d[b, g], writes=[q_.b])
                        C.dma("sp", v_[:], vtok_d[b].rearrange("(t p) c -> p t c", p=128)[:, :, g * 64:(g + 1) * 64], writes=[v_.b])
                        for bi, (s0, n) in enumerate(BLOCKS):
                            kts = range(2) if bi == 0 else range(18)
                            num, numb = C.psb[4]
                            den, denb = C.psb[5]
                            steps = [(kt, hh) for kt in kts for hh in range(2)]

                            def emit_s(kt, hh):
                                p0 = hh * 64
                                pss, pssb = C.ps("sa", [6, 7])
                                C.mm(pss[:, :n], k_[p0:p0 + 64, kt * 128:(kt + 1) * 128], q_[p0:p0 + 64, s0:s0 + n],
                                     True, True, [k_.b, q_.b], [pssb])
                                return pss, pssb
                            cur = emit_s(*steps[0])
                            for si, (kt, hh) in enumerate(steps):
                                p0 = hh * 64
                                nxt = emit_s(*steps[si + 1]) if si + 1 < len(steps) else None
                                pss, pssb = cur
                                p = pT[npT % 2]
                                npT += 1
                                C.act(p[:, :n], pss[:, :n], AF.Exp, [pssb], [p.b], scale=0.125)
                                first, last = kt == kts[0], kt == kts[-1]
                                C.mm(num[p0:p0 + 64, :n], v_[:, kt, :], p[:, :n], first, last, [v_.b, p.b], [numb])
                                C.mm(den[p0:p0 + 64, :n], ones_bf[:, 0:64], p[:, :n], first, last, [ones_bf.b, p.b], [denb])
                                cur = nxt
                                yield "step"
                            ns_, ds_ = numS[nao % 2], denS[nao % 2]
                            C.cp(ns_[:, :n], num[:, :n], [numb], [ns_.b], eng="act")
                            C.cp(ds_[:, :n], den[:, :n], [denb], [ds_.b], eng="act")
                            if pend is not None:
                                finish(*pend)
                            pend = (ns_, ds_, ao[nao % 2], b, g, s0, n)
                            nao += 1
                if pend is not None:
                    finish(*pend)

        s5_sm_in = din("s5_sm", [128, 3, 2, 16])
        s5_bpad_in = din("s5_bpad", [128, 2, 2, 16, 128])
        s5_cpad_in = din("s5_cpad", [128, 2, 2, 16, 128])
        s5_dpad_in = din("s5_dpad", [128, 4, 128])
        s5_wglu_in = din("s5_w_glu", [512, 512])
        s5T_d = dscr("s5T", [NB, 4, 128, T], BF16)
        TL = 256
        NTL = T // TL
        TWO_PI = 6.283185307179586
        MAGIC = 12582912.0

        def gen_l0_s5(es):
            if True:
                sm = SB(es, nc, "s5sm", [128, 3, 32], F32)
                pr_ = SB(es, nc, "s5pr", [128, 13, 32], F32)
                Tc = SB(es, nc, "s5Tc", [128, 32, TL], F32)
                Ts = SB(es, nc, "s5Ts", [128, 32, TL], F32)
                cs_k = SB(es, nc, "s5csk", [128, 4, 32], F32)
                sTpm = SB(es, nc, "s5sTpm", [128, 32, 2], F32)
                Bpad = SB(es, nc, "s5B", [128, 2, 32, 128], BF16)
                Cpad = SB(es, nc, "s5C", [128, 2, 32, 128], BF16)
                Dpad = SB(es, nc, "s5D", [128, 4, 128], BF16)
                Wg = SB(es, nc, "s5Wg", [128, 4, 512], BF16)
                uT2 = [SB(es, nc, "s5uT%d" % i, [128, T], BF16) for i in range(2)]
                ysum = SB(es, nc, "s5ys", [128, T], F32)
                gT = SB(es, nc, "s5gT", [128, 4, T], BF16)
                es2 = ExitStack()
                tmpT = SB(es2, nc, "s5tmpT", [128, 32, TL // 2], F32)
                ldf = SB(es2, nc, "s5ldf", [128, 2, 32, 128], F32)
                cw = tmpT
                dl = SB(es2, nc, "s5dl", [128, 4, 128], F32)
                C.dma("sp", sm[:].rearrange("p a (d j) -> p a d j", d=2), s5_sm_in, writes=[sm.b])
                load_w_bf16(Wg, s5_wglu_in, 4, 512)
                lr, li, ldt = sm[:, 0, :], sm[:, 1, :], sm[:, 2, :]
                R_ = lambda i: pr_[:, i, :]
                DT, MAG, TH, Y, SN, CO, AR, AI, DEN, KR, KI, TMP, TMP2 = range(13)
                P = [pr_.b, sm.b]
                C.act(R_(DT), ldt, AF.Exp, P, [pr_.b])
                C.tt(R_(TH), li, R_(DT), ALU.mult, P, [pr_.b])
                C.tt(R_(TMP), lr, R_(DT), ALU.mult, P, [pr_.b])
                C.act(R_(MAG), R_(TMP), AF.Exp, P, [pr_.b])

                def sin_of(dst, ang_row, shift):
                    C.ts(R_(Y), ang_row, 1.0 / TWO_PI, ALU.mult, P, [pr_.b], s2=shift, op1=ALU.add)
                    C.ts(R_(TMP), R_(Y), MAGIC, ALU.add, P, [pr_.b], s2=MAGIC, op1=ALU.subtract)
                    C.tt(R_(Y), R_(Y), R_(TMP), ALU.subtract, P, [pr_.b])
                    C.act(dst, R_(Y), AF.Sin, P, [pr_.b], scale=TWO_PI * 0.999999)
                sin_of(R_(SN), R_(TH), 0.0)
                sin_of(R_(CO), R_(TH), 0.25)
                C.tt(R_(AR), R_(MAG), R_(CO), ALU.mult, P, [pr_.b])
                C.tt(R_(AI), R_(MAG), R_(SN), ALU.mult, P, [pr_.b])
                C.ts(R_(TMP2), R_(AR), -1.0, ALU.add, P, [pr_.b])
                C.tt(R_(DEN), lr, lr, ALU.mult, P, [pr_.b])
                C.tt(R_(TMP), li, li, ALU.mult, P, [pr_.b])
                C.tt(R_(DEN), R_(DEN), R_(TMP), ALU.add, P, [pr_.b])
                C.recip(R_(DEN), R_(DEN), P, [pr_.b])
                C.tt(R_(KR), R_(TMP2), lr, ALU.mult, P, [pr_.b])
                C.tt(R_(TMP), R_(AI), li, ALU.mult, P, [pr_.b])
                C.tt(R_(KR), R_(KR), R_(TMP), ALU.add, P, [pr_.b])
                C.tt(R_(KR), R_(KR), R_(DEN), ALU.mult, P, [pr_.b])
                C.tt(R_(KI), R_(AI), lr, ALU.mult, P, [pr_.b])
                C.tt(R_(TMP), R_(TMP2), li, ALU.mult, P, [pr_.b])
                C.tt(R_(KI), R_(KI), R_(TMP), ALU.subtract, P, [pr_.b])
                C.tt(R_(KI), R_(KI), R_(DEN), ALU.mult, P, [pr_.b])
                TB = [Tc.b, Ts.b, cs_k.b, tmpT.b, pr_.b]
                C.memset(Tc[:, :, 0:1], 1.0, [Tc.b])
                C.memset(Ts[:, :, 0:1], 0.0, [Ts.b])
                C.cp(cs_k[:, 0, :], R_(CO), TB, [cs_k.b])
                C.cp(cs_k[:, 1, :], R_(SN), TB, [cs_k.b])
                kk = 1
                while kk <= TL:
                    ck, sk = cs_k[:, 0, :], cs_k[:, 1, :]
                    if kk < TL:
                        ckb = ck.unsqueeze(2).broadcast_to([128, 32, kk])
                        skb = sk.unsqueeze(2).broadcast_to([128, 32, kk])
                        C.tt(tmpT[:, :, 0:kk], Ts[:, :, 0:kk], skb, ALU.mult, TB, [tmpT.b])
                        C.tt(Tc[:, :, kk:2 * kk], Tc[:, :, 0:kk], ckb, ALU.mult, TB, [Tc.b])
                        C.tt(Tc[:, :, kk:2 * kk], Tc[:, :, kk:2 * kk], tmpT[:, :, 0:kk], ALU.subtract, TB, [Tc.b])
                        C.tt(tmpT[:, :, 0:kk], Tc[:, :, 0:kk], skb, ALU.mult, TB, [tmpT.b])
                        C.tt(Ts[:, :, kk:2 * kk], Ts[:, :, 0:kk], ckb, ALU.mult, TB, [Ts.b])
                        C.tt(Ts[:, :, kk:2 * kk], Ts[:, :, kk:2 * kk], tmpT[:, :, 0:kk], ALU.add, TB, [Ts.b])
                        C.tt(cs_k[:, 2, :], ck, ck, ALU.mult, TB, [cs_k.b])
                        C.tt(cs_k[:, 3, :], sk, sk, ALU.mult, TB, [cs_k.b])
                        C.tt(cs_k[:, 3, :], cs_k[:, 2, :], cs_k[:, 3, :], ALU.subtract, TB, [cs_k.b])
                        C.tt(cs_k[:, 2, :], ck, sk, ALU.mult, TB, [cs_k.b])
                        C.ts(cs_k[:, 1, :], cs_k[:, 2, :], 2.0, ALU.mult, TB, [cs_k.b])
                        C.cp(cs_k[:, 0, :], cs_k[:, 3, :], TB, [cs_k.b])
                    kk *= 2
                C.ts(sTpm[:, :, 0], cs_k[:, 1, :], -1.0, ALU.mult, [cs_k.b], [], pw=[sTpm.b])
                C.cp(sTpm[:, :, 1], cs_k[:, 1, :], [cs_k.b], [], pw=[sTpm.b])
                C.dma("sp", ldf[:].rearrange("p a (d j) c -> p a d j c", d=2), s5_bpad_in, writes=[ldf.b])
                C.cp(Bpad[:], ldf[:], [ldf.b], [Bpad.b])
                C.dma("sp", ldf[:].rearrange("p a (d j) c -> p a d j c", d=2), s5_cpad_in, writes=[ldf.b])
                for d in range(2):
                    dsl = slice(d * 16, d * 16 + 16)
                    cwv = cw[:].rearrange("p (a j) c -> p a j c", a=2)
                    krb = R_(KR)[:, dsl].unsqueeze(2).broadcast_to([128, 16, 128])
                    kib = R_(KI)[:, dsl].unsqueeze(2).broadcast_to([128, 16, 128])
                    C.tt(cwv[:, 0], ldf[:, 0, dsl, :], krb, ALU.mult, [ldf.b, pr_.b, Cpad.b], [cw.b])
                    C.tt(cwv[:, 1], ldf[:, 1, dsl, :], kib, ALU.mult, [ldf.b, pr_.b, cw.b], [cw.b])
                    C.tt(Cpad[:, 0, dsl, :], cwv[:, 0], cwv[:, 1], ALU.subtract, [cw.b], [], pw=[Cpad.b])
                    C.tt(cwv[:, 0], ldf[:, 0, dsl, :], kib, ALU.mult, [ldf.b, pr_.b, Cpad.b], [cw.b])
                    C.tt(cwv[:, 1], ldf[:, 1, dsl, :], krb, ALU.mult, [ldf.b, pr_.b, cw.b], [cw.b])
                    C.tt(cwv[:, 0], cwv[:, 0], cwv[:, 1], ALU.add, [cw.b], [cw.b])
                    C.ts(Cpad[:, 1, dsl, :], cwv[:, 0], -1.0, ALU.mult, [cw.b], [], pw=[Cpad.b])
                C.dma("sp", dl[:], s5_dpad_in, writes=[dl.b])
                C.cp(Dpad[:], dl[:], [dl.b], [Dpad.b])
                C.barrier()
                es2.close()
                yield "prep"
                ta = [SB(es, nc, "s5ta%d" % i, [128, 2, TL], F32) for i in range(2)]
                tb = [SB(es, nc, "s5tb%d" % i, [128, 2, TL], F32) for i in range(2)]
                cc = [SB(es, nc, "s5cc%d" % i, [128, 2, TL], F32) for i in range(4)]
                hb = [SB(es, nc, "s5hb%d" % i, [128, 2, TL], BF16) for i in range(8)]
                init = [SB(es, nc, "s5in%d" % i, [128, 4], F32) for i in range(4)]
                yv = [SB(es, nc, "s5yv%d" % i, [128, TL], F32) for i in range(2)]
                g1_ = [SB(es, nc, "s5g1%d" % i, [128, TL], F32) for i in range(2)]
                g2_ = [SB(es, nc, "s5g2%d" % i, [128, TL], F32) for i in range(2)]
                sg = [SB(es, nc, "s5sg%d" % i, [128, 512], F32) for i in range(1)] * 2
                so = [SB(es, nc, "s5so%d" % i, [128, 512], BF16) for i in range(1)] * 2


                nt = 0
                nt2 = 0
                nh = 0
                ny = 0
                ns = 0
                ta2 = [SB(es, nc, "s5ta2%d" % i, [128, 2, TL], F32) for i in range(2)]
                tb2 = [SB(es, nc, "s5tb2%d" % i, [128, 2, TL], F32) for i in range(2)]
                nu = 0
                for b in range(NB):
                    for kt in range(4):
                        uT = uT2[nu % 2]
                        nu += 1
                        C.dma("sp", uT[:], uT_d[b, kt * 128:(kt + 1) * 128, :], writes=[uT.b])
                        for d in range(2):
                            order = list(range(NTL)) if d == 0 else [0] + list(range(NTL - 1, 0, -1))
                            units = [(ti, tile_, j) for ti, tile_ in enumerate(order) for j in range(4)]

                            def emitA(ti, tile_, j):
                                nonlocal nt
                                cols = slice(tile_ * TL, tile_ * TL + TL)
                                J = d * 16 + kt * 4 + j
                                ps_, psb_ = C.ps("s", [0, 1])
                                psv = ps_[:].rearrange("p (a t) -> p a t", a=2)
                                C.mm(psv[:, 0, :], Bpad[:, 0, J, :], uT[:, cols], True, True, [Bpad.b, uT.b], [psb_])
                                C.mm(psv[:, 1, :], Bpad[:, 1, J, :], uT[:, cols], True, True, [Bpad.b, uT.b], [psb_])
                                a_, b_ = ta[nt % 2], tb[nt % 2]
                                c_ = cc[nt % 4]
                                nt += 1
                                if d == 0:
                                    tcv, tsv = Tc[:, J, :], Ts[:, J, :]
                                else:
                                    tcv, tsv = Tc[:, J, ::-1], Ts[:, J, ::-1]
                                tcb = tcv.unsqueeze(1).broadcast_to([128, 2, TL])
                                tsb = tsv.unsqueeze(1).broadcast_to([128, 2, TL])
                                C.tt(a_[:], psv, tcb, ALU.mult, [psb_, Tc.b], [a_.b])
                                C.tt(b_[:], psv[:, ::-1, :], tsb, ALU.mult, [psb_, Ts.b], [b_.b])
                                C.tt(c_[:, 0, :], a_[:, 0, :], b_[:, 0, :], ALU.add, [a_.b, b_.b], [], eng=S5E[0], pw=[c_.b])
                                C.tt(c_[:, 1, :], a_[:, 1, :], b_[:, 1, :], ALU.subtract, [a_.b, b_.b], [], eng=S5E[0], pw=[c_.b])
                                return (c_, tcb, tsb, J)

                            def emitB(ti, tile_, j, st):
                                nonlocal nt2, nh
                                c_, tcb, tsb, J = st
                                rb_ = R_(MAG)[:, J:J + 1].broadcast_to([128, TL])
                                iv = init[j]
                                for ri in range(2):
                                    seq = c_[:, ri, :] if d == 0 else c_[:, ri, ::-1]
                                    ini = 0.0 if ti == 0 else iv[:, ri:ri + 1]
                                    C.op("dve", lambda e, seq=seq, ini=ini, rb_=rb_: e.tensor_tensor_scan(
                                        out=seq, data0=rb_, data1=seq, initial=ini, op0=ALU.mult, op1=ALU.add),
                                        [c_.b, pr_.b, iv.b], [c_.b])
                                lc = TL - 1 if d == 0 else 0
                                if ti < len(order) - 1:
                                    cT_, sT_ = cs_k[:, 0, J:J + 1], cs_k[:, 1, J:J + 1]
                                    gre, gim = c_[:, 0, lc:lc + 1], c_[:, 1, lc:lc + 1]
                                    C.tt(iv[:, 2:4], c_[:, ::-1, lc], sTpm[:, J, :], ALU.mult, [c_.b, sTpm.b], [iv.b])
                                    C.stt(iv[:, 0:2], c_[:, :, lc], cT_, iv[:, 2:4], ALU.mult, ALU.add, [c_.b, cs_k.b, iv.b], [iv.b])
                                a2, b2 = ta2[nt2 % 2], tb2[nt2 % 2]
                                nt2 += 1
                                h_ = hb[nh % 8]
                                nh += 1
                                C.tt(a2[:], c_[:], tcb, ALU.mult, [c_.b, Tc.b], [a2.b])
                                C.tt(b2[:], c_[:, ::-1, :], tsb, ALU.mult, [c_.b, Ts.b], [b2.b], eng=S5E[1])
                                C.tt(h_[:, 0, :], a2[:, 0, :], b2[:, 0, :], ALU.subtract, [a2.b, b2.b], [], eng=S5E[2], pw=[h_.b])
                                C.tt(h_[:, 1, :], a2[:, 1, :], b2[:, 1, :], ALU.add, [a2.b, b2.b], [], eng=S5E[2], pw=[h_.b])
                                return (h_, J)

                            def readout(tile_, hbs):
                                nonlocal ny
                                cols = slice(tile_ * TL, tile_ * TL + TL)
                                py, pyb = C.ps("y", [2])
                                nmm = 8 + (1 if d == 0 else 0)
                                i_ = 0
                                for h_, J in hbs:
                                    for ri in range(2):
                                        C.mm(py[:, :TL], Cpad[:, ri, J, :], h_[:, ri, :], i_ == 0, i_ == nmm - 1, [Cpad.b, h_.b], [pyb])
                                        i_ += 1
                                if d == 0:
                                    C.mm(py[:, :TL], Dpad[:, kt, :], uT[:, cols], False, True, [Dpad.b, uT.b], [pyb])
                                    C.cp(ysum[:, cols], py[:, :TL], [pyb], [], eng="act", pw=[ysum.b])
                                else:
                                    y_ = yv[ny % 2]
                                    q1, q2 = g1_[ny % 2], g2_[ny % 2]
                                    ny += 1
                                    C.tt(y_[:], py[:, :TL], ysum[:, cols], ALU.add, [pyb, ysum.b], [y_.b])
                                    C.act(q1[:], y_[:], AF.Square, [y_.b], [q1.b])
                                    C.ts(q1[:], q1[:], 0.044715, ALU.mult, [q1.b], [q1.b], s2=1.0, op1=ALU.add, eng="pool")
                                    C.tt(q1[:], q1[:], y_[:], ALU.mult, [q1.b, y_.b], [q1.b], eng="pool")
                                    C.act(q2[:], q1[:], AF.Sigmoid, [q1.b], [q2.b], scale=1.5957691216057308)
                                    C.tt(gT[:, kt, cols], q2[:], y_[:], ALU.mult, [q2.b, y_.b], [], eng="pool", pw=[gT.b])

                            stA = emitA(*units[0])
                            hbs = []
                            for ui, (ti, tile_, j) in enumerate(units):
                                stN = emitA(*units[ui + 1]) if ui + 1 < len(units) else None
                                hbs.append(emitB(ti, tile_, j, stA))
                                if j == 3:
                                    readout(tile_, hbs)
                                    hbs = []
                                stA = stN
                                yield "unit"
                    for bi, (s0, n) in enumerate(BLOCKS):
                        for m in range(4):
                            pz, pzb = C.ps("z", [3])
                            for k4 in range(4):
                                C.mm(pz[:, :n], Wg[:, k4, m * 128:(m + 1) * 128], gT[:, k4, s0:s0 + n], k4 == 0, k4 == 3, [Wg.b, gT.b], [pzb])
                            s_ = sg[ns % 2]
                            o_ = so[ns % 2]
                            ns += 1
                            C.act(s_[:, :n], pz[:, :n], AF.Sigmoid, [pzb], [s_.b])
                            C.tt(o_[:, :n], s_[:, :n], gT[:, m, s0:s0 + n], ALU.mult, [s_.b, gT.b], [o_.b])
                            C.dma("sp", s5T_d[b, m, :, s0:s0 + n], o_[:, :n], reads=[o_.b])

        def stage_l0_mix():
            es_s, es_a = ExitStack(), ExitStack()
            g5 = gen_l0_s5(es_s)
            assert next(g5) == "prep"
            ga = gen_l0_attn(es_a)
            a_alive, s_alive = True, True
            while a_alive or s_alive:
                if s_alive:
                    try:
                        next(g5)
                    except StopIteration:
                        s_alive = False
                for _ in range(2 if s_alive else 64):
                    if a_alive:
                        try:
                            next(ga)
                        except StopIteration:
                            a_alive = False
            es_a.close()
            es_s.close()

        ev_w_out = din("ev_w_out", [D, D])
        ffn_w1 = din("ffn_w1", [D, 3584])
        ffn_w3 = din("ffn_w3", [D, 3584])
        ffn_w2 = din("ffn_w2", [3584, D])
        h2T_d = dscr("h2T", [NB, D, T], BF16)

        def h2T_view(b, s0, n):
            return h2T_d[b].rearrange("(k p) s -> p k s", p=128)[:, :, s0:s0 + n]

        def stage_out_norm2(l, w_out_ap, mixA_d, mixB_d, blocks, after=None):
            def run():
                with ExitStack() as es:
                    Wo = SB(es, nc, "oWo", [128, KT, D], BF16)
                    xt2 = [SB(es, nc, "ox%d" % i, [128, KT, 512], F32) for i in range(2)]
                    mx2 = [SB(es, nc, "omx%d" % i, [128, KT, 512], BF16) for i in range(2)]
                    sq = SB(es, nc, "osq", [128, KT, 512], BF16)
                    rstd = SB(es, nc, "orstd", [128, 512], F32)
                    h2 = [SB(es, nc, "oh2%d" % i, [128, KT, 512], BF16) for i in range(2)]
                    load_w_bf16(Wo, w_out_ap, KT, D)
                    hook = after(es) if after is not None else None
                    items = [(b, bi) for b in range(NB) for bi in blocks]

                    def load_block(idx):
                        b, bi = items[idx]
                        s0, n = BLOCKS[bi]
                        xt, mx = xt2[idx % 2], mx2[idx % 2]
                        C.dma("sp", xt[:, :, :n], xT_view(b, s0, n), reads=[xT_buf[b][bi]], writes=[xt.b])
                        C.dma("sp", mx[:, 0:4, :n], mixA_d[b].rearrange("m p s -> p m s")[:, :, s0:s0 + n], pwrites=[mx.b])
                        C.dma("sp", mx[:, 4:8, :n], mixB_d[b].rearrange("m p s -> p m s")[:, :, s0:s0 + n], pwrites=[mx.b])
                    load_block(0)
                    for idx, (b, bi) in enumerate(items):
                        if True:
                            s0, n = BLOCKS[bi]
                            j5 = 4 if bi == 0 else b
                            xt, mx, h = xt2[idx % 2], mx2[idx % 2], h2[idx % 2]
                            if idx + 1 < len(items):
                                load_block(idx + 1)
                            for m in range(KT):
                                po, pob = C.ps("g", [0, 1, 2, 3])
                                for k in range(KT):
                                    C.mm(po[:, :n], Wo[:, k, m * 128:(m + 1) * 128], mx[:, k, :n], k == 0, k == KT - 1, [Wo.b, mx.b], [pob])
                                C.stt(xt[:, m, :n], po[:, :n], modvec(l, 2, m, j5), xt[:, m, :n], ALU.mult, ALU.add,
                                      [pob, modT.b, xt.b], [], pw=[xt.b])
                            C.dma("sp", xT_view(b, s0, n), xt[:, :, :n], reads=[xt.b], writes=[xT_buf[b][bi]])
                            norm_block(xt, n, sq, rstd,
                                       VecFn(lambda k, j5=j5: A1[:, l, 1, k, j5:j5 + 1], [A1.b]),
                                       VecFn(lambda k, j5=j5: modvec(l, 3, k, j5), [modT.b]), h)
                            C.dma("sp", h2T_view(b, s0, n), h[:, :, :n], reads=[h.b])
                            if hook is not None:
                                hook(b, bi, s0, n, xt, h)
            return run

        def ffn_multi(l, passes, blocks):
            HT = 14
            NCH = 7
            def run():
                with ExitStack() as es:
                    W1 = SB(es, nc, "fW1", [128, KT, HT * 128], BF16)
                    W3 = SB(es, nc, "fW3", [128, KT, HT * 128], BF16)
                    W2 = SB(es, nc, "fW2", [128, HT, D], BF16)
                    W1b = [Buf("W1c%d" % i) for i in range(NCH)]
                    W3b = [Buf("W3c%d" % i) for i in range(NCH)]
                    W2b = [Buf("W2c%d" % i) for i in range(NCH)]
                    xt2 = [SB(es, nc, "fx%d" % i, [128, KT, 512], F32) for i in range(2)]
                    h2 = [SB(es, nc, "fh%d" % i, [128, KT, 512], BF16) for i in range(2)]
                    G = SB(es, nc, "fG", [128, HT, 512], BF16)
                    sl = [SB(es, nc, "fsl%d" % i, [128, 512], F32) for i in range(2)]
                    use_rw = passes[0][3] is not None
                    rw = [SB(es, nc, "frw%d" % i, [128, 512], F32) for i in range(2)] if use_rw else None
                    tq = [SB(es, nc, "ftq%d" % i, [128, 512], F32) for i in range(2)] if use_rw else None
                    nsl = 0
                    items = [(pi, b, bi) for pi in range(len(passes)) for b in range(NB) for bi in blocks]

                    def load_block(idx):
                        pi, b, bi = items[idx]
                        s0, n = BLOCKS[bi]
                        xt, h = xt2[idx % 2], h2[idx % 2]
                        r_ = rw[idx % 2] if use_rw else None
                        C.dma("sp", h[:, :, :n], h2T_view(b, s0, n), writes=[h.b])
                        C.dma("sp", xt[:, :, :n], xT_view(b, s0, n), reads=[xT_buf[b][bi]], writes=[xt.b])
                        if r_ is not None:
                            C.dma("sp", r_[:, :n], passes[pi][3][b, s0 - LC:s0 - LC + n].partition_broadcast(128), writes=[r_.b])
                        return xt, h, r_

                    def load_weights(pi):
                        w1_ap, w3_ap, w2_ap, _ = passes[pi]
                        v1 = w1_ap.rearrange("(k p) n -> p k n", p=128)
                        v3 = w3_ap.rearrange("(k p) n -> p k n", p=128)
                        v2 = w2_ap.rearrange("(k p) n -> p k n", p=128)
                        for c in range(NCH):
                            cs_ = slice(c * 256, (c + 1) * 256)
                            C.dma("pool", W1[:, :, cs_], v1[:, :, cs_], writes=[W1b[c]])
                            C.dma("pool", W3[:, :, cs_], v3[:, :, cs_], writes=[W3b[c]])
                        for c in range(NCH):
                            C.dma("pool", W2[:, 2 * c:2 * c + 2, :], v2[:, 2 * c:2 * c + 2, :], writes=[W2b[c]])

                    cur = load_block(0)
                    for idx, (pi, b, bi) in enumerate(items):
                        if idx == 0 or items[idx - 1][0] != pi:
                            load_weights(pi)
                        s0, n = BLOCKS[bi]
                        j5 = 4 if bi == 0 else b
                        xt, h, r_ = cur
                        nxt = load_block(idx + 1) if idx + 1 < len(items) else None
                        for mt in range(HT):
                            pa, pab = C.ps("fg", [0, 1, 2, 3, 4, 5])
                            for k in range(KT):
                                C.mm(pa[:, :n], W1[:, k, mt * 128:(mt + 1) * 128], h[:, k, :n], k == 0, k == KT - 1, [W1b[mt // 2], h.b], [pab])
                            pb_, pbb = C.ps("fg", [0, 1, 2, 3, 4, 5])
                            for k in range(KT):
                                C.mm(pb_[:, :n], W3[:, k, mt * 128:(mt + 1) * 128], h[:, k, :n], k == 0, k == KT - 1, [W3b[mt // 2], h.b], [pbb])
                            s_ = sl[nsl % 2]
                            nsl += 1
                            C.act(s_[:, :n], pa[:, :n], AF.Silu, [pab], [s_.b])
                            C.tt(G[:, mt, :n], s_[:, :n], pb_[:, :n], ALU.mult, [s_.b, pbb], [], pw=[G.b])
                        for m in range(KT):
                            po, pob = C.ps("fo", [6, 7])
                            for mt in range(HT):
                                C.mm(po[:, :n], W2[:, mt, m * 128:(m + 1) * 128], G[:, mt, :n], mt == 0, mt == HT - 1, [W2b[mt // 2], G.b], [pob])
                            if r_ is None:
                                C.stt(xt[:, m, :n], po[:, :n], modvec(l, 5, m, j5), xt[:, m, :n], ALU.mult, ALU.add,
                                      [pob, modT.b, xt.b], [], pw=[xt.b])
                            else:
                                t_ = tq[m % 2]
                                C.tt(t_[:, :n], po[:, :n], r_[:, :n], ALU.mult, [pob, r_.b], [t_.b])
                                C.stt(xt[:, m, :n], t_[:, :n], modvec(l, 5, m, j5), xt[:, m, :n], ALU.mult, ALU.add,
                                      [t_.b, modT.b, xt.b], [], pw=[xt.b])
                        C.dma("sp", xT_view(b, s0, n), xt[:, :, :n], reads=[xt.b], writes=[xT_buf[b][bi]])
                        cur = nxt
            return run

        ALLB = list(range(len(BLOCKS)))
        LATB = list(range(1, len(BLOCKS)))

        od_w_in = din("od_w_in", [D, 3072])
        hy_cw_in = din("hy_cw", [128, 12, 4])
        hv_d = dscr("hv", [NB, L, 512], BF16)
        hg1_d = dscr("hg1", [NB, L, 512], BF16)
        hg2T_d = dscr("hg2T", [NB, 512, L], BF16)
        naq_d = dscr("naq", [NB, 4, 128, L], BF16)
        nak_d = dscr("nak", [NB, 4, 128, T], BF16)
        nav_d = dscr("nav", [NB, T, 512], BF16)

        def stage_l1_inproj():
            with ExitStack() as es:
                Wod = SB(es, nc, "Wod", [128, KT, 3072], BF16)
                cw = SB(es, nc, "hcw", [128, 12, 4], F32)
                hT = SB(es, nc, "l1hT", [128, KT, T], BF16)
                xt2 = [SB(es, nc, "l1x%d" % i, [128, KT, 512], F32) for i in range(2)]
                sq = SB(es, nc, "l1sq", [128, KT, 512], BF16)
                rstd = SB(es, nc, "l1rstd", [128, 512], F32)
                hblk = SB(es, nc, "l1hb", [128, KT, 512], BF16)
                pbuf = [SB(es, nc, "l1pb%d" % i, [128, L + 2], F32) for i in range(2)]
                ucv = [SB(es, nc, "l1uc%d" % i, [128, L], F32) for i in range(2)]
                tok = [SB(es, nc, "l1tk%d" % i, [128, 16, 128], BF16) for i in range(2)]
                g2o = [SB(es, nc, "l1g2%d" % i, [128, L], BF16) for i in range(2)]
                qo = [SB(es, nc, "l1qo%d" % i, [128, 512], BF16) for i in range(3)]
                vo = [SB(es, nc, "l1vo%d" % i, [128, 512], BF16) for i in range(3)]
                load_w_bf16(Wod, od_w_in, KT, 3072)
                C.dma("sp", cw[:], hy_cw_in, writes=[cw.b])
                for pb_ in pbuf:
                    C.memset(pb_[:, 0:1], 0.0, [pb_.b])
                    C.op("dve", lambda e, pb_=pb_: e.memset(pb_[:, L + 1:L + 2], 0.0), [], [], [pb_.b])
                nx = 0
                nm = 0
                nq = 0
                nv = 0
                for b in range(NB):
                    for bi, (s0, n) in enumerate(BLOCKS):
                        xt = xt2[nx % 2]
                        nx += 1
                        j5 = 4 if bi == 0 else b
                        C.dma("sp", xt[:, :, :n], xT_view(b, s0, n), reads=[xT_buf[b][bi]], writes=[xt.b])
                        norm_block(xt, n, sq, rstd,
                                   VecFn(lambda k, j5=j5: A1[:, 1, 0, k, j5:j5 + 1], [A1.b]),
                                   VecFn(lambda k, j5=j5: modvec(1, 0, k, j5), [modT.b]), hblk)
                        C.cp(hT[:, :, s0:s0 + n], hblk[:, :, :n], [hblk.b], [], eng="pool", pw=[hT.b])
                    for m in range(12):
                        pbf, uc = pbuf[nm % 2], ucv[nm % 2]
                        nm += 1
                        for i in range(4):
                            pt, pb = C.ps("g", [0, 1, 2, 3])
                            for k in range(KT):
                                C.mm(pt[:, :512], Wod[:, k, m * 128:(m + 1) * 128], hT[:, k, LC + i * 512:LC + (i + 1) * 512],
                                     k == 0, k == KT - 1, [Wod.b, hT.b], [pb])
                            C.cp(pbf[:, 1 + i * 512:1 + (i + 1) * 512], pt[:, :512], [pb], [], eng="act", pw=[pbf.b])
                        C.ts(uc[:], pbf[:, 1:L + 1], cw[:, m, 1:2], ALU.mult, [pbf.b, cw.b], [uc.b], s2=cw[:, m, 3:4], op1=ALU.add)
                        C.stt(uc[:], pbf[:, 0:L], cw[:, m, 0:1], uc[:], ALU.mult, ALU.add, [pbf.b, cw.b, uc.b], [uc.b])
                        C.stt(uc[:], pbf[:, 2:L + 2], cw[:, m, 2:3], uc[:], ALU.mult, ALU.add, [pbf.b, cw.b, uc.b], [uc.b])
                        if m < 8:
                            tk = tok[nm % 2]
                            for t4 in range(4):
                                pt, pb = C.ps("t", [4, 5])
                                for i in range(4):
                                    tt_ = t4 * 4 + i
                                    C.tr(pt[:, i * 128:(i + 1) * 128], uc[:, tt_ * 128:(tt_ + 1) * 128], ident[:], [uc.b, ident.b], [pb])
                                C.cp(tk[:, t4 * 4:(t4 + 1) * 4, :], pt[:].rearrange("p (a c) -> p a c", a=4), [pb], [],
                                     eng=("act" if t4 % 2 else "dve"), pw=[tk.b])
                            dst = (hv_d if m < 4 else hg1_d)[b].rearrange("(tt p) c -> p tt c", p=128)[:, :, (m % 4) * 128:(m % 4 + 1) * 128]
                            C.dma("sp", dst, tk[:], reads=[tk.b])
                        else:
                            go = g2o[nm % 2]
                            C.cp(go[:], uc[:], [uc.b], [go.b], eng="pool")
                            C.dma("sp", hg2T_d[b, (m - 8) * 128:(m - 7) * 128, :], go[:], reads=[go.b])
                    for bi, (s0, n) in enumerate(BLOCKS):
                        for which in range(2):
                            if which == 0 and bi == 0:
                                continue
                            for m in range(4):
                                c0 = 1536 + which * 512 + m * 128
                                pt, pb = C.ps("g", [0, 1, 2, 3])
                                for k in range(KT):
                                    C.mm(pt[:, :n], Wod[:, k, c0:c0 + 128], hT[:, k, s0:s0 + n], k == 0, k == KT - 1, [Wod.b, hT.b], [pb])
                                o = qo[nq % 3]
                                nq += 1
                                if which == 0:
                                    C.act(o[:, :n], pt[:, :n], AF.Copy, [pb], [o.b], scale=0.125)
                                    C.dma("sp", naq_d[b, m, :, s0 - LC:s0 - LC + n], o[:, :n], reads=[o.b])
                                else:
                                    C.cp(o[:, :n], pt[:, :n], [pb], [o.b])
                                    C.dma("sp", nak_d[b, m, :, s0:s0 + n], o[:, :n], reads=[o.b])
                        for tt_ in range(n // 128):
                            pt, pb = C.ps("g", [0, 1, 2, 3])
                            for k in range(KT):
                                C.mm(pt[:, :512], hT[:, k, s0 + tt_ * 128:s0 + (tt_ + 1) * 128], Wod[:, k, 2560:3072], k == 0, k == KT - 1,
                                     [hT.b, Wod.b], [pb])
                            o = vo[nv % 3]
                            nv += 1
                            C.cp(o[:], pt[:, :512], [pb], [o.b], eng="act")
                            C.dma("sp", nav_d[b, s0 + tt_ * 128:s0 + (tt_ + 1) * 128, :], o[:], reads=[o.b])

        dftC_in = din("dftC", [128, 16, 2048])
        dftS_in = din("dftS", [128, 16, 2048])
        hyz_in = din("hyz", [33, L])
        hynt_in = din("hynt", [128, 16])
        hy_w1_in = din("hy_w1", [33, 64])
        hy_w2_in = din("hy_w2", [64, 64])
        hy_w3_in = din("hy_w3", [64, 2048])
        hy_fb_in = din("hy_fb", [64, 3])
        hy_dec_in = din("hy_dec", [128, 2048])
        hy_d_in = din("hy_dbc", [128, 2, 512])
        alt_in = din("alt", [128, 513])
        hn_d = dscr("hn", [4, L, 512], BF16)
        spec_d = dscr("spec", [2, 2, L, 512], F32)
        hyoT_d = dscr("hyoT", [NB, 4, 128, T], BF16)

        def stage_l1_hyena():
            with ExitStack() as es:
                nyq = SB(es, nc, "hynq", [1, 2, 512], F32)
                altf = SB(es, nc, "hyaltf", [128, 513], F32)
                altc = SB(es, nc, "hyaltc", [128, 1], BF16)
                altr = SB(es, nc, "hyaltr", [1, 512], BF16)
                ones_f = SB(es, nc, "hyones", [128, 128], F32)
                C.dma("sp", altf[:], alt_in, writes=[altf.b])
                C.cp(altc[:], altf[:, 0:1], [altf.b], [altc.b])
                C.cp(altr[:], altf[0:1, 1:513], [altf.b], [altr.b])
                C.memset(ones_f[:], 1.0, [ones_f.b])
                with ExitStack() as e1:
                    zT = SB(e1, nc, "hyzT", [33, L], F32)
                    w1 = SB(e1, nc, "hyw1", [33, 64], F32)
                    w2 = SB(e1, nc, "hyw2", [64, 64], F32)
                    w3 = SB(e1, nc, "hyw3", [64, 2048], F32)
                    fb = SB(e1, nc, "hyfb", [64, 3], F32)
                    nt_ = SB(e1, nc, "hynt", [128, 16], F32)
                    dec = SB(e1, nc, "hydec", [128, 2048], F32)
                    h1T = SB(e1, nc, "hyh1", [64, L], F32)
                    h2T = SB(e1, nc, "hyh2", [64, L], F32)
                    ya = SB(e1, nc, "hyya", [64, 512], F32)
                    yb = SB(e1, nc, "hyyb", [64, 512], F32)
                    hraw = SB(e1, nc, "hyraw", [128, 16, 512], F32)
                    wd = [SB(e1, nc, "hywd%d" % i, [128, 512], F32) for i in range(2)]
                    ab = [SB(e1, nc, "hyab%d" % i, [128, 512], F32) for i in range(2)]
                    rn = SB(e1, nc, "hyrn", [128, 512], F32)
                    hnb = SB(e1, nc, "hyhnb", [128, 16, 512], BF16)
                    C.dma("sp", zT[:], hyz_in, writes=[zT.b])
                    C.dma("sp", w1[:], hy_w1_in, writes=[w1.b])
                    C.dma("sp", w2[:], hy_w2_in, writes=[w2.b])
                    C.dma("sp", w3[:], hy_w3_in, writes=[w3.b])
                    C.dma("sp", fb[:], hy_fb_in, writes=[fb.b])
                    C.dma("sp", nt_[:], hynt_in, writes=[nt_.b])
                    C.dma("sp", dec[:], hy_dec_in, writes=[dec.b])
                    C.stt(dec[:], dec[:], -1.0, dec[:], ALU.mult, ALU.max, [dec.b], [dec.b])

                    def sin_layer(dst, W, kdim, src, bcol):
                        for i in range(4):
                            pt, pb = C.ps("g", [0, 1, 2, 3])
                            C.mm(pt[0:64, :512], W[0:kdim, :], src[0:kdim, i * 512:(i + 1) * 512], True, True, [W.b, src.b], [pb])
                            C.ts(ya[:], pt[0:64, :512], fb[:, bcol:bcol + 1], ALU.add, [pb, fb.b], [ya.b], s2=fb[:, 0:1], op1=ALU.mult)
                            C.ts(ya[:], ya[:], 1.0 / TWO_PI, ALU.mult, [ya.b], [ya.b])
                            C.ts(yb[:], ya[:], MAGIC, ALU.add, [ya.b], [yb.b], s2=MAGIC, op1=ALU.subtract)
                            C.tt(ya[:], ya[:], yb[:], ALU.subtract, [ya.b, yb.b], [ya.b])
                            C.act(dst[:, i * 512:(i + 1) * 512], ya[:], AF.Sin, [ya.b], [], scale=TWO_PI * 0.999999, pw=[dst.b])
                    sin_layer(h1T, w1, 33, zT, 1)
                    sin_layer(h2T, w2, 64, h1T, 2)
                    nw = 0
                    for cb in range(4):
                        pn, pnb = C.ps("n", [6, 7])
                        for kt in range(16):
                            pt, pb = C.ps("g", [0, 1, 2, 3])
                            C.mm(pt[:, :512], h2T[:, kt * 128:(kt + 1) * 128], w3[:, cb * 512:(cb + 1) * 512], True, True, [h2T.b, w3.b], [pb])
                            w_, a_ = wd[nw % 2], ab[nw % 2]
                            nw += 1
                            C.act(w_[:], dec[:, cb * 512:(cb + 1) * 512], AF.Exp, [dec.b, nt_.b], [w_.b], scale=nt_[:, kt:kt + 1])
                            C.tt(hraw[:, kt, :], pt[:, :512], w_[:], ALU.mult, [pb, w_.b], [], pw=[hraw.b])
                            C.stt(a_[:], hraw[:, kt, :], -1.0, hraw[:, kt, :], ALU.mult, ALU.max, [hraw.b], [a_.b])
                            C.mm(pn[:, :512], ones_f[:], a_[:], kt == 0, kt == 15, [ones_f.b, a_.b], [pnb])
                        C.ts(rn[:], pn[:, :512], EPS, ALU.add, [pnb], [rn.b])
                        C.recip(rn[:], rn[:], [rn.b], [rn.b])
                        C.tt(hnb[:], hraw[:], rn[:].unsqueeze(1).broadcast_to([128, 16, 512]), ALU.mult, [hraw.b, rn.b], [hnb.b])
                        C.dma("sp", hn_d[cb].rearrange("(kt p) c -> p kt c", p=128), hnb[:], reads=[hnb.b])
                    C.barrier()
                Cm = SB(es, nc, "hyC", [128, 16, 2048], BF16)
                Sm = SB(es, nc, "hyS", [128, 16, 2048], BF16)
                for c0 in range(0, 2048, 512):
                    C.dma("pool", Cm[:, :, c0:c0 + 512], dftC_in[:, :, c0:c0 + 512], pwrites=[Cm.b])
                    C.dma("pool", Sm[:, :, c0:c0 + 512], dftS_in[:, :, c0:c0 + 512], pwrites=[Sm.b])
                with ExitStack() as e2:
                    dbc = SB(e2, nc, "hydb", [128, 2, 512], F32)
                    C.dma("sp", dbc[:], hy_d_in, writes=[dbc.b])
                    hf = SB(e2, nc, "hyhf", [128, 16, 512], BF16)
                    hb_ = SB(e2, nc, "hyhb", [128, 16, 512], BF16)
                    hs = SB(e2, nc, "hyhs", [128, 16, 512], BF16)
                    so = [SB(e2, nc, "hyso%d" % i, [128, 512], F32) for i in range(3)]
                    nso = 0
                    for o in range(2):
                        C.dma("sp", hf[:], hn_d[o].rearrange("(kt p) c -> p kt c", p=128), writes=[hf.b])
                        C.dma("sp", hb_[:], hn_d[2 + o].rearrange("(kt p) c -> p kt c", p=128), writes=[hb_.b])
                        C.tt(hs[:], hf[:], hb_[:], ALU.add, [hf.b, hb_.b], [hs.b])
                        C.tt(hf[:], hf[:], hb_[:], ALU.subtract, [hf.b, hb_.b], [hf.b])
                        for mt in range(16):
                            for ri in range(2):
                                M_, src = (Cm, hs) if ri == 0 else (Sm, hf)
                                pt, pb = C.ps("g", [0, 1, 2, 3])
                                for kt in range(16):
                                    C.mm(pt[:, :512], M_[:, kt, mt * 128:(mt + 1) * 128], src[:, kt, :], kt == 0, kt == 15, [M_.b, src.b], [pb])
                                s_ = so[nso % 3]
                                nso += 1
                                if ri == 0:
                                    C.tt(s_[:], pt[:, :512], dbc[:, o, :], ALU.add, [pb, dbc.b], [s_.b])
                                else:
                                    C.ts(s_[:], pt[:, :512], -1.0, ALU.mult, [pb], [s_.b])
                                C.dma("sp", spec_d[o, ri, mt * 128:(mt + 1) * 128, :], s_[:], reads=[s_.b])
                        pt, pb = C.ps("g", [0, 1, 2, 3])
                        for kt in range(16):
                            C.mm(pt[0:1, :512], altc[:, 0:1], hs[:, kt, :], kt == 0, kt == 15, [altc.b, hs.b], [pb])
                        C.tt(nyq[0:1, o, :], pt[0:1, :512], dbc[0:1, o, :], ALU.add, [pb, dbc.b], [], pw=[nyq.b])
                    C.barrier()
                z = SB(es, nc, "hyz", [128, 16, 512], BF16)
                Y = SB(es, nc, "hyY", [128, 16, 2, 512], BF16)
                sp_ = [SB(es, nc, "hysp%d" % i, [128, 2, 512], F32) for i in range(2)]
                tq = [SB(es, nc, "hytq%d" % i, [128, 512], F32) for i in range(2)]
                tq = tq + tq
                ynq = SB(es, nc, "hyynq", [1, 512], BF16)
                g1t = [SB(es, nc, "hyg1%d" % i, [128, 512], BF16) for i in range(2)]
                oo = [SB(es, nc, "hyoo%d" % i, [128, 512], BF16) for i in range(2)]
                nsp = 0
                ng = 0
                SC = 2.0 / 4096.0
                for b in range(NB):
                    C.dma("sp", z[:], hv_d[b].rearrange("(tt p) c -> p tt c", p=128), writes=[z.b])
                    for o in range(2):
                        for mt in range(16):
                            sp = sp_[nsp % 2]
                            nsp += 1
                            C.dma("sp", sp[:], spec_d[o, :, mt * 128:(mt + 1) * 128, :].rearrange("r p c -> p r c"), writes=[sp.b])
                            pc, pcb = C.ps("g", [0, 1, 2, 3])
                            for tt_ in range(16):
                                C.mm(pc[:, :512], Cm[:, tt_, mt * 128:(mt + 1) * 128], z[:, tt_, :], tt_ == 0, tt_ == 15, [Cm.b, z.b], [pcb])
                            pz, pzb = C.ps("g", [0, 1, 2, 3])
                            for tt_ in range(16):
                                C.mm(pz[:, :512], Sm[:, tt_, mt * 128:(mt + 1) * 128], z[:, tt_, :], tt_ == 0, tt_ == 15, [Sm.b, z.b], [pzb])
                            C.tt(tq[0][:], pc[:, :512], sp[:, 0, :], ALU.mult, [pcb, sp.b], [tq[0].b])
                            C.tt(tq[1][:], pz[:, :512], sp[:, 1, :], ALU.mult, [pzb, sp.b], [tq[1].b])
                            C.tt(Y[:, mt, 0, :], tq[0][:], tq[1][:], ALU.add, [tq[0].b, tq[1].b], [], eng="pool", pw=[Y.b])
                            C.tt(tq[2][:], pz[:, :512], sp[:, 0, :], ALU.mult, [pzb, sp.b], [tq[2].b])
                            C.tt(tq[3][:], pc[:, :512], sp[:, 1, :], ALU.mult, [pcb, sp.b], [tq[3].b])
                            C.tt(Y[:, mt, 1, :], tq[2][:], tq[3][:], ALU.subtract, [tq[2].b, tq[3].b], [], eng="pool", pw=[Y.b])
                            if mt == 0:
                                C.ts(Y[0:1, 0, 0, :], Y[0:1, 0, 0, :], 0.5, ALU.mult, [Y.b], [], eng="pool", pw=[Y.b])
                        pq, pqb = C.ps("g", [0, 1, 2, 3])
                        for tt_ in range(16):
                            C.mm(pq[0:1, :512], altc[:, 0:1], z[:, tt_, :], tt_ == 0, tt_ == 15, [altc.b, z.b], [pqb])
                        C.stt(ynq[:], pq[0:1, :512], 0.5, nyq[0:1, o, :], ALU.mult, ALU.mult, [pqb, nyq.b], [ynq.b])
                        if o == 0:
                            for tt_ in range(16):
                                g_ = g1t[ng % 2]
                                ng += 1
                                C.dma("sp", g_[:], hg1_d[b, tt_ * 128:(tt_ + 1) * 128, :], writes=[g_.b])
                                pt, pb = C.ps("o", [4, 5])
                                for mt in range(16):
                                    C.mm(pt[:, :512], Cm[:, mt, tt_ * 128:(tt_ + 1) * 128], Y[:, mt, 0, :], mt == 0, False, [Cm.b, Y.b], [pb])
                                    C.mm(pt[:, :512], Sm[:, mt, tt_ * 128:(tt_ + 1) * 128], Y[:, mt, 1, :], False, False, [Sm.b, Y.b], [pb])
                                C.mm(pt[:, :512], altr[0:1, 0:128], ynq[0:1, :], False, True, [altr.b, ynq.b], [pb])
                                C.stt(z[:, tt_, :], pt[:, :512], SC, g_[:], ALU.mult, ALU.mult, [pb, g_.b], [], pw=[z.b])
                        else:
                            for cm in range(4):
                                for tb in range(4):
                                    g_ = g1t[ng % 2]
                                    o_ = oo[ng % 2]
                                    ng += 1
                                    C.dma("sp", g_[:], hg2T_d[b, cm * 128:(cm + 1) * 128, tb * 512:(tb + 1) * 512], writes=[g_.b])
                                    pt, pb = C.ps("o", [4, 5])
                                    for mt in range(16):
                                        C.mm(pt[:, :512], Y[:, mt, 0, cm * 128:(cm + 1) * 128], Cm[:, mt, tb * 512:(tb + 1) * 512], mt == 0, False, [Cm.b, Y.b], [pb])
                                        C.mm(pt[:, :512], Y[:, mt, 1, cm * 128:(cm + 1) * 128], Sm[:, mt, tb * 512:(tb + 1) * 512], False, False, [Sm.b, Y.b], [pb])
                                    C.mm(pt[:, :512], ynq[0:1, cm * 128:(cm + 1) * 128], altr[0:1, :], False, True, [altr.b, ynq.b], [pb])
                                    C.stt(o_[:], pt[:, :512], SC, g_[:], ALU.mult, ALU.mult, [pb, g_.b], [o_.b])
                                    C.dma("sp", hyoT_d[b, cm, :, LC + tb * 512:LC + (tb + 1) * 512], o_[:], reads=[o_.b])

        nab_in = din("nab", [128, 8, 9, 128])
        naoT_d = dscr("naoT", [NB, 4, 128, T], BF16)

        def stage_l1_na():
            def r0(r):
                return min(max(r - 4, 0), 24)
            with ExitStack() as es:
                nab = SB(es, nc, "nab", [128, 8, 9, 128], BF16)
                idb = SB(es, nc, "naidb", [128, 128], BF16)
                kT = SB(es, nc, "nakT", [128, 4, T], BF16)
                qT = SB(es, nc, "naqT", [128, 4, L], BF16)
                V = SB(es, nc, "naV", [128, 18, 512], BF16)
                oT = [SB(es, nc, "naoT%d" % i, [128, L], BF16) for i in range(2)]
                PA = [SB(es, nc, "naPA%d" % i, [128, 4, 128], BF16) for i in range(3)]
                PB = [SB(es, nc, "naPB%d" % i, [128, 4, 128], BF16) for i in range(3)]
                rec = [SB(es, nc, "narec%d" % i, [128, 128], F32) for i in range(2)]
                C.dma("pool", nab[:], nab_in, writes=[nab.b])
                C.cp(idb[:], ident[:], [ident.b], [idb.b])
                nstep = 0
                no = 0
                nq = 0
                for b in range(NB):
                    C.dma("sp", kT[:], nak_d[b].rearrange("m p s -> p m s"), writes=[kT.b])
                    C.dma("sp", qT[:], naq_d[b].rearrange("m p s -> p m s"), writes=[qT.b])
                    C.dma("sp", V[:], nav_d[b].rearrange("(t p) c -> p t c", p=128), writes=[V.b])
                    for m in range(4):
                        o_ = oT[no % 2]
                        no += 1
                        steps = []
                        for qt in range(16):
                            for hh in range(2):
                                tiles = [(0, None), (1, None)]
                                for kp in range(16):
                                    val = [[r0(2 * qt + bq) <= 2 * kp + a < r0(2 * qt + bq) + 8 for bq in range(2)] for a in range(2)]
                                    if any(val[0]) or any(val[1]):
                                        di = (2 * kp - 2 * qt + 6) // 2
                                        if not (all(val[0]) and all(val[1])):
                                            di = 7 if di == 1 else 8
                                        tiles.append((2 + kp, di))
                                steps.append((qt, hh, tiles))

                        def emit_s(qt, hh, tiles):
                            h = 2 * m + hh
                            p0 = hh * 64
                            qc = slice(qt * 128, (qt + 1) * 128)
                            banks = [C.ps("s", [2, 3, 4, 5]), C.ps("s", [2, 3, 4, 5])]
                            for ti_, (tk, di) in enumerate(tiles):
                                pt_, ptb_ = banks[ti_ // 4]
                                dst = pt_[:, (ti_ % 4) * 128:(ti_ % 4 + 1) * 128]
                                C.mm(dst, kT[p0:p0 + 64, m, tk * 128:(tk + 1) * 128], qT[p0:p0 + 64, m, qc], True, di is None,
                                     [kT.b, qT.b], [ptb_])
                                if di is not None:
                                    C.mm(dst, idb[:], nab[:, h, di, :], False, True, [idb.b, nab.b], [ptb_])
                            return banks
                        cur = emit_s(*steps[0])
                        for si, (qt, hh, tiles) in enumerate(steps):
                            h = 2 * m + hh
                            p0 = hh * 64
                            qc = slice(qt * 128, (qt + 1) * 128)
                            nxt = emit_s(*steps[si + 1]) if si + 1 < len(steps) else None
                            num, numb = C.psb[0 if qt % 2 == 0 else 6]
                            den, denb = C.psb[1 if qt % 2 == 0 else 7]
                            pa, pb_ = PA[nstep % 3], PB[nstep % 3]
                            nstep += 1
                            nA = min(4, len(tiles))
                            nB = len(tiles) - nA
                            C.act(pa[:, :nA, :], cur[0][0][:, :nA * 128].rearrange("p (a c) -> p a c", c=128), AF.Exp, [cur[0][1]], [pa.b])
                            if nB > 0:
                                C.act(pb_[:, :nB, :], cur[1][0][:, :nB * 128].rearrange("p (a c) -> p a c", c=128), AF.Exp, [cur[1][1]], [pb_.b])
                            for ti_, (tk, di) in enumerate(tiles):
                                P_ = pa if ti_ < 4 else pb_
                                pv = P_[:, ti_ % 4, :]
                                first, last = ti_ == 0, ti_ == len(tiles) - 1
                                C.mm(num[p0:p0 + 64, 0:128], V[:, tk, h * 64:(h + 1) * 64], pv, first, last, [V.b, P_.b], [numb])
                                C.mm(den[p0:p0 + 64, 0:128], ones_bf[:, 0:64], pv, first, last, [ones_bf.b, P_.b], [denb])
                            if hh == 1:
                                r_ = rec[nq % 2]
                                nq += 1
                                C.recip(r_[:], den[:, 0:128], [denb], [r_.b])
                                C.tt(o_[:, qc], num[:, 0:128], r_[:], ALU.mult, [numb, r_.b], [], pw=[o_.b])
                            cur = nxt
                        C.dma("sp", naoT_d[b, m, :, LC:T], o_[:], reads=[o_.b])

        od_w_out = din("od_w_out", [D, D])
        moe_router_in = din("moe_router", [D, 8])
        moe_w1 = din("moe_w1", [8, D, 3584])
        moe_w3 = din("moe_w3", [8, D, 3584])
        moe_w2 = din("moe_w2", [8, 3584, D])
        rw_d = dscr("rw", [8, NB, L], F32)
        AX = mybir.AxisListType

        def router_hook(es):
            Wr = SB(es, nc, "rWr", [128, KT, 8], F32)
            h2f = SB(es, nc, "rh2f", [128, KT, 512], F32)
            lgs = SB(es, nc, "rlgs", [8, 512], F32)
            lt = SB(es, nc, "rlt", [128, 4, 8], F32)
            eq = SB(es, nc, "req", [128, 4, 8], F32)
            msk = SB(es, nc, "rmsk", [128, 4, 8], F32)
            ex = SB(es, nc, "rex", [128, 4, 8], F32)
            m1 = SB(es, nc, "rm1", [128, 4], F32)
            m2 = SB(es, nc, "rm2", [128, 4], F32)
            dn = SB(es, nc, "rdn", [128, 4], F32)
            wts = SB(es, nc, "rwts", [8, 512], F32)
            C.dma("sp", Wr[:], moe_router_in.rearrange("(k p) e -> p k e", p=128), writes=[Wr.b])

            def hook(b, bi, s0, n, xt, h):
                j5 = b
                nch = n // 128
                for k in range(KT):
                    if k % 2 == 0:
                        C.act(h2f[:, k, :n], xt[:, k, :n], AF.Identity, [xt.b, A1.b, modT.b], [], scale=A1[:, 1, 1, k, j5:j5 + 1],
                              bias=modvec(1, 3, k, j5), pw=[h2f.b])
                    else:
                        C.ts(h2f[:, k, :n], xt[:, k, :n], A1[:, 1, 1, k, j5:j5 + 1], ALU.mult, [xt.b, A1.b, modT.b], [],
                             s2=modvec(1, 3, k, j5), op1=ALU.add, pw=[h2f.b])
                pl, plb = C.ps("g", [0, 1, 2, 3])
                for k in range(KT):
                    C.mm(pl[0:8, :n], Wr[:, k, :], h2f[:, k, :n], k == 0, k == KT - 1, [Wr.b, h2f.b], [plb])
                C.cp(lgs[:, :n], pl[0:8, :n], [plb], [lgs.b])
                pt, ptb = C.ps("g", [0, 1, 2, 3])
                for ch in range(nch):
                    C.tr(pt[:, ch * 8:(ch + 1) * 8], lgs[0:8, ch * 128:(ch + 1) * 128], ident[0:8, 0:8], [lgs.b, ident.b], [ptb])
                ltv, eqv, mkv, exv = lt[:, :nch, :], eq[:, :nch, :], msk[:, :nch, :], ex[:, :nch, :]
                C.cp(ltv, pt[:, :nch * 8].rearrange("p (c e) -> p c e", e=8), [ptb], [lt.b])
                bc = lambda v: v[:, :nch].unsqueeze(2).broadcast_to([128, nch, 8])
                C.op("dve", lambda e: e.tensor_reduce(out=m1[:, :nch], in_=ltv, axis=AX.X, op=ALU.max), [lt.b], [m1.b])
                C.tt(eqv, ltv, bc(m1), ALU.is_equal, [lt.b, m1.b], [eq.b])
                C.stt(mkv, eqv, -1e30, ltv, ALU.mult, ALU.add, [eq.b, lt.b], [msk.b])
                C.op("dve", lambda e: e.tensor_reduce(out=m2[:, :nch], in_=mkv, axis=AX.X, op=ALU.max), [msk.b], [m2.b])
                C.tt(eqv, ltv, bc(m2), ALU.is_ge, [lt.b, m2.b], [eq.b])
                C.tt(mkv, ltv, bc(m1), ALU.subtract, [lt.b, m1.b], [msk.b])
                C.act(exv, mkv, AF.Exp, [msk.b], [ex.b])
                C.tt(exv, exv, eqv, ALU.mult, [ex.b, eq.b], [ex.b])
                C.op("dve", lambda e: e.tensor_reduce(out=dn[:, :nch], in_=exv, axis=AX.X, op=ALU.add), [ex.b], [dn.b])
                C.recip(dn[:, :nch], dn[:, :nch], [dn.b], [dn.b])
                C.tt(exv, exv, bc(dn), ALU.mult, [ex.b, dn.b], [ex.b])
                pw_, pwb = C.ps("g", [0, 1, 2, 3])
                for ch in range(nch):
                    C.tr(pw_[0:8, ch * 128:(ch + 1) * 128], ex[:, ch, :], ident[:], [ex.b, ident.b], [pwb])
                C.cp(wts[:, :n], pw_[0:8, :n], [pwb], [wts.b])
                C.dma("sp", rw_d[:, b, s0 - LC:s0 - LC + n], wts[:, :n], reads=[wts.b])
            return hook

        def stage_final():
            with ExitStack() as es:
                xt2 = [SB(es, nc, "fx%d" % i, [128, KT, 512], F32) for i in range(2)]
                sq = SB(es, nc, "fsq", [128, KT, 512], BF16)
                rstd = SB(es, nc, "frstd", [128, 512], F32)
                hf = [SB(es, nc, "fh%d" % i, [128, KT, 512], F32) for i in range(2)]
                ot = [SB(es, nc, "fo%d" % i, [128, D], F32) for i in range(3)]
                n = 0
                no = 0
                fscale = VecFn(lambda k: gvec[:, 4, k:k + 1], [gvec.b])
                for b in range(NB):
                    for bi in range(1, len(BLOCKS)):
                        s0, nn = BLOCKS[bi]
                        xt = xt2[n % 2]
                        h = hf[n % 2]
                        n += 1
                        C.dma("sp", xt[:, :, :nn], xT_d[b].rearrange("(k p) s -> p k s", p=128)[:, :, s0:s0 + nn],
                              reads=[xT_buf[b][bi]], writes=[xt.b])
                        norm_block(xt, nn, sq, rstd, fscale, None, h)
                        for tt_ in range(nn // 128):
                            o = ot[no % 3]
                            no += 1
                            for kk in range(2):
                                pt, pb = C.ps("g", [0, 1, 2, 3])
                                for k4 in range(4):
                                    k = kk * 4 + k4
                                    C.tr(pt[:, k4 * 128:(k4 + 1) * 128], h[:, k, tt_ * 128:(tt_ + 1) * 128], ident[:],
                                         [h.b, ident.b], [pb])
                                dst = o[:, kk * 512:(kk + 1) * 512]
                                C.cp(dst, pt[:], [pb], [], eng=("dve" if kk == 0 else "act"), pw=[o.b])
                            t0 = s0 - LC + tt_ * 128
                            C.dma("sp", out_d[b, t0:t0 + 128, :], o[:], reads=[o.b])

        def xT_view(b, s0, n):
            return xT_d[b].rearrange("(k p) s -> p k s", p=128)[:, :, s0:s0 + n]

        stages = [("l0_inproj", stage_l0_inproj), ("l0_s5", stage_l0_mix),
                  ("l0_out", stage_out_norm2(0, ev_w_out, s5T_d, attnT_d, ALLB)),
                  ("l0_ffn1", ffn_multi(0, [(ffn_w1[:, 0:1792], ffn_w3[:, 0:1792], ffn_w2[0:1792, :], None),
                                            (ffn_w1[:, 1792:3584], ffn_w3[:, 1792:3584], ffn_w2[1792:3584, :], None)], ALLB)),
                  ("l1_inproj", stage_l1_inproj),
                  ("l1_hyena", stage_l1_hyena),
                  ("l1_na", stage_l1_na),
                  ("l1_out", stage_out_norm2(1, od_w_out, hyoT_d, naoT_d, LATB, after=router_hook)),
                  ]
        moe_passes = []
        for e_ in range(8):
            for hf_ in range(2):
                hs_ = slice(hf_ * 1792, (hf_ + 1) * 1792)
                moe_passes.append((moe_w1[e_][:, hs_], moe_w3[e_][:, hs_], moe_w2[e_][hs_, :], rw_d[e_]))
        stages.append(("moe", ffn_multi(1, moe_passes, LATB)))
        for name, fn in stages:
            fn()
            C.barrier()
            if stop_after == name:
                break
        if stop_after is None:
            stage_final()
        C.finish()
    return nc, C


def _blk_index(s):
    for i, (s0, n) in enumerate(BLOCKS):
        if s0 <= s < s0 + n:
            return i
    raise ValueError(s)


def host_inputs(inp, core):
    b0 = core * NB
    f = lambda a: np.ascontiguousarray(a, dtype=np.float32)
    m = {}
    m["x"] = f(inp["x"][b0:b0 + NB])
    m["ctx"] = f(inp["ctx"][b0:b0 + NB])
    c5 = np.concatenate([inp["c"][b0:b0 + NB], inp["c_ctx"][None, :]], 0)
    m["cT"] = f(c5.reshape(5, KT, 128).transpose(2, 1, 0))
    m["mod_w"] = f(inp["mod_w"])
    m["mod_bT"] = f(inp["mod_b"].reshape(2, 48, 128).transpose(2, 0, 1))
    gv = np.stack([inp["norm1_g"][0], inp["norm1_g"][1], inp["norm2_g"][0], inp["norm2_g"][1], inp["final_g"]], 0)
    m["gvecT"] = f(gv.reshape(5, KT, 128).transpose(2, 0, 1))
    m["ident"] = np.eye(128, dtype=np.float32)
    m["ev_w_in"] = f(inp["ev_w_in"][0])
    m["rope_cs"] = _rope_tables()
    gq, gk = inp["gqa_q_g"][0], inp["gqa_k_g"][0]
    dd = np.arange(64)
    partner = np.where((dd // 16) % 2 == 0, dd + 16, dd - 16)
    G, N, P = 32, 64, 16
    sm = np.zeros((128, 3, 2, 16), np.float32)
    bpad = np.zeros((128, 2, 2, 16, 128), np.float32)
    cpad = np.zeros((128, 2, 2, 16, 128), np.float32)
    lam_re, lam_im, log_dt = inp["s5_lam_re"][0], inp["s5_lam_im"][0], inp["s5_log_dt"][0]
    b_ri = [inp["s5_b_re"][0], inp["s5_b_im"][0]]
    c_ri = [inp["s5_c_re"][0], inp["s5_c_im"][0]]
    for j in range(16):
        for gl in range(2):
            g = 2 * j + gl
            sp = slice(gl * 64, gl * 64 + 64)
            cc = slice((g % 8) * 16, (g % 8) * 16 + 16)
            for d in range(2):
                sm[sp, 0, d, j] = lam_re[d, g]
                sm[sp, 1, d, j] = lam_im[d, g]
                sm[sp, 2, d, j] = log_dt[d, g]
                for ri in range(2):
                    bpad[cc, ri, d, j, sp] = b_ri[ri][d, g].T
                    cpad[sp, ri, d, j, cc] = c_ri[ri][d, g].T
    m["s5_sm"], m["s5_bpad"], m["s5_cpad"] = sm, bpad, cpad
    dpad = np.zeros((128, 4, 128), np.float32)
    dd_ = inp["s5_d"][0].reshape(4, 128)
    for kt in range(4):
        dpad[np.arange(128), kt, np.arange(128)] = dd_[kt]
    m["s5_dpad"] = dpad
    m["s5_w_glu"] = f(inp["s5_w_glu"][0])
    m["ev_w_out"] = f(inp["ev_w_out"][0])
    m["ffn_w1"], m["ffn_w3"], m["ffn_w2"] = f(inp["ffn_w1"][0]), f(inp["ffn_w3"][0]), f(inp["ffn_w2"][0])
    m["od_w_in"] = f(inp["od_w_in"][0])
    cw = np.concatenate([inp["hy_conv_w"][0], inp["hy_conv_b"][0][None, :]], 0)
    m["hy_cw"] = f(cw.reshape(4, 12, 128).transpose(2, 1, 0))
    m["dftC"], m["dftS"] = _dft_tables()
    m["hyz"], m["hynt"] = _hyena_pos()
    m["hy_w1"], m["hy_w2"], m["hy_w3"] = f(inp["hy_w1"][0]), f(inp["hy_w2"][0]), f(inp["hy_w3"][0])
    m["hy_fb"] = f(np.stack([inp["hy_freq"][0], inp["hy_b1"][0], inp["hy_b2"][0]], 1))
    m["hy_dec"] = f(np.broadcast_to(inp["hy_decay"][0][None, :], (128, 2048)))
    m["hy_dbc"] = f(np.broadcast_to(inp["hy_d"][0][None], (128, 2, 512)))
    alt = np.zeros((128, 513), np.float32)
    alt[:, 0] = (-1.0) ** np.arange(128)
    alt[:, 1:] = ((-1.0) ** np.arange(512))[None, :]
    m["alt"] = alt
    m["nab"] = _na_bias(inp["na_rpb"][0])
    m["od_w_out"] = f(inp["od_w_out"][0])
    m["moe_router"] = f(inp["moe_router"][0])
    m["moe_w1"], m["moe_w3"], m["moe_w2"] = f(inp["moe_w1"][0]), f(inp["moe_w3"][0]), f(inp["moe_w2"][0])
    m["gqk"] = f(np.stack([np.tile(gq, 2), np.tile(gq[partner], 2), np.tile(gk, 2), np.tile(gk[partner], 2)], 1))
    return m


_TAB = {}


def _dft_tables():
    if "dft" not in _TAB:
        k = np.arange(2048, dtype=np.int64)
        km = (k[:, None] * k[None, :]) % 4096
        ang = 2.0 * np.pi * km.astype(np.float64) / 4096.0
        c = np.cos(ang).astype(np.float32).reshape(16, 128, 2048).transpose(1, 0, 2)
        s_ = np.sin(ang).astype(np.float32).reshape(16, 128, 2048).transpose(1, 0, 2)
        _TAB["dft"] = (np.ascontiguousarray(c), np.ascontiguousarray(s_))
    return _TAB["dft"]


def _hyena_pos():
    k = np.arange(L, dtype=np.float32)
    t = (k / np.float32(L - 1)).astype(np.float32)
    bands = np.linspace(1e-4, 15, 16, dtype=np.float32)
    ang = (np.float32(2.0 * np.pi / L) * k[:, None] * bands[None, :]).astype(np.float32)
    z = np.concatenate([t[:, None], np.cos(ang), -np.sin(ang)], -1).astype(np.float32)
    nt = (-t).reshape(16, 128).T
    return np.ascontiguousarray(z.T), np.ascontiguousarray(nt, dtype=np.float32)


def _na_bias(rpb):
    NEG = np.float32(-30000.0)
    out = np.full((128, 8, 9, 128), NEG, np.float32)
    ck = np.arange(64)[:, None]
    cq = np.arange(64)[None, :]
    cs = np.clip(cq - 8, 0, 48)
    inwin = (ck >= cs) & (ck < cs + 16)
    dc = np.clip(ck - cq + 15, 0, 30)
    for di in range(7):
        delta = 2 * di - 6
        for a in range(2):
            for bq in range(2):
                dr = delta + a - bq + 7
                if dr < 0 or dr > 14:
                    continue
                for h in range(8):
                    blk = np.where(inwin, rpb[h, dr][dc], NEG)
                    out[a * 64:(a + 1) * 64, h, di, bq * 64:(bq + 1) * 64] = blk
    out[:, :, 7, :] = out[:, :, 1, :]
    out[0:64, :, 7, 64:128] = NEG
    out[0:64, :, 8, 64:128] = out[0:64, :, 5, 64:128]
    return out


def _rope_tables():
    s = np.arange(T)
    t = np.maximum(s - LC, 0)
    d = np.arange(64)
    a = d // 32
    fq = d % 16
    inv = (10000.0 ** (-(np.arange(16, dtype=np.float32)) / np.float32(16))).astype(np.float32)
    pos = np.where(a[:, None] == 0, (t // 64)[None, :], (t % 64)[None, :]).astype(np.float32)
    ang = (pos * inv[fq][:, None]).astype(np.float32)
    ang = np.where(s[None, :] < LC, np.float32(0), ang)
    cs = np.stack([np.cos(ang.astype(np.float64)), np.sin(ang.astype(np.float64))], 1)
    return np.ascontiguousarray(np.tile(cs, (2, 1, 1)), dtype=np.float32)


_CACHE = {}


def kernel(**inputs):
    inp = {k: np.asarray(v) for k, v in inputs.items()}
    if "nc" not in _CACHE:
        _CACHE["nc"] = build_program()[0]
    nc = _CACHE["nc"]
    in_maps = [host_inputs(inp, c) for c in range(NCORES)]
    res = run_bass_kernel_spmd(nc, in_maps, core_ids=list(range(NCORES)))
    out = np.concatenate([np.asarray(r["out"]) for r in res.results], axis=0)
    return out.astype(np.float32)
```

```python
import numpy as np
import ml_dtypes
from contextlib import ExitStack
import concourse.bass as bass
import concourse.mybir as mybir
from concourse.bass_utils import run_bass_kernel_spmd

F32 = mybir.dt.float32
BF16 = mybir.dt.bfloat16
AF = mybir.ActivationFunctionType
ALU = mybir.AluOpType

NCORES = 8
NB = 4
L = 2048
LC = 256
T = L + LC
D = 1024
KT = 8
EPS = 1e-6
NDMA = 48
import os as _os
S5E = _os.environ.get("S5E", "pool,dve,pool").split(",")


class Buf:
    __slots__ = ("w", "r", "name")

    def __init__(self, name=""):
        self.w = {}
        self.r = {}
        self.name = name


class Ctx:
    def __init__(self, nc):
        self.nc = nc
        self.eng = {"pe": nc.tensor, "act": nc.scalar, "dve": nc.vector, "pool": nc.gpsimd, "sp": nc.sync}
        self.csem = {e: nc.alloc_semaphore("c_" + e) for e in ("pe", "act", "dve", "pool")}
        self.tick = {e: 0 for e in ("pe", "act", "dve", "pool")}
        self.seen = {e: {} for e in self.eng}
        self.dsem = [nc.alloc_semaphore("d%d" % i) for i in range(NDMA)]
        self.dcum = [0] * NDMA
        self.dnext = 0
        self.nwait = 0
        self.ninst = 0
        self.psb = []
        for i in range(8):
            t = nc.alloc_psum_tensor("psb%d" % i, [128, 512], F32)
            self.psb.append((t, Buf("ps%d" % i)))
        self.psn = {}

    def _sem(self, k):
        return self.csem[k[1]] if k[0] == "c" else self.dsem[k[1]]

    def _wait(self, e, toks):
        need = {}
        for k, v in toks:
            if self.seen[e].get(k, 0) >= v:
                continue
            if need.get(k, 0) < v:
                need[k] = v
        for k, v in need.items():
            self.eng[e].wait_ge(self._sem(k), v)
            self.seen[e][k] = v
            self.nwait += 1

    def _deps(self, e, reads, writes, pwrites, is_dma):
        toks = []
        me = ("c", e)
        for b in reads:
            for k, v in b.w.items():
                if k == me and e == "pe":
                    continue
                toks.append((k, v))
        for b in writes:
            for k, v in list(b.w.items()) + list(b.r.items()):
                if k == me and not is_dma:
                    continue
                toks.append((k, v))
        for b in pwrites:
            for k, v in b.r.items():
                if k == me and not is_dma:
                    continue
                toks.append((k, v))
        return toks

    def _book(self, tok, reads, writes, pwrites):
        k, v = tok
        for b in reads:
            b.r[k] = v
        for b in writes:
            b.w = {k: v}
            b.r = {}
        for b in pwrites:
            b.w[k] = v

    def op(self, e, fn, reads=(), writes=(), pwrites=()):
        self._wait(e, self._deps(e, reads, writes, pwrites, False))
        inst = fn(self.eng[e])
        self.tick[e] += 1
        inst.then_inc(self.csem[e], 1)
        self.ninst += 1
        self._book((("c", e), self.tick[e]), reads, writes, pwrites)
        return inst

    def dma(self, q, out, in_, reads=(), writes=(), pwrites=(), **kw):
        self._wait(q, self._deps(q, reads, writes, pwrites, True))
        slot = self.dnext
        self.dnext = (self.dnext + 1) % NDMA
        if self.dcum[slot] > 0:
            self._wait(q, [(("d", slot), self.dcum[slot])])
        inst = self.eng[q].dma_start(out=out, in_=in_, **kw)
        self.dcum[slot] += 16
        inst.then_inc(self.dsem[slot], 16)
        self.ninst += 1
        self._book((("d", slot), self.dcum[slot]), reads, writes, pwrites)

    def barrier(self):
        toks = [(("c", e), self.tick[e]) for e in self.tick if self.tick[e] > 0]
        toks += [(("d", i), self.dcum[i]) for i in range(NDMA) if self.dcum[i] > 0]
        for e in self.eng:
            self._wait(e, toks)

    def finish(self):
        toks = [(("d", i), self.dcum[i]) for i in range(NDMA) if self.dcum[i] > 0]
        toks += [(("c", e), self.tick[e]) for e in self.tick if self.tick[e] > 0]
        self._wait("sp", toks)

    def ps(self, group, banks):
        i = self.psn.get(group, 0)
        self.psn[group] = i + 1
        return self.psb[banks[i % len(banks)]]

    def mm(self, out, lhsT, rhs, start, stop, reads, writes):
        return self.op("pe", lambda e: e.matmul(out, lhsT=lhsT, rhs=rhs, start=start, stop=stop), reads, writes)

    def tr(self, out, in_, ident, reads, writes):
        return self.op("pe", lambda e: e.transpose(out, in_, ident), reads, writes)

    def act(self, out, in_, func, reads, writes, scale=1.0, bias=None, pw=()):
        if bias is None:
            return self.op("act", lambda e: e.activation(out=out, in_=in_, func=func, scale=scale), reads, writes, pw)
        return self.op("act", lambda e: e.activation(out=out, in_=in_, func=func, scale=scale, bias=bias), reads, writes, pw)

    def tt(self, out, in0, in1, op, reads, writes, eng="dve", pw=()):
        return self.op(eng, lambda e: e.tensor_tensor(out=out, in0=in0, in1=in1, op=op), reads, writes, pw)

    def ts(self, out, in0, s1, op0, reads, writes, s2=None, op1=None, eng="dve", pw=()):
        if op1 is None:
            return self.op(eng, lambda e: e.tensor_scalar(out=out, in0=in0, scalar1=s1, scalar2=None, op0=op0), reads, writes, pw)
        return self.op(eng, lambda e: e.tensor_scalar(out=out, in0=in0, scalar1=s1, scalar2=s2, op0=op0, op1=op1), reads, writes, pw)

    def stt(self, out, in0, scalar, in1, op0, op1, reads, writes, pw=()):
        return self.op("dve", lambda e: e.scalar_tensor_tensor(out=out, in0=in0, scalar=scalar, in1=in1, op0=op0, op1=op1), reads, writes, pw)

    def cp(self, out, in_, reads, writes, eng="dve", pw=()):
        if eng == "act":
            return self.op("act", lambda e: e.copy(out=out, in_=in_), reads, writes, pw)
        return self.op(eng, lambda e: e.tensor_copy(out=out, in_=in_), reads, writes, pw)

    def recip(self, out, in_, reads, writes, pw=()):
        return self.op("dve", lambda e: e.reciprocal(out=out, in_=in_), reads, writes, pw)

    def memset(self, out, val, writes, eng="dve"):
        return self.op(eng, lambda e: e.memset(out, val), (), writes)


class SB:
    _n = [0]

    def __init__(self, es, nc, name, shape, dtype):
        SB._n[0] += 1
        self.t = es.enter_context(nc.sbuf_tensor("sb%d_%s" % (SB._n[0], name), list(shape), dtype))
        self.b = Buf(name)

    def __getitem__(self, idx):
        return self.t[idx]


BLOCKS = [(0, 256)] + [(256 + 512 * i, 512) for i in range(4)]


def build_program(stop_after=None, dbg=()):
    nc = bass.Bass("TRN2", target_bir_lowering=False)
    C = Ctx(nc)
    dbg = set(dbg)

    def din(name, shape, dt=F32):
        return nc.dram_tensor(name, list(shape), dt, kind="ExternalInput").ap()

    def dscr(name, shape, dt=F32):
        kind = "ExternalOutput" if name in dbg else "Internal"
        return nc.dram_tensor(name, list(shape), dt, kind=kind).ap()

    x_in = din("x", [NB, L, D])
    ctx_in = din("ctx", [NB, LC, D])
    cT_in = din("cT", [128, KT, 5])
    modw_in = din("mod_w", [2, D, 6 * D])
    modbT_in = din("mod_bT", [128, 2, 48])
    gvec_in = din("gvecT", [128, 5, KT])
    ident_in = din("ident", [128, 128])
    out_d = nc.dram_tensor("out", [NB, L, D], F32, kind="ExternalOutput").ap()

    xT_d = dscr("xT", [NB, D, T])
    xT_buf = [[Buf("xT%d_%d" % (b, i)) for i in range(len(BLOCKS))] for b in range(NB)]

    with ExitStack() as gs:
        ident = SB(gs, nc, "ident", [128, 128], F32)
        ones_bf = SB(gs, nc, "ones_bf", [128, 128], BF16)
        eps_t = SB(gs, nc, "eps_t", [128, 1], F32)
        modT = SB(gs, nc, "modT", [128, 2, 48, 5], F32)
        gvec = SB(gs, nc, "gvec", [128, 5, KT], F32)
        A1 = SB(gs, nc, "A1", [128, 2, 2, KT, 5], F32)
        C.dma("sp", ident[:], ident_in, writes=[ident.b])
        C.dma("sp", gvec[:], gvec_in, writes=[gvec.b])
        C.memset(ones_bf[:], 1.0, [ones_bf.b])
        C.memset(eps_t[:], EPS, [eps_t.b])

        es0 = ExitStack()
        with ExitStack() as es_dummy:
            es = es0
            cT = SB(es, nc, "cT", [128, KT, 5], F32)
            scT = SB(es, nc, "scT", [128, KT, 5], F32)
            mbT = SB(es, nc, "mbT", [128, 2, 48], F32)
            wch = [SB(es, nc, "wch%d" % i, [128, KT, 768], F32) for i in range(2)]
            C.dma("sp", cT[:], cT_in, writes=[cT.b])
            C.dma("sp", mbT[:], modbT_in, writes=[mbT.b])
            C.act(scT[:], cT[:], AF.Silu, [cT.b], [scT.b])
            n = 0
            for l in range(2):
                for ch in range(8):
                    w = wch[n % 2]
                    n += 1
                    C.dma("sp", w[:], modw_in[l].rearrange("(k p) n -> p k n", p=128)[:, :, ch * 768:(ch + 1) * 768],
                          writes=[w.b])
                    pt, pb = C.ps("g", [0, 1])
                    for jj in range(6):
                        for k in range(KT):
                            C.mm(pt[:, jj * 8:jj * 8 + 5], w[:, k, jj * 128:(jj + 1) * 128], scT[:, k, :],
                                 k == 0, k == KT - 1, [w.b, scT.b], [pb])
                    for jj in range(6):
                        j = ch * 6 + jj
                        C.ts(modT[:, l, j, :], pt[:, jj * 8:jj * 8 + 5], mbT[:, l, j:j + 1], ALU.add,
                             [pb, mbT.b], [], pw=[modT.b])
            for l in range(2):
                for nrm in range(2):
                    gi = l if nrm == 0 else 2 + l
                    for j5 in range(5):
                        C.stt(A1[:, l, nrm, :, j5], modT[:, l, (3 * nrm + 1) * 8:(3 * nrm + 2) * 8, j5], 1.0,
                              gvec[:, gi, :], ALU.add, ALU.mult, [modT.b, gvec.b], [], pw=[A1.b])

        def modvec(l, which, k, j5):
            return modT[:, l, which * 8 + k, j5:j5 + 1]

        with ExitStack() as es:
            xin = [SB(es, nc, "xin%d" % i, [128, D], F32) for i in range(3)]
            xo = [SB(es, nc, "xo%d" % i, [128, KT, 256], F32) for i in range(2)]
            n = 0
            for b in range(NB):
                for blk in range(T // 256):
                    o = xo[blk % 2]
                    for half in range(2):
                        xi = xin[n % 3]
                        n += 1
                        s0 = blk * 256 + half * 128
                        src = ctx_in[b, s0:s0 + 128, :] if s0 < LC else x_in[b, s0 - LC:s0 - LC + 128, :]
                        C.dma("sp", xi[:], src, writes=[xi.b])
                        for kk in range(2):
                            pt, pb = C.ps("g", [0, 1, 2, 3])
                            for k4 in range(4):
                                k = kk * 4 + k4
                                C.tr(pt[:, k4 * 128:(k4 + 1) * 128], xi[:, k * 128:(k + 1) * 128], ident[:],
                                     [xi.b, ident.b], [pb])
                            dst = o[:, kk * 4:(kk + 1) * 4, half * 128:(half + 1) * 128]
                            srcp = pt[:].rearrange("p (a t) -> p a t", a=4)
                            C.cp(dst, srcp, [pb], [], eng=("dve" if kk == 0 else "act"), pw=[o.b])
                    bi = _blk_index(blk * 256)
                    C.dma("sp", xT_d[b].rearrange("(k p) s -> p k s", p=128)[:, :, blk * 256:(blk + 1) * 256], o[:],
                          reads=[o.b], pwrites=[xT_buf[b][bi]])
        C.barrier()
        es0.close()

        def norm_block(xt, n, sq, rstd, scale, bias, hT):
            C.act(sq[:, :, :n], xt[:, :, :n], AF.Square, [xt.b], [sq.b])
            pt, pb = C.ps("n", [6, 7])
            for k in range(KT):
                C.mm(pt[:, :n], ones_bf[:], sq[:, k, :n], k == 0, k == KT - 1, [ones_bf.b, sq.b], [pb])
            C.act(rstd[:, :n], pt[:, :n], AF.Sqrt, [pb, eps_t.b], [rstd.b], scale=1.0 / D, bias=eps_t[:, 0:1])
            C.recip(rstd[:, :n], rstd[:, :n], [rstd.b], [rstd.b])
            C.tt(xt[:, :, :n], xt[:, :, :n], rstd[:, :n].unsqueeze(1).broadcast_to([128, KT, n]), ALU.mult,
                 [xt.b, rstd.b], [xt.b])
            rb = [xt.b] + scale.bufs + (bias.bufs if bias is not None else [])
            for k in range(KT):
                if bias is None:
                    if k % 2 == 0:
                        C.act(hT[:, k, :n], xt[:, k, :n], AF.Identity, rb, [], scale=scale(k), pw=[hT.b])
                    else:
                        C.ts(hT[:, k, :n], xt[:, k, :n], scale(k), ALU.mult, rb, [], pw=[hT.b])
                else:
                    if k % 2 == 0:
                        C.act(hT[:, k, :n], xt[:, k, :n], AF.Identity, rb, [], scale=scale(k), bias=bias(k), pw=[hT.b])
                    else:
                        C.ts(hT[:, k, :n], xt[:, k, :n], scale(k), ALU.mult, rb, [], s2=bias(k), op1=ALU.add, pw=[hT.b])

        class VecFn:
            def __init__(self, fn, bufs):
                self.fn = fn
                self.bufs = bufs

            def __call__(self, k):
                return self.fn(k)

        ev_w_in = din("ev_w_in", [D, 1536])
        rope_cs = din("rope_cs", [128, 2, T])
        gqk_in = din("gqk", [128, 4])
        uT_d = dscr("uT", [NB, 512, T], BF16)
        qT_d = dscr("qT", [NB, 4, 128, T], BF16)
        kT_d = dscr("kT", [NB, 4, 128, T], BF16)
        vtok_d = dscr("vtok", [NB, T, 256], BF16)
        attnT_d = dscr("attnT", [NB, 4, 128, T], BF16)

        def load_w_bf16(dst_sb, src_ap, nk, ncols, chunk=512):
            v = src_ap.rearrange("(k p) n -> p k n", p=128)
            for c0 in range(0, ncols, chunk):
                c1 = min(ncols, c0 + chunk)
                C.dma("pool", dst_sb[:, :, c0:c1], v[:, :, c0:c1], pwrites=[dst_sb.b])

        def stage_l0_inproj():
            with ExitStack() as es:
                Win = SB(es, nc, "Win", [128, KT, 1536], BF16)
                Wqr = SB(es, nc, "Wqr", [128, KT, 512], BF16)
                Wkd = SB(es, nc, "Wkd", [128, KT, 512], BF16)
                Wkr = SB(es, nc, "Wkr", [128, KT, 512], BF16)
                bd = SB(es, nc, "bd", [128, 128], BF16)
                rcs = SB(es, nc, "rcs", [128, 2, T], F32)
                gqk = SB(es, nc, "gqk", [128, 4], F32)
                tabs = SB(es, nc, "tabs", [128, 4, T], F32)
                xt2 = [SB(es, nc, "ax%d" % i, [128, KT, 512], F32) for i in range(2)]
                sq = SB(es, nc, "asq", [128, KT, 512], BF16)
                rstd = SB(es, nc, "arstd", [128, 512], F32)
                hT = SB(es, nc, "ahT", [128, KT, 512], BF16)
                ub = [SB(es, nc, "aub%d" % i, [128, 4, 512], BF16) for i in range(2)]
                sqh = [SB(es, nc, "asqh%d" % i, [128, 512], BF16) for i in range(2)]
                rsh = [SB(es, nc, "arsh%d" % i, [128, 512], F32) for i in range(2)]
                t1 = [SB(es, nc, "at1%d" % i, [128, 512], F32) for i in range(2)]
                t2 = [SB(es, nc, "at2%d" % i, [128, 512], F32) for i in range(2)]
                qo = [SB(es, nc, "aqo%d" % i, [128, 512], BF16) for i in range(3)]
                vo = [SB(es, nc, "avo%d" % i, [128, 256], BF16) for i in range(3)]
                load_w_bf16(Win, ev_w_in, KT, 1536)
                C.dma("sp", rcs[:], rope_cs, writes=[rcs.b])
                C.dma("sp", gqk[:], gqk_in, writes=[gqk.b])
                C.memset(bd[:], 0.0, [bd.b])
                C.op("dve", lambda e: e.memset(bd[0:64, 0:64], 1.0), [], [], [bd.b])
                C.op("dve", lambda e: e.memset(bd[64:128, 64:128], 1.0), [], [], [bd.b])
                for i, (g, cs) in enumerate([(0, 0), (1, 1), (2, 0), (3, 1)]):
                    C.ts(tabs[:, i, :], rcs[:, cs, :], gqk[:, g:g + 1], ALU.mult, [rcs.b, gqk.b], [], pw=[tabs.b])
                for g in range(4):
                    C.cp(Wkd[:, :, g * 128:(g + 1) * 128].rearrange("p k (u d) -> p k u d", u=2),
                         Win[:, :, 1024 + g * 64:1024 + (g + 1) * 64].unsqueeze(2).broadcast_to([128, KT, 2, 64]),
                         [Win.b], [], pw=[Wkd.b])
                for src, dst in ((Win[:, :, 512:1024], Wqr), (Wkd[:, :, :], Wkr)):
                    sv = src.rearrange("p k (ha hf f) -> p k ha hf f", hf=2, f=16)
                    dv = dst[:, :, :].rearrange("p k (ha hf f) -> p k ha hf f", hf=2, f=16)
                    C.ts(dv[:, :, :, 0, :], sv[:, :, :, 1, :], -1.0, ALU.mult, [Win.b, Wkd.b], [], pw=[dst.b])
                    C.cp(dv[:, :, :, 1, :], sv[:, :, :, 0, :], [Win.b, Wkd.b], [], pw=[dst.b])
                nblk = 0
                nq = 0
                nv = 0
                items0 = [(b, bi) for b in range(NB) for bi in range(len(BLOCKS))]

                def load_x(idx):
                    b, bi = items0[idx]
                    s0, n = BLOCKS[bi]
                    C.dma("sp", xt2[idx % 2][:, :, :n], xT_view(b, s0, n), writes=[xt2[idx % 2].b])
                load_x(0)
                for b in range(NB):
                    for bi, (s0, n) in enumerate(BLOCKS):
                        xt = xt2[nblk % 2]
                        u = ub[nblk % 2]
                        nblk += 1
                        j5 = 4 if bi == 0 else b
                        if nblk < len(items0):
                            load_x(nblk)
                        norm_block(xt, n, sq, rstd,
                                   VecFn(lambda k, j5=j5: A1[:, 0, 0, k, j5:j5 + 1], [A1.b]),
                                   VecFn(lambda k, j5=j5: modvec(0, 0, k, j5), [modT.b]), hT)
                        for m in range(4):
                            pt, pb = C.ps("g", [0, 1, 2, 3, 4, 5])
                            for k in range(KT):
                                C.mm(pt[:, :n], Win[:, k, m * 128:(m + 1) * 128], hT[:, k, :n], k == 0, k == KT - 1,
                                     [Win.b, hT.b], [pb])
                            C.cp(u[:, m, :n], pt[:, :n], [pb], [], eng="act", pw=[u.b])
                        C.dma("sp", uT_d[b].rearrange("(m p) s -> p m s", p=128)[:, :, s0:s0 + n], u[:, :, :n], reads=[u.b])
                        for which in range(2):
                            for m in range(4):
                                if which == 0:
                                    W0, c0, W1_ = Win, 512 + m * 128, Wqr
                                    dst = qT_d[b, m, :, s0:s0 + n]
                                else:
                                    W0, c0, W1_ = Wkd, m * 128, Wkr
                                    dst = kT_d[b, m, :, s0:s0 + n]
                                pq, pqb = C.ps("g", [0, 1, 2, 3, 4, 5])
                                for k in range(KT):
                                    C.mm(pq[:, :n], W0[:, k, c0:c0 + 128], hT[:, k, :n], k == 0, k == KT - 1, [W0.b, hT.b], [pqb])
                                pr, prb = C.ps("g", [0, 1, 2, 3, 4, 5])
                                for k in range(KT):
                                    C.mm(pr[:, :n], W1_[:, k, m * 128:(m + 1) * 128], hT[:, k, :n], k == 0, k == KT - 1,
                                         [W1_.b, hT.b], [prb])
                                i2 = nq % 2
                                o = qo[nq % 3]
                                nq += 1
                                C.act(sqh[i2][:, :n], pq[:, :n], AF.Square, [pqb], [sqh[i2].b])
                                pss, psb_ = C.ps("g", [0, 1, 2, 3, 4, 5])
                                C.mm(pss[:, :n], bd[:], sqh[i2][:, :n], True, True, [bd.b, sqh[i2].b], [psb_])
                                C.act(rsh[i2][:, :n], pss[:, :n], AF.Sqrt, [psb_, eps_t.b], [rsh[i2].b], scale=1.0 / 64, bias=eps_t[:, 0:1])
                                C.recip(rsh[i2][:, :n], rsh[i2][:, :n], [rsh[i2].b], [rsh[i2].b])
                                C.tt(t1[i2][:, :n], pq[:, :n], tabs[:, 2 * which, s0:s0 + n], ALU.mult, [pqb, tabs.b], [t1[i2].b])
                                C.tt(t2[i2][:, :n], pr[:, :n], tabs[:, 2 * which + 1, s0:s0 + n], ALU.mult, [prb, tabs.b], [t2[i2].b])
                                C.tt(t1[i2][:, :n], t1[i2][:, :n], t2[i2][:, :n], ALU.add, [t1[i2].b, t2[i2].b], [t1[i2].b], eng="pool")
                                C.tt(o[:, :n], t1[i2][:, :n], rsh[i2][:, :n], ALU.mult, [t1[i2].b, rsh[i2].b], [o.b])
                                C.dma("sp", dst, o[:, :n], reads=[o.b])
                        for tt_ in range(n // 128):
                            pv, pvb = C.ps("g", [0, 1, 2, 3, 4, 5])
                            for k in range(KT):
                                C.mm(pv[:, :256], hT[:, k, tt_ * 128:(tt_ + 1) * 128], Win[:, k, 1280:1536], k == 0, k == KT - 1,
                                     [hT.b, Win.b], [pvb])
                            o = vo[nv % 3]
                            nv += 1
                            C.cp(o[:], pv[:, :256], [pvb], [o.b], eng="act")
                            C.dma("sp", vtok_d[b, s0 + tt_ * 128:s0 + (tt_ + 1) * 128, :], o[:], reads=[o.b])

        def gen_l0_attn(es):
            if True:
                k_ = SB(es, nc, "bkT", [128, T], BF16)
                q_ = SB(es, nc, "bqT", [128, T], BF16)
                v_ = SB(es, nc, "bvt", [128, 18, 64], BF16)
                pT = [SB(es, nc, "bpT%d" % i, [128, 512], BF16) for i in range(2)]
                numS = [SB(es, nc, "bnumS%d" % i, [128, 512], F32) for i in range(2)]
                denS = [SB(es, nc, "bdenS%d" % i, [128, 512], F32) for i in range(2)]
                ao = [SB(es, nc, "bao%d" % i, [128, 512], BF16) for i in range(2)]
                npT = 0
                nao = 0
                pend = None

                def finish(ns_, ds_, o, b, g, s0, n):
                    C.recip(ds_[:, :n], ds_[:, :n], [ds_.b], [ds_.b])
                    C.tt(o[:, :n], ns_[:, :n], ds_[:, :n], ALU.mult, [ns_.b, ds_.b], [o.b])
                    C.dma("sp", attnT_d[b, g, :, s0:s0 + n], o[:, :n], reads=[o.b])
                for b in range(NB):
                    for g in range(4):
                        C.dma("sp", k_[:], kT_d[b, g], writes=[k_.b])
                        C.dma("sp", q_[:], qT_d[b, g], writes=[q_.b])
                        C.dma("sp", v_[:], vtok_d[b].rearrange("(t p) c -> p t c", p=128)[:, :, g * 64:(g + 1) * 64], writes=[v_.b])
                        for bi, (s0, n) in enumerate(BLOCKS):
                            kts = range(2) if bi == 0 else range(18)
                            num, numb = C.psb[4]
                            den, denb = C.psb[5]
                            steps = [(kt, hh) for kt in kts for hh in range(2)]

                            def emit_s(kt, hh):
                                p0 = hh * 64
                                pss, pssb = C.ps("sa", [6, 7])
                                C.mm(pss[:, :n], k_[p0:p0 + 64, kt * 128:(kt + 1) * 128], q_[p0:p0 + 64, s0:s0 + n],
                                     True, True, [k_.b, q_.b], [pssb])
                                return pss, pssb
                            cur = emit_s(*steps[0])
                            for si, (kt, hh) in enumerate(steps):
                                p0 = hh * 64
                                nxt = emit_s(*steps[si + 1]) if si + 1 < len(steps) else None
                                pss, pssb = cur
                                p = pT[npT % 2]
                                npT += 1
                                C.act(p[:, :n], pss[:, :n], AF.Exp, [pssb], [p.b], scale=0.125)
                                first, last = kt == kts[0], kt == kts[-1]
                                C.mm(num[p0:p0 + 64, :n], v_[:, kt, :], p[:, :n], first, last, [v_.b, p.b], [numb])
                                C.mm(den[p0:p0 + 64, :n], ones_bf[:, 0:64], p[:, :n], first, last, [ones_bf.b, p.b], [denb])
                                cur = nxt
                                yield "step"
                            ns_, ds_ = numS[nao % 2], denS[nao % 2]
                            C.cp(ns_[:, :n], num[:, :n], [numb], [ns_.b], eng="act")
                            C.cp(ds_[:, :n], den[:, :n], [denb], [ds_.b], eng="act")
                            if pend is not None:
                                finish(*pend)
                            pend = (ns_, ds_, ao[nao % 2], b, g, s0, n)
                            nao += 1
                if pend is not None:
                    finish(*pend)

        s5_sm_in = din("s5_sm", [128, 3, 2, 16])
        s5_bpad_in = din("s5_bpad", [128, 2, 2, 16, 128])
        s5_cpad_in = din("s5_cpad", [128, 2, 2, 16, 128])
        s5_dpad_in = din("s5_dpad", [128, 4, 128])
        s5_wglu_in = din("s5_w_glu", [512, 512])
        s5T_d = dscr("s5T", [NB, 4, 128, T], BF16)
        TL = 256
        NTL = T // TL
        TWO_PI = 6.283185307179586
        MAGIC = 12582912.0

        def gen_l0_s5(es):
            if True:
                sm = SB(es, nc, "s5sm", [128, 3, 32], F32)
                pr_ = SB(es, nc, "s5pr", [128, 13, 32], F32)
                Tc = SB(es, nc, "s5Tc", [128, 32, TL], F32)
                Ts = SB(es, nc, "s5Ts", [128, 32, TL], F32)
                cs_k = SB(es, nc, "s5csk", [128, 4, 32], F32)
                sTpm = SB(es, nc, "s5sTpm", [128, 32, 2], F32)
                Bpad = SB(es, nc, "s5B", [128, 2, 32, 128], BF16)
                Cpad = SB(es, nc, "s5C", [128, 2, 32, 128], BF16)
                Dpad = SB(es, nc, "s5D", [128, 4, 128], BF16)
                Wg = SB(es, nc, "s5Wg", [128, 4, 512], BF16)
                uT2 = [SB(es, nc, "s5uT%d" % i, [128, T], BF16) for i in range(2)]
                ysum = SB(es, nc, "s5ys", [128, T], F32)
                gT = SB(es, nc, "s5gT", [128, 4, T], BF16)
                es2 = ExitStack()
                tmpT = SB(es2, nc, "s5tmpT", [128, 32, TL // 2], F32)
                ldf = SB(es2, nc, "s5ldf", [128, 2, 32, 128], F32)
                cw = tmpT
                dl = SB(es2, nc, "s5dl", [128, 4, 128], F32)
                C.dma("sp", sm[:].rearrange("p a (d j) -> p a d j", d=2), s5_sm_in, writes=[sm.b])
                load_w_bf16(Wg, s5_wglu_in, 4, 512)
                lr, li, ldt = sm[:, 0, :], sm[:, 1, :], sm[:, 2, :]
                R_ = lambda i: pr_[:, i, :]
                DT, MAG, TH, Y, SN, CO, AR, AI, DEN, KR, KI, TMP, TMP2 = range(13)
                P = [pr_.b, sm.b]
                C.act(R_(DT), ldt, AF.Exp, P, [pr_.b])
                C.tt(R_(TH), li, R_(DT), ALU.mult, P, [pr_.b])
                C.tt(R_(TMP), lr, R_(DT), ALU.mult, P, [pr_.b])
                C.act(R_(MAG), R_(TMP), AF.Exp, P, [pr_.b])

                def sin_of(dst, ang_row, shift):
                    C.ts(R_(Y), ang_row, 1.0 / TWO_PI, ALU.mult, P, [pr_.b], s2=shift, op1=ALU.add)
                    C.ts(R_(TMP), R_(Y), MAGIC, ALU.add, P, [pr_.b], s2=MAGIC, op1=ALU.subtract)
                    C.tt(R_(Y), R_(Y), R_(TMP), ALU.subtract, P, [pr_.b])
                    C.act(dst, R_(Y), AF.Sin, P, [pr_.b], scale=TWO_PI * 0.999999)
                sin_of(R_(SN), R_(TH), 0.0)
                sin_of(R_(CO), R_(TH), 0.25)
                C.tt(R_(AR), R_(MAG), R_(CO), ALU.mult, P, [pr_.b])
                C.tt(R_(AI), R_(MAG), R_(SN), ALU.mult, P, [pr_.b])
                C.ts(R_(TMP2), R_(AR), -1.0, ALU.add, P, [pr_.b])
                C.tt(R_(DEN), lr, lr, ALU.mult, P, [pr_.b])
                C.tt(R_(TMP), li, li, ALU.mult, P, [pr_.b])
                C.tt(R_(DEN), R_(DEN), R_(TMP), ALU.add, P, [pr_.b])
                C.recip(R_(DEN), R_(DEN), P, [pr_.b])
                C.tt(R_(KR), R_(TMP2), lr, ALU.mult, P, [pr_.b])
                C.tt(R_(TMP), R_(AI), li, ALU.mult, P, [pr_.b])
                C.tt(R_(KR), R_(KR), R_(TMP), ALU.add, P, [pr_.b])
                C.tt(R_(KR), R_(KR), R_(DEN), ALU.mult, P, [pr_.b])
                C.tt(R_(KI), R_(AI), lr, ALU.mult, P, [pr_.b])
                C.tt(R_(TMP), R_(TMP2), li, ALU.mult, P, [pr_.b])
                C.tt(R_(KI), R_(KI), R_(TMP), ALU.subtract, P, [pr_.b])
                C.tt(R_(KI), R_(KI), R_(DEN), ALU.mult, P, [pr_.b])
                TB = [Tc.b, Ts.b, cs_k.b, tmpT.b, pr_.b]
                C.memset(Tc[:, :, 0:1], 1.0, [Tc.b])
                C.memset(Ts[:, :, 0:1], 0.0, [Ts.b])
                C.cp(cs_k[:, 0, :], R_(CO), TB, [cs_k.b])
                C.cp(cs_k[:, 1, :], R_(SN), TB, [cs_k.b])
                kk = 1
                while kk <= TL:
                    ck, sk = cs_k[:, 0, :], cs_k[:, 1, :]
                    if kk < TL:
                        ckb = ck.unsqueeze(2).broadcast_to([128, 32, kk])
                        skb = sk.unsqueeze(2).broadcast_to([128, 32, kk])
                        C.tt(tmpT[:, :, 0:kk], Ts[:, :, 0:kk], skb, ALU.mult, TB, [tmpT.b])
                        C.tt(Tc[:, :, kk:2 * kk], Tc[:, :, 0:kk], ckb, ALU.mult, TB, [Tc.b])
                        C.tt(Tc[:, :, kk:2 * kk], Tc[:, :, kk:2 * kk], tmpT[:, :, 0:kk], ALU.subtract, TB, [Tc.b])
                        C.tt(tmpT[:, :, 0:kk], Tc[:, :, 0:kk], skb, ALU.mult, TB, [tmpT.b])
                        C.tt(Ts[:, :, kk:2 * kk], Ts[:, :, 0:kk], ckb, ALU.mult, TB, [Ts.b])
                        C.tt(Ts[:, :, kk:2 * kk], Ts[:, :, kk:2 * kk], tmpT[:, :, 0:kk], ALU.add, TB, [Ts.b])
                        C.tt(cs_k[:, 2, :], ck, ck, ALU.mult, TB, [cs_k.b])
                        C.tt(cs_k[:, 3, :], sk, sk, ALU.mult, TB, [cs_k.b])
                        C.tt(cs_k[:, 3, :], cs_k[:, 2, :], cs_k[:, 3, :], ALU.subtract, TB, [cs_k.b])
                        C.tt(cs_k[:, 2, :], ck, sk, ALU.mult, TB, [cs_k.b])
                        C.ts(cs_k[:, 1, :], cs_k[:, 2, :], 2.0, ALU.mult, TB, [cs_k.b])
                        C.cp(cs_k[:, 0, :], cs_k[:, 3, :], TB, [cs_k.b])
                    kk *= 2
                C.ts(sTpm[:, :, 0], cs_k[:, 1, :], -1.0, ALU.mult, [cs_k.b], [], pw=[sTpm.b])
                C.cp(sTpm[:, :, 1], cs_k[:, 1, :], [cs_k.b], [], pw=[sTpm.b])
                C.dma("sp", ldf[:].rearrange("p a (d j) c -> p a d j c", d=2), s5_bpad_in, writes=[ldf.b])
                C.cp(Bpad[:], ldf[:], [ldf.b], [Bpad.b])
                C.dma("sp", ldf[:].rearrange("p a (d j) c -> p a d j c", d=2), s5_cpad_in, writes=[ldf.b])
                for d in range(2):
                    dsl = slice(d * 16, d * 16 + 16)
                    cwv = cw[:].rearrange("p (a j) c -> p a j c", a=2)
                    krb = R_(KR)[:, dsl].unsqueeze(2).broadcast_to([128, 16, 128])
                    kib = R_(KI)[:, dsl].unsqueeze(2).broadcast_to([128, 16, 128])
                    C.tt(cwv[:, 0], ldf[:, 0, dsl, :], krb, ALU.mult, [ldf.b, pr_.b, Cpad.b], [cw.b])
                    C.tt(cwv[:, 1], ldf[:, 1, dsl, :], kib, ALU.mult, [ldf.b, pr_.b, cw.b], [cw.b])
                    C.tt(Cpad[:, 0, dsl, :], cwv[:, 0], cwv[:, 1], ALU.subtract, [cw.b], [], pw=[Cpad.b])
                    C.tt(cwv[:, 0], ldf[:, 0, dsl, :], kib, ALU.mult, [ldf.b, pr_.b, Cpad.b], [cw.b])
                    C.tt(cwv[:, 1], ldf[:, 1, dsl, :], krb, ALU.mult, [ldf.b, pr_.b, cw.b], [cw.b])
                    C.tt(cwv[:, 0], cwv[:, 0], cwv[:, 1], ALU.add, [cw.b], [cw.b])
                    C.ts(Cpad[:, 1, dsl, :], cwv[:, 0], -1.0, ALU.mult, [cw.b], [], pw=[Cpad.b])
                C.dma("sp", dl[:], s5_dpad_in, writes=[dl.b])
                C.cp(Dpad[:], dl[:], [dl.b], [Dpad.b])
                C.barrier()
                es2.close()
                yield "prep"
                ta = [SB(es, nc, "s5ta%d" % i, [128, 2, TL], F32) for i in range(2)]
                tb = [SB(es, nc, "s5tb%d" % i, [128, 2, TL], F32) for i in range(2)]
                cc = [SB(es, nc, "s5cc%d" % i, [128, 2, TL], F32) for i in range(4)]
                hb = [SB(es, nc, "s5hb%d" % i, [128, 2, TL], BF16) for i in range(8)]
                init = [SB(es, nc, "s5in%d" % i, [128, 4], F32) for i in range(4)]
                yv = [SB(es, nc, "s5yv%d" % i, [128, TL], F32) for i in range(2)]
                g1_ = [SB(es, nc, "s5g1%d" % i, [128, TL], F32) for i in range(2)]
                g2_ = [SB(es, nc, "s5g2%d" % i, [128, TL], F32) for i in range(2)]
                sg = [SB(es, nc, "s5sg%d" % i, [128, 512], F32) for i in range(1)] * 2
                so = [SB(es, nc, "s5so%d" % i, [128, 512], BF16) for i in range(1)] * 2


                nt = 0
                nt2 = 0
                nh = 0
                ny = 0
                ns = 0
                ta2 = [SB(es, nc, "s5ta2%d" % i, [128, 2, TL], F32) for i in range(2)]
                tb2 = [SB(es, nc, "s5tb2%d" % i, [128, 2, TL], F32) for i in range(2)]
                nu = 0
                for b in range(NB):
                    for kt in range(4):
                        uT = uT2[nu % 2]
                        nu += 1
                        C.dma("sp", uT[:], uT_d[b, kt * 128:(kt + 1) * 128, :], writes=[uT.b])
                        for d in range(2):
                            order = list(range(NTL)) if d == 0 else [0] + list(range(NTL - 1, 0, -1))
                            units = [(ti, tile_, j) for ti, tile_ in enumerate(order) for j in range(4)]

                            def emitA(ti, tile_, j):
                                nonlocal nt
                                cols = slice(tile_ * TL, tile_ * TL + TL)
                                J = d * 16 + kt * 4 + j
                                ps_, psb_ = C.ps("s", [0, 1])
                                psv = ps_[:].rearrange("p (a t) -> p a t", a=2)
                                C.mm(psv[:, 0, :], Bpad[:, 0, J, :], uT[:, cols], True, True, [Bpad.b, uT.b], [psb_])
                                C.mm(psv[:, 1, :], Bpad[:, 1, J, :], uT[:, cols], True, True, [Bpad.b, uT.b], [psb_])
                                a_, b_ = ta[nt % 2], tb[nt % 2]
                                c_ = cc[nt % 4]
                                nt += 1
                                if d == 0:
                                    tcv, tsv = Tc[:, J, :], Ts[:, J, :]
                                else:
                                    tcv, tsv = Tc[:, J, ::-1], Ts[:, J, ::-1]
                                tcb = tcv.unsqueeze(1).broadcast_to([128, 2, TL])
                                tsb = tsv.unsqueeze(1).broadcast_to([128, 2, TL])
                                C.tt(a_[:], psv, tcb, ALU.mult, [psb_, Tc.b], [a_.b])
                                C.tt(b_[:], psv[:, ::-1, :], tsb, ALU.mult, [psb_, Ts.b], [b_.b])
                                C.tt(c_[:, 0, :], a_[:, 0, :], b_[:, 0, :], ALU.add, [a_.b, b_.b], [], eng=S5E[0], pw=[c_.b])
                                C.tt(c_[:, 1, :], a_[:, 1, :], b_[:, 1, :], ALU.subtract, [a_.b, b_.b], [], eng=S5E[0], pw=[c_.b])
                                return (c_, tcb, tsb, J)

                            def emitB(ti, tile_, j, st):
                                nonlocal nt2, nh
                                c_, tcb, tsb, J = st
                                rb_ = R_(MAG)[:, J:J + 1].broadcast_to([128, TL])
                                iv = init[j]
                                for ri in range(2):
                                    seq = c_[:, ri, :] if d == 0 else c_[:, ri, ::-1]
                                    ini = 0.0 if ti == 0 else iv[:, ri:ri + 1]
                                    C.op("dve", lambda e, seq=seq, ini=ini, rb_=rb_: e.tensor_tensor_scan(
                                        out=seq, data0=rb_, data1=seq, initial=ini, op0=ALU.mult, op1=ALU.add),
                                        [c_.b, pr_.b, iv.b], [c_.b])
                                lc = TL - 1 if d == 0 else 0
                                if ti < len(order) - 1:
                                    cT_, sT_ = cs_k[:, 0, J:J + 1], cs_k[:, 1, J:J + 1]
                                    gre, gim = c_[:, 0, lc:lc + 1], c_[:, 1, lc:lc + 1]
                                    C.tt(iv[:, 2:4], c_[:, ::-1, lc], sTpm[:, J, :], ALU.mult, [c_.b, sTpm.b], [iv.b])
                                    C.stt(iv[:, 0:2], c_[:, :, lc], cT_, iv[:, 2:4], ALU.mult, ALU.add, [c_.b, cs_k.b, iv.b], [iv.b])
                                a2, b2 = ta2[nt2 % 2], tb2[nt2 % 2]
                                nt2 += 1
                                h_ = hb[nh % 8]
                                nh += 1
                                C.tt(a2[:], c_[:], tcb, ALU.mult, [c_.b, Tc.b], [a2.b])
                                C.tt(b2[:], c_[:, ::-1, :], tsb, ALU.mult, [c_.b, Ts.b], [b2.b], eng=S5E[1])
                                C.tt(h_[:, 0, :], a2[:, 0, :], b2[:, 0, :], ALU.subtract, [a2.b, b2.b], [], eng=S5E[2], pw=[h_.b])
                                C.tt(h_[:, 1, :], a2[:, 1, :], b2[:, 1, :], ALU.add, [a2.b, b2.b], [], eng=S5E[2], pw=[h_.b])
                                return (h_, J)

                            def readout(tile_, hbs):
                                nonlocal ny
                                cols = slice(tile_ * TL, tile_ * TL + TL)
                                py, pyb = C.ps("y", [2])
                                nmm = 8 + (1 if d == 0 else 0)
                                i_ = 0
                                for h_, J in hbs:
                                    for ri in range(2):
                                        C.mm(py[:, :TL], Cpad[:, ri, J, :], h_[:, ri, :], i_ == 0, i_ == nmm - 1, [Cpad.b, h_.b], [pyb])
                                        i_ += 1
                                if d == 0:
                                    C.mm(py[:, :TL], Dpad[:, kt, :], uT[:, cols], False, True, [Dpad.b, uT.b], [pyb])
                                    C.cp(ysum[:, cols], py[:, :TL], [pyb], [], eng="act", pw=[ysum.b])
                                else:
                                    y_ = yv[ny % 2]
                                    q1, q2 = g1_[ny % 2], g2_[ny % 2]
                                    ny += 1
                                    C.tt(y_[:], py[:, :TL], ysum[:, cols], ALU.add, [pyb, ysum.b], [y_.b])
                                    C.act(q1[:], y_[:], AF.Square, [y_.b], [q1.b])
                                    C.ts(q1[:], q1[:], 0.044715, ALU.mult, [q1.b], [q1.b], s2=1.0, op1=ALU.add, eng="pool")
                                    C.tt(q1[:], q1[:], y_[:], ALU.mult, [q1.b, y_.b], [q1.b], eng="pool")
                                    C.act(q2[:], q1[:], AF.Sigmoid, [q1.b], [q2.b], scale=1.5957691216057308)
                                    C.tt(gT[:, kt, cols], q2[:], y_[:], ALU.mult, [q2.b, y_.b], [], eng="pool", pw=[gT.b])

                            stA = emitA(*units[0])
                            hbs = []
                            for ui, (ti, tile_, j) in enumerate(units):
                                stN = emitA(*units[ui + 1]) if ui + 1 < len(units) else None
                                hbs.append(emitB(ti, tile_, j, stA))
                                if j == 3:
                                    readout(tile_, hbs)
                                    hbs = []
                                stA = stN
                                yield "unit"
                    for bi, (s0, n) in enumerate(BLOCKS):
                        for m in range(4):
                            pz, pzb = C.ps("z", [3])
                            for k4 in range(4):
                                C.mm(pz[:, :n], Wg[:, k4, m * 128:(m + 1) * 128], gT[:, k4, s0:s0 + n], k4 == 0, k4 == 3, [Wg.b, gT.b], [pzb])
                            s_ = sg[ns % 2]
                            o_ = so[ns % 2]
                            ns += 1
                            C.act(s_[:, :n], pz[:, :n], AF.Sigmoid, [pzb], [s_.b])
                            C.tt(o_[:, :n], s_[:, :n], gT[:, m, s0:s0 + n], ALU.mult, [s_.b, gT.b], [o_.b])
                            C.dma("sp", s5T_d[b, m, :, s0:s0 + n], o_[:, :n], reads=[o_.b])

        def stage_l0_mix():
            es_s, es_a = ExitStack(), ExitStack()
            g5 = gen_l0_s5(es_s)
            assert next(g5) == "prep"
            ga = gen_l0_attn(es_a)
            a_alive, s_alive = True, True
            while a_alive or s_alive:
                if s_alive:
                    try:
                        next(g5)
                    except StopIteration:
                        s_alive = False
                for _ in range(2 if s_alive else 64):
                    if a_alive:
                        try:
                            next(ga)
                        except StopIteration:
                            a_alive = False
            es_a.close()
            es_s.close()

        ev_w_out = din("ev_w_out", [D, D])
        ffn_w1 = din("ffn_w1", [D, 3584])
        ffn_w3 = din("ffn_w3", [D, 3584])
        ffn_w2 = din("ffn_w2", [3584, D])
        h2T_d = dscr("h2T", [NB, D, T], BF16)

        def h2T_view(b, s0, n):
            return h2T_d[b].rearrange("(k p) s -> p k s", p=128)[:, :, s0:s0 + n]

        def stage_out_norm2(l, w_out_ap, mixA_d, mixB_d, blocks, after=None):
            def run():
                with ExitStack() as es:
                    Wo = SB(es, nc, "oWo", [128, KT, D], BF16)
                    xt2 = [SB(es, nc, "ox%d" % i, [128, KT, 512], F32) for i in range(2)]
                    mx2 = [SB(es, nc, "omx%d" % i, [128, KT, 512], BF16) for i in range(2)]
                    sq = SB(es, nc, "osq", [128, KT, 512], BF16)
                    rstd = SB(es, nc, "orstd", [128, 512], F32)
                    h2 = [SB(es, nc, "oh2%d" % i, [128, KT, 512], BF16) for i in range(2)]
                    load_w_bf16(Wo, w_out_ap, KT, D)
                    hook = after(es) if after is not None else None
                    items = [(b, bi) for b in range(NB) for bi in blocks]

                    def load_block(idx):
                        b, bi = items[idx]
                        s0, n = BLOCKS[bi]
                        xt, mx = xt2[idx % 2], mx2[idx % 2]
                        C.dma("sp", xt[:, :, :n], xT_view(b, s0, n), reads=[xT_buf[b][bi]], writes=[xt.b])
                        C.dma("sp", mx[:, 0:4, :n], mixA_d[b].rearrange("m p s -> p m s")[:, :, s0:s0 + n], pwrites=[mx.b])
                        C.dma("sp", mx[:, 4:8, :n], mixB_d[b].rearrange("m p s -> p m s")[:, :, s0:s0 + n], pwrites=[mx.b])
                    load_block(0)
                    for idx, (b, bi) in enumerate(items):
                        if True:
                            s0, n = BLOCKS[bi]
                            j5 = 4 if bi == 0 else b
                            xt, mx, h = xt2[idx % 2], mx2[idx % 2], h2[idx % 2]
                            if idx + 1 < len(items):
                                load_block(idx + 1)
                            for m in range(KT):
                                po, pob = C.ps("g", [0, 1, 2, 3])
                                for k in range(KT):
                                    C.mm(po[:, :n], Wo[:, k, m * 128:(m + 1) * 128], mx[:, k, :n], k == 0, k == KT - 1, [Wo.b, mx.b], [pob])
                                C.stt(xt[:, m, :n], po[:, :n], modvec(l, 2, m, j5), xt[:, m, :n], ALU.mult, ALU.add,
                                      [pob, modT.b, xt.b], [], pw=[xt.b])
                            C.dma("sp", xT_view(b, s0, n), xt[:, :, :n], reads=[xt.b], writes=[xT_buf[b][bi]])
                            norm_block(xt, n, sq, rstd,
                                       VecFn(lambda k, j5=j5: A1[:, l, 1, k, j5:j5 + 1], [A1.b]),
                                       VecFn(lambda k, j5=j5: modvec(l, 3, k, j5), [modT.b]), h)
                            C.dma("sp", h2T_view(b, s0, n), h[:, :, :n], reads=[h.b])
                            if hook is not None:
                                hook(b, bi, s0, n, xt, h)
            return run

        def ffn_multi(l, passes, blocks):
            HT = 14
            NCH = 7
            def run():
                with ExitStack() as es:
                    W1 = SB(es, nc, "fW1", [128, KT, HT * 128], BF16)
                    W3 = SB(es, nc, "fW3", [128, KT, HT * 128], BF16)
                    W2 = SB(es, nc, "fW2", [128, HT, D], BF16)
                    W1b = [Buf("W1c%d" % i) for i in range(NCH)]
                    W3b = [Buf("W3c%d" % i) for i in range(NCH)]
                    W2b = [Buf("W2c%d" % i) for i in range(NCH)]
                    xt2 = [SB(es, nc, "fx%d" % i, [128, KT, 512], F32) for i in range(2)]
                    h2 = [SB(es, nc, "fh%d" % i, [128, KT, 512], BF16) for i in range(2)]
                    G = SB(es, nc, "fG", [128, HT, 512], BF16)
                    sl = [SB(es, nc, "fsl%d" % i, [128, 512], F32) for i in range(2)]
                    use_rw = passes[0][3] is not None
                    rw = [SB(es, nc, "frw%d" % i, [128, 512], F32) for i in range(2)] if use_rw else None
                    tq = [SB(es, nc, "ftq%d" % i, [128, 512], F32) for i in range(2)] if use_rw else None
                    nsl = 0
                    items = [(pi, b, bi) for pi in range(len(passes)) for b in range(NB) for bi in blocks]

                    def load_block(idx):
                        pi, b, bi = items[idx]
                        s0, n = BLOCKS[bi]
                        xt, h = xt2[idx % 2], h2[idx % 2]
                        r_ = rw[idx % 2] if use_rw else None
                        C.dma("sp", h[:, :, :n], h2T_view(b, s0, n), writes=[h.b])
                        C.dma("sp", xt[:, :, :n], xT_view(b, s0, n), reads=[xT_buf[b][bi]], writes=[xt.b])
                        if r_ is not None:
                            C.dma("sp", r_[:, :n], passes[pi][3][b, s0 - LC:s0 - LC + n].partition_broadcast(128), writes=[r_.b])
                        return xt, h, r_

                    def load_weights(pi):
                        w1_ap, w3_ap, w2_ap, _ = passes[pi]
                        v1 = w1_ap.rearrange("(k p) n -> p k n", p=128)
                        v3 = w3_ap.rearrange("(k p) n -> p k n", p=128)
                        v2 = w2_ap.rearrange("(k p) n -> p k n", p=128)
                        for c in range(NCH):
                            cs_ = slice(c * 256, (c + 1) * 256)
                            C.dma("pool", W1[:, :, cs_], v1[:, :, cs_], writes=[W1b[c]])
                            C.dma("pool", W3[:, :, cs_], v3[:, :, cs_], writes=[W3b[c]])
                        for c in range(NCH):
                            C.dma("pool", W2[:, 2 * c:2 * c + 2, :], v2[:, 2 * c:2 * c + 2, :], writes=[W2b[c]])

                    cur = load_block(0)
                    for idx, (pi, b, bi) in enumerate(items):
                        if idx == 0 or items[idx - 1][0] != pi:
                            load_weights(pi)
                        s0, n = BLOCKS[bi]
                        j5 = 4 if bi == 0 else b
                        xt, h, r_ = cur
                        nxt = load_block(idx + 1) if idx + 1 < len(items) else None
                        for mt in range(HT):
                            pa, pab = C.ps("fg", [0, 1, 2, 3, 4, 5])
                            for k in range(KT):
                                C.mm(pa[:, :n], W1[:, k, mt * 128:(mt + 1) * 128], h[:, k, :n], k == 0, k == KT - 1, [W1b[mt // 2], h.b], [pab])
                            pb_, pbb = C.ps("fg", [0, 1, 2, 3, 4, 5])
                            for k in range(KT):
                                C.mm(pb_[:, :n], W3[:, k, mt * 128:(mt + 1) * 128], h[:, k, :n], k == 0, k == KT - 1, [W3b[mt // 2], h.b], [pbb])
                            s_ = sl[nsl % 2]
                            nsl += 1
                            C.act(s_[:, :n], pa[:, :n], AF.Silu, [pab], [s_.b])
                            C.tt(G[:, mt, :n], s_[:, :n], pb_[:, :n], ALU.mult, [s_.b, pbb], [], pw=[G.b])
                        for m in range(KT):
                            po, pob = C.ps("fo", [6, 7])
                            for mt in range(HT):
                                C.mm(po[:, :n], W2[:, mt, m * 128:(m + 1) * 128], G[:, mt, :n], mt == 0, mt == HT - 1, [W2b[mt // 2], G.b], [pob])
                            if r_ is None:
                                C.stt(xt[:, m, :n], po[:, :n], modvec(l, 5, m, j5), xt[:, m, :n], ALU.mult, ALU.add,
                                      [pob, modT.b, xt.b], [], pw=[xt.b])
                            else:
                                t_ = tq[m % 2]
                                C.tt(t_[:, :n], po[:, :n], r_[:, :n], ALU.mult, [pob, r_.b], [t_.b])
                                C.stt(xt[:, m, :n], t_[:, :n], modvec(l, 5, m, j5), xt[:, m, :n], ALU.mult, ALU.add,
                                      [t_.b, modT.b, xt.b], [], pw=[xt.b])
                        C.dma("sp", xT_view(b, s0, n), xt[:, :, :n], reads=[xt.b], writes=[xT_buf[b][bi]])
                        cur = nxt
            return run

        ALLB = list(range(len(BLOCKS)))
        LATB = list(range(1, len(BLOCKS)))

        od_w_in = din("od_w_in", [D, 3072])
        hy_cw_in = din("hy_cw", [128, 12, 4])
        hv_d = dscr("hv", [NB, L, 512], BF16)
        hg1_d = dscr("hg1", [NB, L, 512], BF16)
        hg2T_d = dscr("hg2T", [NB, 512, L], BF16)
        naq_d = dscr("naq", [NB, 4, 128, L], BF16)
        nak_d = dscr("nak", [NB, 4, 128, T], BF16)
        nav_d = dscr("nav", [NB, T, 512], BF16)

        def stage_l1_inproj():
            with ExitStack() as es:
                Wod = SB(es, nc, "Wod", [128, KT, 3072], BF16)
                cw = SB(es, nc, "hcw", [128, 12, 4], F32)
                hT = SB(es, nc, "l1hT", [128, KT, T], BF16)
                xt2 = [SB(es, nc, "l1x%d" % i, [128, KT, 512], F32) for i in range(2)]
                sq = SB(es, nc, "l1sq", [128, KT, 512], BF16)
                rstd = SB(es, nc, "l1rstd", [128, 512], F32)
                hblk = SB(es, nc, "l1hb", [128, KT, 512], BF16)
                pbuf = [SB(es, nc, "l1pb%d" % i, [128, L + 2], F32) for i in range(2)]
                ucv = [SB(es, nc, "l1uc%d" % i, [128, L], F32) for i in range(2)]
                tok = [SB(es, nc, "l1tk%d" % i, [128, 16, 128], BF16) for i in range(2)]
                g2o = [SB(es, nc, "l1g2%d" % i, [128, L], BF16) for i in range(2)]
                qo = [SB(es, nc, "l1qo%d" % i, [128, 512], BF16) for i in range(3)]
                vo = [SB(es, nc, "l1vo%d" % i, [128, 512], BF16) for i in range(3)]
                load_w_bf16(Wod, od_w_in, KT, 3072)
                C.dma("sp", cw[:], hy_cw_in, writes=[cw.b])
                for pb_ in pbuf:
                    C.memset(pb_[:, 0:1], 0.0, [pb_.b])
                    C.op("dve", lambda e, pb_=pb_: e.memset(pb_[:, L + 1:L + 2], 0.0), [], [], [pb_.b])
                nx = 0
                nm = 0
                nq = 0
                nv = 0
                for b in range(NB):
                    for bi, (s0, n) in enumerate(BLOCKS):
                        xt = xt2[nx % 2]
                        nx += 1
                        j5 = 4 if bi == 0 else b
                        C.dma("sp", xt[:, :, :n], xT_view(b, s0, n), reads=[xT_buf[b][bi]], writes=[xt.b])
                        norm_block(xt, n, sq, rstd,
                                   VecFn(lambda k, j5=j5: A1[:, 1, 0, k, j5:j5 + 1], [A1.b]),
                                   VecFn(lambda k, j5=j5: modvec(1, 0, k, j5), [modT.b]), hblk)
                        C.cp(hT[:, :, s0:s0 + n], hblk[:, :, :n], [hblk.b], [], eng="pool", pw=[hT.b])
                    for m in range(12):
                        pbf, uc = pbuf[nm % 2], ucv[nm % 2]
                        nm += 1
                        for i in range(4):
                            pt, pb = C.ps("g", [0, 1, 2, 3])
                            for k in range(KT):
                                C.mm(pt[:, :512], Wod[:, k, m * 128:(m + 1) * 128], hT[:, k, LC + i * 512:LC + (i + 1) * 512],
                                     k == 0, k == KT - 1, [Wod.b, hT.b], [pb])
                            C.cp(pbf[:, 1 + i * 512:1 + (i + 1) * 512], pt[:, :512], [pb], [], eng="act", pw=[pbf.b])
                        C.ts(uc[:], pbf[:, 1:L + 1], cw[:, m, 1:2], ALU.mult, [pbf.b, cw.b], [uc.b], s2=cw[:, m, 3:4], op1=ALU.add)
                        C.stt(uc[:], pbf[:, 0:L], cw[:, m, 0:1], uc[:], ALU.mult, ALU.add, [pbf.b, cw.b, uc.b], [uc.b])
                        C.stt(uc[:], pbf[:, 2:L + 2], cw[:, m, 2:3], uc[:], ALU.mult, ALU.add, [pbf.b, cw.b, uc.b], [uc.b])
                        if m < 8:
                            tk = tok[nm % 2]
                            for t4 in range(4):
                                pt, pb = C.ps("t", [4, 5])
                                for i in range(4):
                                    tt_ = t4 * 4 + i
                                    C.tr(pt[:, i * 128:(i + 1) * 128], uc[:, tt_ * 128:(tt_ + 1) * 128], ident[:], [uc.b, ident.b], [pb])
                                C.cp(tk[:, t4 * 4:(t4 + 1) * 4, :], pt[:].rearrange("p (a c) -> p a c", a=4), [pb], [],
                                     eng=("act" if t4 % 2 else "dve"), pw=[tk.b])
                            dst = (hv_d if m < 4 else hg1_d)[b].rearrange("(tt p) c -> p tt c", p=128)[:, :, (m % 4) * 128:(m % 4 + 1) * 128]
                            C.dma("sp", dst, tk[:], reads=[tk.b])
                        else:
                            go = g2o[nm % 2]
                            C.cp(go[:], uc[:], [uc.b], [go.b], eng="pool")
                            C.dma("sp", hg2T_d[b, (m - 8) * 128:(m - 7) * 128, :], go[:], reads=[go.b])
                    for bi, (s0, n) in enumerate(BLOCKS):
                        for which in range(2):
                            if which == 0 and bi == 0:
                                continue
                            for m in range(4):
                                c0 = 1536 + which * 512 + m * 128
                                pt, pb = C.ps("g", [0, 1, 2, 3])
                                for k in range(KT):
                                    C.mm(pt[:, :n], Wod[:, k, c0:c0 + 128], hT[:, k, s0:s0 + n], k == 0, k == KT - 1, [Wod.b, hT.b], [pb])
                                o = qo[nq % 3]
                                nq += 1
                                if which == 0:
                                    C.act(o[:, :n], pt[:, :n], AF.Copy, [pb], [o.b], scale=0.125)
                                    C.dma("sp", naq_d[b, m, :, s0 - LC:s0 - LC + n], o[:, :n], reads=[o.b])
                                else:
                                    C.cp(o[:, :n], pt[:, :n], [pb], [o.b])
                                    C.dma("sp", nak_d[b, m, :, s0:s0 + n], o[:, :n], reads=[o.b])
                        for tt_ in range(n // 128):
                            pt, pb = C.ps("g", [0, 1, 2, 3])
                            for k in range(KT):
                                C.mm(pt[:, :512], hT[:, k, s0 + tt_ * 128:s0 + (tt_ + 1) * 128], Wod[:, k, 2560:3072], k == 0, k == KT - 1,
                                     [hT.b, Wod.b], [pb])
                            o = vo[nv % 3]
                            nv += 1
                            C.cp(o[:], pt[:, :512], [pb], [o.b], eng="act")
                            C.dma("sp", nav_d[b, s0 + tt_ * 128:s0 + (tt_ + 1) * 128, :], o[:], reads=[o.b])

        dftC_in = din("dftC", [128, 16, 2048])
        dftS_in = din("dftS", [128, 16, 2048])
        hyz_in = din("hyz", [33, L])
        hynt_in = din("hynt", [128, 16])
        hy_w1_in = din("hy_w1", [33, 64])
        hy_w2_in = din("hy_w2", [64, 64])
        hy_w3_in = din("hy_w3", [64, 2048])
        hy_fb_in = din("hy_fb", [64, 3])
        hy_dec_in = din("hy_dec", [128, 2048])
        hy_d_in = din("hy_dbc", [128, 2, 512])
        alt_in = din("alt", [128, 513])
        hn_d = dscr("hn", [4, L, 512], BF16)
        spec_d = dscr("spec", [2, 2, L, 512], F32)
        hyoT_d = dscr("hyoT", [NB, 4, 128, T], BF16)

        def stage_l1_hyena():
            with ExitStack() as es:
                nyq = SB(es, nc, "hynq", [1, 2, 512], F32)
                altf = SB(es, nc, "hyaltf", [128, 513], F32)
                altc = SB(es, nc, "hyaltc", [128, 1], BF16)
                altr = SB(es, nc, "hyaltr", [1, 512], BF16)
                ones_f = SB(es, nc, "hyones", [128, 128], F32)
                C.dma("sp", altf[:], alt_in, writes=[altf.b])
                C.cp(altc[:], altf[:, 0:1], [altf.b], [altc.b])
                C.cp(altr[:], altf[0:1, 1:513], [altf.b], [altr.b])
                C.memset(ones_f[:], 1.0, [ones_f.b])
                with ExitStack() as e1:
                    zT = SB(e1, nc, "hyzT", [33, L], F32)
                    w1 = SB(e1, nc, "hyw1", [33, 64], F32)
                    w2 = SB(e1, nc, "hyw2", [64, 64], F32)
                    w3 = SB(e1, nc, "hyw3", [64, 2048], F32)
                    fb = SB(e1, nc, "hyfb", [64, 3], F32)
                    nt_ = SB(e1, nc, "hynt", [128, 16], F32)
                    dec = SB(e1, nc, "hydec", [128, 2048], F32)
                    h1T = SB(e1, nc, "hyh1", [64, L], F32)
                    h2T = SB(e1, nc, "hyh2", [64, L], F32)
                    ya = SB(e1, nc, "hyya", [64, 512], F32)
                    yb = SB(e1, nc, "hyyb", [64, 512], F32)
                    hraw = SB(e1, nc, "hyraw", [128, 16, 512], F32)
                    wd = [SB(e1, nc, "hywd%d" % i, [128, 512], F32) for i in range(2)]
                    ab = [SB(e1, nc, "hyab%d" % i, [128, 512], F32) for i in range(2)]
                    rn = SB(e1, nc, "hyrn", [128, 512], F32)
                    hnb = SB(e1, nc, "hyhnb", [128, 16, 512], BF16)
                    C.dma("sp", zT[:], hyz_in, writes=[zT.b])
                    C.dma("sp", w1[:], hy_w1_in, writes=[w1.b])
                    C.dma("sp", w2[:], hy_w2_in, writes=[w2.b])
                    C.dma("sp", w3[:], hy_w3_in, writes=[w3.b])
                    C.dma("sp", fb[:], hy_fb_in, writes=[fb.b])
                    C.dma("sp", nt_[:], hynt_in, writes=[nt_.b])
                    C.dma("sp", dec[:], hy_dec_in, writes=[dec.b])
                    C.stt(dec[:], dec[:], -1.0, dec[:], ALU.mult, ALU.max, [dec.b], [dec.b])

                    def sin_layer(dst, W, kdim, src, bcol):
                        for i in range(4):
                            pt, pb = C.ps("g", [0, 1, 2, 3])
                            C.mm(pt[0:64, :512], W[0:kdim, :], src[0:kdim, i * 512:(i + 1) * 512], True, True, [W.b, src.b], [pb])
                            C.ts(ya[:], pt[0:64, :512], fb[:, bcol:bcol + 1], ALU.add, [pb, fb.b], [ya.b], s2=fb[:, 0:1], op1=ALU.mult)
                            C.ts(ya[:], ya[:], 1.0 / TWO_PI, ALU.mult, [ya.b], [ya.b])
                            C.ts(yb[:], ya[:], MAGIC, ALU.add, [ya.b], [yb.b], s2=MAGIC, op1=ALU.subtract)
                            C.tt(ya[:], ya[:], yb[:], ALU.subtract, [ya.b, yb.b], [ya.b])
                            C.act(dst[:, i * 512:(i + 1) * 512], ya[:], AF.Sin, [ya.b], [], scale=TWO_PI * 0.999999, pw=[dst.b])
                    sin_layer(h1T, w1, 33, zT, 1)
                    sin_layer(h2T, w2, 64, h1T, 2)
                    nw = 0
                    for cb in range(4):
                        pn, pnb = C.ps("n", [6, 7])
                        for kt in range(16):
                            pt, pb = C.ps("g", [0, 1, 2, 3])
                            C.mm(pt[:, :512], h2T[:, kt * 128:(kt + 1) * 128], w3[:, cb * 512:(cb + 1) * 512], True, True, [h2T.b, w3.b], [pb])
                            w_, a_ = wd[nw % 2], ab[nw % 2]
                            nw += 1
                            C.act(w_[:], dec[:, cb * 512:(cb + 1) * 512], AF.Exp, [dec.b, nt_.b], [w_.b], scale=nt_[:, kt:kt + 1])
                            C.tt(hraw[:, kt, :], pt[:, :512], w_[:], ALU.mult, [pb, w_.b], [], pw=[hraw.b])
                            C.stt(a_[:], hraw[:, kt, :], -1.0, hraw[:, kt, :], ALU.mult, ALU.max, [hraw.b], [a_.b])
                            C.mm(pn[:, :512], ones_f[:], a_[:], kt == 0, kt == 15, [ones_f.b, a_.b], [pnb])
                        C.ts(rn[:], pn[:, :512], EPS, ALU.add, [pnb], [rn.b])
                        C.recip(rn[:], rn[:], [rn.b], [rn.b])
                        C.tt(hnb[:], hraw[:], rn[:].unsqueeze(1).broadcast_to([128, 16, 512]), ALU.mult, [hraw.b, rn.b], [hnb.b])
                        C.dma("sp", hn_d[cb].rearrange("(kt p) c -> p kt c", p=128), hnb[:], reads=[hnb.b])
                    C.barrier()
                Cm = SB(es, nc, "hyC", [128, 16, 2048], BF16)
                Sm = SB(es, nc, "hyS", [128, 16, 2048], BF16)
                for c0 in range(0, 2048, 512):
                    C.dma("pool", Cm[:, :, c0:c0 + 512], dftC_in[:, :, c0:c0 + 512], pwrites=[Cm.b])
                    C.dma("pool", Sm[:, :, c0:c0 + 512], dftS_in[:, :, c0:c0 + 512], pwrites=[Sm.b])
                with ExitStack() as e2:
                    dbc = SB(e2, nc, "hydb", [128, 2, 512], F32)
                    C.dma("sp", dbc[:], hy_d_in, writes=[dbc.b])
                    hf = SB(e2, nc, "hyhf", [128, 16, 512], BF16)
                    hb_ = SB(e2, nc, "hyhb", [128, 16, 512], BF16)
                    hs = SB(e2, nc, "hyhs", [128, 16, 512], BF16)
                    so = [SB(e2, nc, "hyso%d" % i, [128, 512], F32) for i in range(3)]
                    nso = 0
                    for o in range(2):
                        C.dma("sp", hf[:], hn_d[o].rearrange("(kt p) c -> p kt c", p=128), writes=[hf.b])
                        C.dma("sp", hb_[:], hn_d[2 + o].rearrange("(kt p) c -> p kt c", p=128), writes=[hb_.b])
                        C.tt(hs[:], hf[:], hb_[:], ALU.add, [hf.b, hb_.b], [hs.b])
                        C.tt(hf[:], hf[:], hb_[:], ALU.subtract, [hf.b, hb_.b], [hf.b])
                        for mt in range(16):
                            for ri in range(2):
                                M_, src = (Cm, hs) if ri == 0 else (Sm, hf)
                                pt, pb = C.ps("g", [0, 1, 2, 3])
                                for kt in range(16):
                                    C.mm(pt[:, :512], M_[:, kt, mt * 128:(mt + 1) * 128], src[:, kt, :], kt == 0, kt == 15, [M_.b, src.b], [pb])
                                s_ = so[nso % 3]
                                nso += 1
                                if ri == 0:
                                    C.tt(s_[:], pt[:, :512], dbc[:, o, :], ALU.add, [pb, dbc.b], [s_.b])
                                else:
                                    C.ts(s_[:], pt[:, :512], -1.0, ALU.mult, [pb], [s_.b])
                                C.dma("sp", spec_d[o, ri, mt * 128:(mt + 1) * 128, :], s_[:], reads=[s_.b])
                        pt, pb = C.ps("g", [0, 1, 2, 3])
                        for kt in range(16):
                            C.mm(pt[0:1, :512], altc[:, 0:1], hs[:, kt, :], kt == 0, kt == 15, [altc.b, hs.b], [pb])
                        C.tt(nyq[0:1, o, :], pt[0:1, :512], dbc[0:1, o, :], ALU.add, [pb, dbc.b], [], pw=[nyq.b])
                    C.barrier()
                z = SB(es, nc, "hyz", [128, 16, 512], BF16)
                Y = SB(es, nc, "hyY", [128, 16, 2, 512], BF16)
                sp_ = [SB(es, nc, "hysp%d" % i, [128, 2, 512], F32) for i in range(2)]
                tq = [SB(es, nc, "hytq%d" % i, [128, 512], F32) for i in range(2)]
                tq = tq + tq
                ynq = SB(es, nc, "hyynq", [1, 512], BF16)
                g1t = [SB(es, nc, "hyg1%d" % i, [128, 512], BF16) for i in range(2)]
                oo = [SB(es, nc, "hyoo%d" % i, [128, 512], BF16) for i in range(2)]
                nsp = 0
                ng = 0
                SC = 2.0 / 4096.0
                for b in range(NB):
                    C.dma("sp", z[:], hv_d[b].rearrange("(tt p) c -> p tt c", p=128), writes=[z.b])
                    for o in range(2):
                        for mt in range(16):
                            sp = sp_[nsp % 2]
                            nsp += 1
                            C.dma("sp", sp[:], spec_d[o, :, mt * 128:(mt + 1) * 128, :].rearrange("r p c -> p r c"), writes=[sp.b])
                            pc, pcb = C.ps("g", [0, 1, 2, 3])
                            for tt_ in range(16):
                                C.mm(pc[:, :512], Cm[:, tt_, mt * 128:(mt + 1) * 128], z[:, tt_, :], tt_ == 0, tt_ == 15, [Cm.b, z.b], [pcb])
                            pz, pzb = C.ps("g", [0, 1, 2, 3])
                            for tt_ in range(16):
                                C.mm(pz[:, :512], Sm[:, tt_, mt * 128:(mt + 1) * 128], z[:, tt_, :], tt_ == 0, tt_ == 15, [Sm.b, z.b], [pzb])
                            C.tt(tq[0][:], pc[:, :512], sp[:, 0, :], ALU.mult, [pcb, sp.b], [tq[0].b])
                            C.tt(tq[1][:], pz[:, :512], sp[:, 1, :], ALU.mult, [pzb, sp.b], [tq[1].b])
                            C.tt(Y[:, mt, 0, :], tq[0][:], tq[1][:], ALU.add, [tq[0].b, tq[1].b], [], eng="pool", pw=[Y.b])
                            C.tt(tq[2][:], pz[:, :512], sp[:, 0, :], ALU.mult, [pzb, sp.b], [tq[2].b])
                            C.tt(tq[3][:], pc[:, :512], sp[:, 1, :], ALU.mult, [pcb, sp.b], [tq[3].b])
                            C.tt(Y[:, mt, 1, :], tq[2][:], tq[3][:], ALU.subtract, [tq[2].b, tq[3].b], [], eng="pool", pw=[Y.b])
                            if mt == 0:
                                C.ts(Y[0:1, 0, 0, :], Y[0:1, 0, 0, :], 0.5, ALU.mult, [Y.b], [], eng="pool", pw=[Y.b])
                        pq, pqb = C.ps("g", [0, 1, 2, 3])
                        for tt_ in range(16):
                            C.mm(pq[0:1, :512], altc[:, 0:1], z[:, tt_, :], tt_ == 0, tt_ == 15, [altc.b, z.b], [pqb])
                        C.stt(ynq[:], pq[0:1, :512], 0.5, nyq[0:1, o, :], ALU.mult, ALU.mult, [pqb, nyq.b], [ynq.b])
                        if o == 0:
                            for tt_ in range(16):
                                g_ = g1t[ng % 2]
                                ng += 1
                                C.dma("sp", g_[:], hg1_d[b, tt_ * 128:(tt_ + 1) * 128, :], writes=[g_.b])
                                pt, pb = C.ps("o", [4, 5])
                                for mt in range(16):
                                    C.mm(pt[:, :512], Cm[:, mt, tt_ * 128:(tt_ + 1) * 128], Y[:, mt, 0, :], mt == 0, False, [Cm.b, Y.b], [pb])
                                    C.mm(pt[:, :512], Sm[:, mt, tt_ * 128:(tt_ + 1) * 128], Y[:, mt, 1, :], False, False, [Sm.b, Y.b], [pb])
                                C.mm(pt[:, :512], altr[0:1, 0:128], ynq[0:1, :], False, True, [altr.b, ynq.b], [pb])
                                C.stt(z[:, tt_, :], pt[:, :512], SC, g_[:], ALU.mult, ALU.mult, [pb, g_.b], [], pw=[z.b])
                        else:
                            for cm in range(4):
                                for tb in range(4):
                                    g_ = g1t[ng % 2]
                                    o_ = oo[ng % 2]
                                    ng += 1
                                    C.dma("sp", g_[:], hg2T_d[b, cm * 128:(cm + 1) * 128, tb * 512:(tb + 1) * 512], writes=[g_.b])
                                    pt, pb = C.ps("o", [4, 5])
                                    for mt in range(16):
                                        C.mm(pt[:, :512], Y[:, mt, 0, cm * 128:(cm + 1) * 128], Cm[:, mt, tb * 512:(tb + 1) * 512], mt == 0, False, [Cm.b, Y.b], [pb])
                                        C.mm(pt[:, :512], Y[:, mt, 1, cm * 128:(cm + 1) * 128], Sm[:, mt, tb * 512:(tb + 1) * 512], False, False, [Sm.b, Y.b], [pb])
                                    C.mm(pt[:, :512], ynq[0:1, cm * 128:(cm + 1) * 128], altr[0:1, :], False, True, [altr.b, ynq.b], [pb])
                                    C.stt(o_[:], pt[:, :512], SC, g_[:], ALU.mult, ALU.mult, [pb, g_.b], [o_.b])
                                    C.dma("sp", hyoT_d[b, cm, :, LC + tb * 512:LC + (tb + 1) * 512], o_[:], reads=[o_.b])

        nab_in = din("nab", [128, 8, 9, 128])
        naoT_d = dscr("naoT", [NB, 4, 128, T], BF16)

        def stage_l1_na():
            def r0(r):
                return min(max(r - 4, 0), 24)
            with ExitStack() as es:
                nab = SB(es, nc, "nab", [128, 8, 9, 128], BF16)
                idb = SB(es, nc, "naidb", [128, 128], BF16)
                kT = SB(es, nc, "nakT", [128, 4, T], BF16)
                qT = SB(es, nc, "naqT", [128, 4, L], BF16)
                V = SB(es, nc, "naV", [128, 18, 512], BF16)
                oT = [SB(es, nc, "naoT%d" % i, [128, L], BF16) for i in range(2)]
                PA = [SB(es, nc, "naPA%d" % i, [128, 4, 128], BF16) for i in range(3)]
                PB = [SB(es, nc, "naPB%d" % i, [128, 4, 128], BF16) for i in range(3)]
                rec = [SB(es, nc, "narec%d" % i, [128, 128], F32) for i in range(2)]
                C.dma("pool", nab[:], nab_in, writes=[nab.b])
                C.cp(idb[:], ident[:], [ident.b], [idb.b])
                nstep = 0
                no = 0
                nq = 0
                for b in range(NB):
                    C.dma("sp", kT[:], nak_d[b].rearrange("m p s -> p m s"), writes=[kT.b])
                    C.dma("sp", qT[:], naq_d[b].rearrange("m p s -> p m s"), writes=[qT.b])
                    C.dma("sp", V[:], nav_d[b].rearrange("(t p) c -> p t c", p=128), writes=[V.b])
                    for m in range(4):
                        o_ = oT[no % 2]
                        no += 1
                        steps = []
                        for qt in range(16):
                            for hh in range(2):
                                tiles = [(0, None), (1, None)]
                                for kp in range(16):
                                    val = [[r0(2 * qt + bq) <= 2 * kp + a < r0(2 * qt + bq) + 8 for bq in range(2)] for a in range(2)]
                                    if any(val[0]) or any(val[1]):
                                        di = (2 * kp - 2 * qt + 6) // 2
                                        if not (all(val[0]) and all(val[1])):
                                            di = 7 if di == 1 else 8
                                        tiles.append((2 + kp, di))
                                steps.append((qt, hh, tiles))

                        def emit_s(qt, hh, tiles):
                            h = 2 * m + hh
                            p0 = hh * 64
                            qc = slice(qt * 128, (qt + 1) * 128)
                            banks = [C.ps("s", [2, 3, 4, 5]), C.ps("s", [2, 3, 4, 5])]
                            for ti_, (tk, di) in enumerate(tiles):
                                pt_, ptb_ = banks[ti_ // 4]
                                dst = pt_[:, (ti_ % 4) * 128:(ti_ % 4 + 1) * 128]
                                C.mm(dst, kT[p0:p0 + 64, m, tk * 128:(tk + 1) * 128], qT[p0:p0 + 64, m, qc], True, di is None,
                                     [kT.b, qT.b], [ptb_])
                                if di is not None:
                                    C.mm(dst, idb[:], nab[:, h, di, :], False, True, [idb.b, nab.b], [ptb_])
                            return banks
                        cur = emit_s(*steps[0])
                        for si, (qt, hh, tiles) in enumerate(steps):
                            h = 2 * m + hh
                            p0 = hh * 64
                            qc = slice(qt * 128, (qt + 1) * 128)
                            nxt = emit_s(*steps[si + 1]) if si + 1 < len(steps) else None
                            num, numb = C.psb[0 if qt % 2 == 0 else 6]
                            den, denb = C.psb[1 if qt % 2 == 0 else 7]
                            pa, pb_ = PA[nstep % 3], PB[nstep % 3]
                            nstep += 1
                            nA = min(4, len(tiles))
                            nB = len(tiles) - nA
                            C.act(pa[:, :nA, :], cur[0][0][:, :nA * 128].rearrange("p (a c) -> p a c", c=128), AF.Exp, [cur[0][1]], [pa.b])
                            if nB > 0:
                                C.act(pb_[:, :nB, :], cur[1][0][:, :nB * 128].rearrange("p (a c) -> p a c", c=128), AF.Exp, [cur[1][1]], [pb_.b])
                            for ti_, (tk, di) in enumerate(tiles):
                                P_ = pa if ti_ < 4 else pb_
                                pv = P_[:, ti_ % 4, :]
                                first, last = ti_ == 0, ti_ == len(tiles) - 1
                                C.mm(num[p0:p0 + 64, 0:128], V[:, tk, h * 64:(h + 1) * 64], pv, first, last, [V.b, P_.b], [numb])
                                C.mm(den[p0:p0 + 64, 0:128], ones_bf[:, 0:64], pv, first, last, [ones_bf.b, P_.b], [denb])
                            if hh == 1:
                                r_ = rec[nq % 2]
                                nq += 1
                                C.recip(r_[:], den[:, 0:128], [denb], [r_.b])
                                C.tt(o_[:, qc], num[:, 0:128], r_[:], ALU.mult, [numb, r_.b], [], pw=[o_.b])
                            cur = nxt
                        C.dma("sp", naoT_d[b, m, :, LC:T], o_[:], reads=[o_.b])

        od_w_out = din("od_w_out", [D, D])
        moe_router_in = din("moe_router", [D, 8])
        moe_w1 = din("moe_w1", [8, D, 3584])
        moe_w3 = din("moe_w3", [8, D, 3584])
        moe_w2 = din("moe_w2", [8, 3584, D])
        rw_d = dscr("rw", [8, NB, L], F32)
        AX = mybir.AxisListType

        def router_hook(es):
            Wr = SB(es, nc, "rWr", [128, KT, 8], F32)
            h2f = SB(es, nc, "rh2f", [128, KT, 512], F32)
            lgs = SB(es, nc, "rlgs", [8, 512], F32)
            lt = SB(es, nc, "rlt", [128, 4, 8], F32)
            eq = SB(es, nc, "req", [128, 4, 8], F32)
            msk = SB(es, nc, "rmsk", [128, 4, 8], F32)
            ex = SB(es, nc, "rex", [128, 4, 8], F32)
            m1 = SB(es, nc, "rm1", [128, 4], F32)
            m2 = SB(es, nc, "rm2", [128, 4], F32)
            dn = SB(es, nc, "rdn", [128, 4], F32)
            wts = SB(es, nc, "rwts", [8, 512], F32)
            C.dma("sp", Wr[:], moe_router_in.rearrange("(k p) e -> p k e", p=128), writes=[Wr.b])

            def hook(b, bi, s0, n, xt, h):
                j5 = b
                nch = n // 128
                for k in range(KT):
                    if k % 2 == 0:
                        C.act(h2f[:, k, :n], xt[:, k, :n], AF.Identity, [xt.b, A1.b, modT.b], [], scale=A1[:, 1, 1, k, j5:j5 + 1],
                              bias=modvec(1, 3, k, j5), pw=[h2f.b])
                    else:
                        C.ts(h2f[:, k, :n], xt[:, k, :n], A1[:, 1, 1, k, j5:j5 + 1], ALU.mult, [xt.b, A1.b, modT.b], [],
                             s2=modvec(1, 3, k, j5), op1=ALU.add, pw=[h2f.b])
                pl, plb = C.ps("g", [0, 1, 2, 3])
                for k in range(KT):
                    C.mm(pl[0:8, :n], Wr[:, k, :], h2f[:, k, :n], k == 0, k == KT - 1, [Wr.b, h2f.b], [plb])
                C.cp(lgs[:, :n], pl[0:8, :n], [plb], [lgs.b])
                pt, ptb = C.ps("g", [0, 1, 2, 3])
                for ch in range(nch):
                    C.tr(pt[:, ch * 8:(ch + 1) * 8], lgs[0:8, ch * 128:(ch + 1) * 128], ident[0:8, 0:8], [lgs.b, ident.b], [ptb])
                ltv, eqv, mkv, exv = lt[:, :nch, :], eq[:, :nch, :], msk[:, :nch, :], ex[:, :nch, :]
                C.cp(ltv, pt[:, :nch * 8].rearrange("p (c e) -> p c e", e=8), [ptb], [lt.b])
                bc = lambda v: v[:, :nch].unsqueeze(2).broadcast_to([128, nch, 8])
                C.op("dve", lambda e: e.tensor_reduce(out=m1[:, :nch], in_=ltv, axis=AX.X, op=ALU.max), [lt.b], [m1.b])
                C.tt(eqv, ltv, bc(m1), ALU.is_equal, [lt.b, m1.b], [eq.b])
                C.stt(mkv, eqv, -1e30, ltv, ALU.mult, ALU.add, [eq.b, lt.b], [msk.b])
                C.op("dve", lambda e: e.tensor_reduce(out=m2[:, :nch], in_=mkv, axis=AX.X, op=ALU.max), [msk.b], [m2.b])
                C.tt(eqv, ltv, bc(m2), ALU.is_ge, [lt.b, m2.b], [eq.b])
                C.tt(mkv, ltv, bc(m1), ALU.subtract, [lt.b, m1.b], [msk.b])
                C.act(exv, mkv, AF.Exp, [msk.b], [ex.b])
                C.tt(exv, exv, eqv, ALU.mult, [ex.b, eq.b], [ex.b])
                C.op("dve", lambda e: e.tensor_reduce(out=dn[:, :nch], in_=exv, axis=AX.X, op=ALU.add), [ex.b], [dn.b])
                C.recip(dn[:, :nch], dn[:, :nch], [dn.b], [dn.b])
                C.tt(exv, exv, bc(dn), ALU.mult, [ex.b, dn.b], [ex.b])
                pw_, pwb = C.ps("g", [0, 1, 2, 3])
                for ch in range(nch):
                    C.tr(pw_[0:8, ch * 128:(ch + 1) * 128], ex[:, ch, :], ident[:], [ex.b, ident.b], [pwb])
                C.cp(wts[:, :n], pw_[0:8, :n], [pwb], [wts.b])
                C.dma("sp", rw_d[:, b, s0 - LC:s0 - LC + n], wts[:, :n], reads=[wts.b])
            return hook

        def stage_final():
            with ExitStack() as es:
                xt2 = [SB(es, nc, "fx%d" % i, [128, KT, 512], F32) for i in range(2)]
                sq = SB(es, nc, "fsq", [128, KT, 512], BF16)
                rstd = SB(es, nc, "frstd", [128, 512], F32)
                hf = [SB(es, nc, "fh%d" % i, [128, KT, 512], F32) for i in range(2)]
                ot = [SB(es, nc, "fo%d" % i, [128, D], F32) for i in range(3)]
                n = 0
                no = 0
                fscale = VecFn(lambda k: gvec[:, 4, k:k + 1], [gvec.b])
                itemsf = [(b, bi) for b in range(NB) for bi in range(1, len(BLOCKS))]

                def load_xf(idx):
                    b, bi = itemsf[idx]
                    s0, nn = BLOCKS[bi]
                    C.dma("sp", xt2[idx % 2][:, :, :nn], xT_d[b].rearrange("(k p) s -> p k s", p=128)[:, :, s0:s0 + nn],
                          reads=[xT_buf[b][bi]], writes=[xt2[idx % 2].b])
                load_xf(0)
                for b in range(NB):
                    for bi in range(1, len(BLOCKS)):
                        s0, nn = BLOCKS[bi]
                        xt = xt2[n % 2]
                        h = hf[n % 2]
                        n += 1
                        if n < len(itemsf):
                            load_xf(n)
                        norm_block(xt, nn, sq, rstd, fscale, None, h)
                        for tt_ in range(nn // 128):
                            o = ot[no % 3]
                            no += 1
                            for kk in range(2):
                                pt, pb = C.ps("g", [0, 1, 2, 3])
                                for k4 in range(4):
                                    k = kk * 4 + k4
                                    C.tr(pt[:, k4 * 128:(k4 + 1) * 128], h[:, k, tt_ * 128:(tt_ + 1) * 128], ident[:],
                                         [h.b, ident.b], [pb])
                                dst = o[:, kk * 512:(kk + 1) * 512]
                                C.cp(dst, pt[:], [pb], [], eng=("dve" if kk == 0 else "act"), pw=[o.b])
                            t0 = s0 - LC + tt_ * 128
                            C.dma("sp", out_d[b, t0:t0 + 128, :], o[:], reads=[o.b])

        def xT_view(b, s0, n):
            return xT_d[b].rearrange("(k p) s -> p k s", p=128)[:, :, s0:s0 + n]

        stages = [("l0_inproj", stage_l0_inproj), ("l0_s5", stage_l0_mix),
                  ("l0_out", stage_out_norm2(0, ev_w_out, s5T_d, attnT_d, ALLB)),
                  ("l0_ffn1", ffn_multi(0, [(ffn_w1[:, 0:1792], ffn_w3[:, 0:1792], ffn_w2[0:1792, :], None),
                                            (ffn_w1[:, 1792:3584], ffn_w3[:, 1792:3584], ffn_w2[1792:3584, :], None)], ALLB)),
                  ("l1_inproj", stage_l1_inproj),
                  ("l1_hyena", stage_l1_hyena),
                  ("l1_na", stage_l1_na),
                  ("l1_out", stage_out_norm2(1, od_w_out, hyoT_d, naoT_d, LATB, after=router_hook)),
                  ]
        moe_passes = []
        for e_ in range(8):
            for hf_ in range(2):
                hs_ = slice(hf_ * 1792, (hf_ + 1) * 1792)
                moe_passes.append((moe_w1[e_][:, hs_], moe_w3[e_][:, hs_], moe_w2[e_][hs_, :], rw_d[e_]))
        stages.append(("moe", ffn_multi(1, moe_passes, LATB)))
        for name, fn in stages:
            fn()
            C.barrier()
            if stop_after == name:
                break
        if stop_after is None:
            stage_final()
        C.finish()
    return nc, C


def _blk_index(s):
    for i, (s0, n) in enumerate(BLOCKS):
        if s0 <= s < s0 + n:
            return i
    raise ValueError(s)


def host_inputs(inp, core):
    b0 = core * NB
    f = lambda a: np.ascontiguousarray(a, dtype=np.float32)
    m = {}
    m["x"] = f(inp["x"][b0:b0 + NB])
    m["ctx"] = f(inp["ctx"][b0:b0 + NB])
    c5 = np.concatenate([inp["c"][b0:b0 + NB], inp["c_ctx"][None, :]], 0)
    m["cT"] = f(c5.reshape(5, KT, 128).transpose(2, 1, 0))
    m["mod_w"] = f(inp["mod_w"])
    m["mod_bT"] = f(inp["mod_b"].reshape(2, 48, 128).transpose(2, 0, 1))
    gv = np.stack([inp["norm1_g"][0], inp["norm1_g"][1], inp["norm2_g"][0], inp["norm2_g"][1], inp["final_g"]], 0)
    m["gvecT"] = f(gv.reshape(5, KT, 128).transpose(2, 0, 1))
    m["ident"] = np.eye(128, dtype=np.float32)
    m["ev_w_in"] = f(inp["ev_w_in"][0])
    m["rope_cs"] = _rope_tables()
    gq, gk = inp["gqa_q_g"][0], inp["gqa_k_g"][0]
    dd = np.arange(64)
    partner = np.where((dd // 16) % 2 == 0, dd + 16, dd - 16)
    G, N, P = 32, 64, 16
    sm = np.zeros((128, 3, 2, 16), np.float32)
    bpad = np.zeros((128, 2, 2, 16, 128), np.float32)
    cpad = np.zeros((128, 2, 2, 16, 128), np.float32)
    lam_re, lam_im, log_dt = inp["s5_lam_re"][0], inp["s5_lam_im"][0], inp["s5_log_dt"][0]
    b_ri = [inp["s5_b_re"][0], inp["s5_b_im"][0]]
    c_ri = [inp["s5_c_re"][0], inp["s5_c_im"][0]]
    for j in range(16):
        for gl in range(2):
            g = 2 * j + gl
            sp = slice(gl * 64, gl * 64 + 64)
            cc = slice((g % 8) * 16, (g % 8) * 16 + 16)
            for d in range(2):
                sm[sp, 0, d, j] = lam_re[d, g]
                sm[sp, 1, d, j] = lam_im[d, g]
                sm[sp, 2, d, j] = log_dt[d, g]
                for ri in range(2):
                    bpad[cc, ri, d, j, sp] = b_ri[ri][d, g].T
                    cpad[sp, ri, d, j, cc] = c_ri[ri][d, g].T
    m["s5_sm"], m["s5_bpad"], m["s5_cpad"] = sm, bpad, cpad
    dpad = np.zeros((128, 4, 128), np.float32)
    dd_ = inp["s5_d"][0].reshape(4, 128)
    for kt in range(4):
        dpad[np.arange(128), kt, np.arange(128)] = dd_[kt]
    m["s5_dpad"] = dpad
    m["s5_w_glu"] = f(inp["s5_w_glu"][0])
    m["ev_w_out"] = f(inp["ev_w_out"][0])
    m["ffn_w1"], m["ffn_w3"], m["ffn_w2"] = f(inp["ffn_w1"][0]), f(inp["ffn_w3"][0]), f(inp["ffn_w2"][0])
    m["od_w_in"] = f(inp["od_w_in"][0])
    cw = np.concatenate([inp["hy_conv_w"][0], inp["hy_conv_b"][0][None, :]], 0)
    m["hy_cw"] = f(cw.reshape(4, 12, 128).transpose(2, 1, 0))
    m["dftC"], m["dftS"] = _dft_tables()
    m["hyz"], m["hynt"] = _hyena_pos()
    m["hy_w1"], m["hy_w2"], m["hy_w3"] = f(inp["hy_w1"][0]), f(inp["hy_w2"][0]), f(inp["hy_w3"][0])
    m["hy_fb"] = f(np.stack([inp["hy_freq"][0], inp["hy_b1"][0], inp["hy_b2"][0]], 1))
    m["hy_dec"] = f(np.broadcast_to(inp["hy_decay"][0][None, :], (128, 2048)))
    m["hy_dbc"] = f(np.broadcast_to(inp["hy_d"][0][None], (128, 2, 512)))
    alt = np.zeros((128, 513), np.float32)
    alt[:, 0] = (-1.0) ** np.arange(128)
    alt[:, 1:] = ((-1.0) ** np.arange(512))[None, :]
    m["alt"] = alt
    m["nab"] = _na_bias(inp["na_rpb"][0])
    m["od_w_out"] = f(inp["od_w_out"][0])
    m["moe_router"] = f(inp["moe_router"][0])
    m["moe_w1"], m["moe_w3"], m["moe_w2"] = f(inp["moe_w1"][0]), f(inp["moe_w3"][0]), f(inp["moe_w2"][0])
    m["gqk"] = f(np.stack([np.tile(gq, 2), np.tile(gq[partner], 2), np.tile(gk, 2), np.tile(gk[partner], 2)], 1))
    return m


_TAB = {}


def _dft_tables():
    if "dft" not in _TAB:
        k = np.arange(2048, dtype=np.int64)
        km = (k[:, None] * k[None, :]) % 4096
        ang = 2.0 * np.pi * km.astype(np.float64) / 4096.0
        c = np.cos(ang).astype(np.float32).reshape(16, 128, 2048).transpose(1, 0, 2)
        s_ = np.sin(ang).astype(np.float32).reshape(16, 128, 2048).transpose(1, 0, 2)
        _TAB["dft"] = (np.ascontiguousarray(c), np.ascontiguousarray(s_))
    return _TAB["dft"]


def _hyena_pos():
    k = np.arange(L, dtype=np.float32)
    t = (k / np.float32(L - 1)).astype(np.float32)
    bands = np.linspace(1e-4, 15, 16, dtype=np.float32)
    ang = (np.float32(2.0 * np.pi / L) * k[:, None] * bands[None, :]).astype(np.float32)
    z = np.concatenate([t[:, None], np.cos(ang), -np.sin(ang)], -1).astype(np.float32)
    nt = (-t).reshape(16, 128).T
    return np.ascontiguousarray(z.T), np.ascontiguousarray(nt, dtype=np.float32)


def _na_bias(rpb):
    NEG = np.float32(-30000.0)
    out = np.full((128, 8, 9, 128), NEG, np.float32)
    ck = np.arange(64)[:, None]
    cq = np.arange(64)[None, :]
    cs = np.clip(cq - 8, 0, 48)
    inwin = (ck >= cs) & (ck < cs + 16)
    dc = np.clip(ck - cq + 15, 0, 30)
    for di in range(7):
        delta = 2 * di - 6
        for a in range(2):
            for bq in range(2):
                dr = delta + a - bq + 7
                if dr < 0 or dr > 14:
                    continue
                for h in range(8):
                    blk = np.where(inwin, rpb[h, dr][dc], NEG)
                    out[a * 64:(a + 1) * 64, h, di, bq * 64:(bq + 1) * 64] = blk
    out[:, :, 7, :] = out[:, :, 1, :]
    out[0:64, :, 7, 64:128] = NEG
    out[0:64, :, 8, 64:128] = out[0:64, :, 5, 64:128]
    return out


def _rope_tables():
    s = np.arange(T)
    t = np.maximum(s - LC, 0)
    d = np.arange(64)
    a = d // 32
    fq = d % 16
    inv = (10000.0 ** (-(np.arange(16, dtype=np.float32)) / np.float32(16))).astype(np.float32)
    pos = np.where(a[:, None] == 0, (t // 64)[None, :], (t % 64)[None, :]).astype(np.float32)
    ang = (pos * inv[fq][:, None]).astype(np.float32)
    ang = np.where(s[None, :] < LC, np.float32(0), ang)
    cs = np.stack([np.cos(ang.astype(np.float64)), np.sin(ang.astype(np.float64))], 1)
    return np.ascontiguousarray(np.tile(cs, (2, 1, 1)), dtype=np.float32)


_CACHE = {}


def kernel(**inputs):
    inp = {k: np.asarray(v) for k, v in inputs.items()}
    if "nc" not in _CACHE:
        _CACHE["nc"] = build_program()[0]
    nc = _CACHE["nc"]
    in_maps = [host_inputs(inp, c) for c in range(NCORES)]
    res = run_bass_kernel_spmd(nc, in_maps, core_ids=list(range(NCORES)))
    out = np.concatenate([np.asarray(r["out"]) for r in res.results], axis=0)
    return out.astype(np.float32)
```
